# Optimizing a Trainium2 kernel written in Bass

```python
import jax, jax.numpy as jnp
from jax import lax
import numpy as np

D_MODEL = 1024
BATCH = 8
SEQ = 4096
DEPTH = 1

EPS = 1e-6
ROPE_THETA = 10000.0
NEG_INF = -1e30

SWA_HEADS = 8
SWA_KV_HEADS = 2
SWA_GROUP = SWA_HEADS // SWA_KV_HEADS
SWA_HEAD_DIM = 64
SWA_WINDOW = 128
SWA_BLOCK = 128

MLA_HEADS = 8
MLA_Q_RANK = 256
MLA_KV_RANK = 128
MLA_NOPE_DIM = 64
MLA_ROPE_DIM = 32
MLA_V_DIM = 64
MLA_QK_DIM = MLA_NOPE_DIM + MLA_ROPE_DIM
MLA_Q_BLOCK = 128

SWA_WIDTH = SWA_HEADS * SWA_HEAD_DIM
MLA_WIDTH = MLA_HEADS * MLA_V_DIM
MIX_WIDTH = SWA_WIDTH + MLA_WIDTH

IN_SIZES = (SWA_HEADS * SWA_HEAD_DIM,
            SWA_KV_HEADS * SWA_HEAD_DIM,
            SWA_KV_HEADS * SWA_HEAD_DIM,
            MLA_Q_RANK,
            MLA_KV_RANK,
            MLA_ROPE_DIM)
IN_WIDTH = int(sum(IN_SIZES))
IN_SPLITS = tuple(int(v) for v in np.cumsum(IN_SIZES)[:-1])

PEER_HEADS = 8
PEER_N_KEYS = 128
PEER_EXPERTS = PEER_N_KEYS * PEER_N_KEYS
PEER_QUERY_DIM = 256
PEER_HALF_DIM = PEER_QUERY_DIM // 2
PEER_TOPK = 16
PEER_TOKEN_BLOCK = 128

N_MOD = 6

kernel_name = "hymba_swa_mla_peer_adaln_encoder"


def rmsnorm(x, gain=None):
    xf = x.astype(jnp.float32)
    y = xf * lax.rsqrt(jnp.mean(xf * xf, axis=-1, keepdims=True) + EPS)
    if gain is not None:
        y = y * gain.astype(jnp.float32)
    return y.astype(x.dtype)


def modulate(h, shift, scale):
    return h * (1.0 + scale[:, None, :]) + shift[:, None, :]


def rope_tables(seq, dim):
    inv = 1.0 / (ROPE_THETA ** (jnp.arange(0, dim, 2, dtype=jnp.float32) / dim))
    ang = jnp.arange(seq, dtype=jnp.float32)[:, None] * inv[None, :]
    return jnp.cos(ang), jnp.sin(ang)


def apply_rope(x, cos, sin):
    xf = x.astype(jnp.float32)
    half = xf.shape[-1] // 2
    x1, x2 = xf[..., :half], xf[..., half:]
    c = cos[None, :, None, :]
    s = sin[None, :, None, :]
    return jnp.concatenate([x1 * c - x2 * s, x2 * c + x1 * s], axis=-1).astype(x.dtype)


def windowed_gqa(q, k, v, sink):
    B, S = q.shape[0], q.shape[1]
    nb = S // SWA_BLOCK
    cos, sin = rope_tables(S, SWA_HEAD_DIM)
    q = apply_rope(q, cos, sin)
    k = apply_rope(k, cos, sin)
    qb = q.reshape(B, nb, SWA_BLOCK, SWA_KV_HEADS, SWA_GROUP, SWA_HEAD_DIM)
    pad = ((0, 0), (SWA_BLOCK, SWA_BLOCK), (0, 0), (0, 0))
    kp = jnp.pad(k, pad).reshape(B, nb + 2, SWA_BLOCK, SWA_KV_HEADS, SWA_HEAD_DIM)
    vp = jnp.pad(v, pad).reshape(B, nb + 2, SWA_BLOCK, SWA_KV_HEADS, SWA_HEAD_DIM)
    kb = jnp.concatenate([kp[:, :-2], kp[:, 1:-1], kp[:, 2:]], axis=2)
    vb = jnp.concatenate([vp[:, :-2], vp[:, 1:-1], vp[:, 2:]], axis=2)
    scores = jnp.einsum('bnqhgd,bnkhd->bnhgqk', qb, kb).astype(jnp.float32) * (SWA_HEAD_DIM ** -0.5)
    q_off = jnp.arange(SWA_BLOCK)[:, None]
    k_off = jnp.arange(3 * SWA_BLOCK)[None, :] - SWA_BLOCK
    band = jnp.abs(k_off - q_off) <= SWA_WINDOW
    k_pos = (jnp.arange(nb) * SWA_BLOCK)[:, None] + k_off
    valid = (k_pos >= 0) & (k_pos < S)
    mask = band[None, :, :] & valid[:, None, :]
    scores = jnp.where(mask[None, :, None, None], scores, NEG_INF)
    sink_l = jnp.broadcast_to(sink.astype(jnp.float32).reshape(1, 1, SWA_KV_HEADS, SWA_GROUP, 1, 1),
                              scores.shape[:-1] + (1,))
    probs = jax.nn.softmax(jnp.concatenate([scores, sink_l], axis=-1), axis=-1)[..., :-1]
    out = jnp.einsum('bnhgqk,bnkhd->bnqhgd', probs.astype(v.dtype), vb)
    return out.reshape(B, S, SWA_WIDTH)


def latent_attention(q_lat, kv_lat, k_rope_raw, q_norm, w_q_up, kv_norm, w_kv_up):
    B, S = q_lat.shape[0], q_lat.shape[1]
    nb = S // MLA_Q_BLOCK
    cos, sin = rope_tables(S, MLA_ROPE_DIM)
    q = (rmsnorm(q_lat, q_norm) @ w_q_up).reshape(B, S, MLA_HEADS, MLA_QK_DIM)
    q_nope = q[..., :MLA_NOPE_DIM]
    q_rope = apply_rope(q[..., MLA_NOPE_DIM:], cos, sin)
    kv = (rmsnorm(kv_lat, kv_norm) @ w_kv_up).reshape(B, S, MLA_HEADS, MLA_NOPE_DIM + MLA_V_DIM)
    k_nope = kv[..., :MLA_NOPE_DIM]
    v = kv[..., MLA_NOPE_DIM:]
    k_rope = apply_rope(k_rope_raw[:, :, None, :], cos, sin)[:, :, 0, :]
    scale = MLA_QK_DIM ** -0.5
    qn_blocks = q_nope.reshape(B, nb, MLA_Q_BLOCK, MLA_HEADS, MLA_NOPE_DIM).transpose(1, 0, 2, 3, 4)
    qr_blocks = q_rope.reshape(B, nb, MLA_Q_BLOCK, MLA_HEADS, MLA_ROPE_DIM).transpose(1, 0, 2, 3, 4)

    def attend(blk):
        qn, qr = blk
        s = (jnp.einsum('bqhd,bkhd->bhqk', qn, k_nope)
             + jnp.einsum('bqhd,bkd->bhqk', qr, k_rope)).astype(jnp.float32) * scale
        p = jax.nn.softmax(s, axis=-1).astype(v.dtype)
        return jnp.einsum('bhqk,bkhd->bqhd', p, v)

    out = lax.map(attend, (qn_blocks, qr_blocks))
    return out.transpose(1, 0, 2, 3, 4).reshape(B, S, MLA_WIDTH)


def peer(h, w_query, sub_keys, expert_u, expert_v):
    B, S, D = h.shape
    blocks = h.reshape((B * S) // PEER_TOKEN_BLOCK, PEER_TOKEN_BLOCK, D)

    def block(xb):
        q = (xb @ w_query).reshape(PEER_TOKEN_BLOCK, PEER_HEADS, 2, PEER_HALF_DIM)
        s = jnp.einsum('chpd,hpnd->chpn', q, sub_keys).astype(jnp.float32)
        top_s, top_i = lax.top_k(s, PEER_TOPK)
        cand_s = (top_s[:, :, 0, :, None] + top_s[:, :, 1, None, :]).reshape(
            PEER_TOKEN_BLOCK, PEER_HEADS, PEER_TOPK * PEER_TOPK)
        cand_i = (top_i[:, :, 0, :, None] * PEER_N_KEYS + top_i[:, :, 1, None, :]).reshape(
            PEER_TOKEN_BLOCK, PEER_HEADS, PEER_TOPK * PEER_TOPK)
        best_s, pos = lax.top_k(cand_s, PEER_TOPK)
        idx = jnp.take_along_axis(cand_i, pos, axis=-1)
        g = jax.nn.softmax(best_s, axis=-1)
        u = expert_u[idx]
        act = jax.nn.gelu(jnp.einsum('cd,chkd->chk', xb, u).astype(jnp.float32), approximate=False)
        w = (g * act).astype(xb.dtype)
        return jnp.einsum('chk,chkd->cd', w, expert_v[idx])

    return lax.map(block, blocks).reshape(B, S, D)


def setup_inputs(seed: int = 0) -> dict:
    key = jax.random.key(seed)
    ks = jax.random.split(key, 20)
    f32 = jnp.float32
    nrm = lambda k, shape, s: jax.random.normal(k, shape, f32) * s
    L, D = DEPTH, D_MODEL
    return {
        "x": nrm(ks[0], (BATCH, SEQ, D), 1.0),
        "c": nrm(ks[1], (BATCH, D), 1.0),
        "w_ada": nrm(ks[2], (L, D, N_MOD * D), D ** -0.5),
        "b_ada": nrm(ks[3], (L, N_MOD * D), 0.01),
        "w_in": nrm(ks[4], (L, D, IN_WIDTH), D ** -0.5),
        "swa_sink": nrm(ks[5], (L, SWA_HEADS), 0.5),
        "mla_q_norm": 1.0 + nrm(ks[6], (L, MLA_Q_RANK), 0.02),
        "w_mla_q_up": nrm(ks[7], (L, MLA_Q_RANK, MLA_HEADS * MLA_QK_DIM), MLA_Q_RANK ** -0.5),
        "mla_kv_norm": 1.0 + nrm(ks[8], (L, MLA_KV_RANK), 0.02),
        "w_mla_kv_up": nrm(ks[9], (L, MLA_KV_RANK, MLA_HEADS * (MLA_NOPE_DIM + MLA_V_DIM)), MLA_KV_RANK ** -0.5),
        "out_norm_swa": 1.0 + nrm(ks[10], (L, SWA_WIDTH), 0.02),
        "out_norm_mla": 1.0 + nrm(ks[11], (L, MLA_WIDTH), 0.02),
        "w_out": nrm(ks[12], (L, MIX_WIDTH, D), MIX_WIDTH ** -0.5),
        "w_peer_query": nrm(ks[13], (L, D, PEER_HEADS * PEER_QUERY_DIM), D ** -0.5),
        "peer_sub_keys": nrm(ks[14], (L, PEER_HEADS, 2, PEER_N_KEYS, PEER_HALF_DIM), PEER_HALF_DIM ** -0.5),
        "peer_expert_u": nrm(ks[15], (L, PEER_EXPERTS, D), D ** -0.5),
        "peer_expert_v": nrm(ks[16], (L, PEER_EXPERTS, D), PEER_HEADS ** -0.5),
        "final_norm": 1.0 + nrm(ks[17], (D,), 0.02),
    }


def reference(x, c, w_ada, b_ada, w_in, swa_sink, mla_q_norm, w_mla_q_up, mla_kv_norm, w_mla_kv_up,
              out_norm_swa, out_norm_mla, w_out, w_peer_query, peer_sub_keys, peer_expert_u,
              peer_expert_v, final_norm):
    B, S, D = x.shape
    c_act = jax.nn.silu(c)
    for l in range(DEPTH):
        mod = c_act @ w_ada[l] + b_ada[l]
        sh_a, sc_a, g_a, sh_f, sc_f, g_f = jnp.split(mod, N_MOD, axis=-1)

        h = modulate(rmsnorm(x), sh_a, sc_a)
        proj = h @ w_in[l]
        qa, ka, va, q_lat, kv_lat, k_rope_raw = jnp.split(proj, IN_SPLITS, axis=-1)
        o_a = windowed_gqa(qa.reshape(B, S, SWA_HEADS, SWA_HEAD_DIM),
                           ka.reshape(B, S, SWA_KV_HEADS, SWA_HEAD_DIM),
                           va.reshape(B, S, SWA_KV_HEADS, SWA_HEAD_DIM),
                           swa_sink[l])
        o_b = latent_attention(q_lat, kv_lat, k_rope_raw, mla_q_norm[l], w_mla_q_up[l],
                               mla_kv_norm[l], w_mla_kv_up[l])
        mixed = jnp.concatenate([rmsnorm(o_a, out_norm_swa[l]), rmsnorm(o_b, out_norm_mla[l])], axis=-1)
        x = x + g_a[:, None, :] * (mixed @ w_out[l])

        h = modulate(rmsnorm(x), sh_f, sc_f)
        x = x + g_f[:, None, :] * peer(h, w_peer_query[l], peer_sub_keys[l], peer_expert_u[l], peer_expert_v[l])
    return rmsnorm(x, final_norm)
```

```python
from contextlib import ExitStack
import numpy as np
import concourse.bass as bass
import concourse.mybir as mybir
from concourse.bass_utils import run_bass_kernel_spmd

F32 = mybir.dt.float32
BF16 = mybir.dt.bfloat16
I32 = mybir.dt.int32
U32 = mybir.dt.uint32
AF = mybir.ActivationFunctionType
ALU = mybir.AluOpType
AX = mybir.AxisListType

S = 4096
D = 1024
NB = S // 128
EPS = 1e-6
NEG = -1e30


class Prog:
    ENGS = ("pe", "act", "dve", "pool", "sp")

    def __init__(self, nc, stack):
        self.nc = nc
        self.stack = stack
        self.ops = {e: [] for e in self.ENGS}
        self.sem = {}
        self.cnt = {}
        self.waited = {e: {} for e in self.ENGS}
        self.lastw = {}
        self.reads = {}
        self.defer = None
        for e in self.ENGS:
            self.newsem("c_" + e)

    def capture(self, fn):
        self.defer = []
        fn()
        lst, self.defer = self.defer, None
        return lst

    def replay(self, thunks):
        for t in thunks:
            self.op(*t)

    def newsem(self, name):
        if name not in self.sem:
            self.sem[name] = self.stack.enter_context(self.nc.semaphore(name))
            self.cnt[name] = 0
        return name

    def op(self, eng, fn, reads=(), writes=(), dma_sem=None):
        if self.defer is not None:
            self.defer.append((eng, fn, tuple(reads), tuple(writes), dma_sem))
            return None
        waits = {}

        def need(tok):
            if tok is None:
                return
            s, v = tok
            if waits.get(s, 0) < v:
                waits[s] = v

        for k in reads:
            need(self.lastw.get(k))
        for k in writes:
            need(self.lastw.get(k))
            for s, v in self.reads.get(k, {}).items():
                need((s, v))
        w = []
        for s, v in waits.items():
            if self.waited[eng].get(s, 0) < v:
                self.waited[eng][s] = v
                w.append((s, v))
        if dma_sem is None:
            s = "c_" + eng
            inc = 1
        else:
            s = self.newsem(dma_sem)
            inc = 16
        self.cnt[s] += inc
        tok = (s, self.cnt[s])
        for k in writes:
            self.lastw[k] = tok
            self.reads[k] = {}
        for k in reads:
            self.reads.setdefault(k, {})[s] = self.cnt[s]
        self.ops[eng].append((fn, w, s, inc))
        return tok

    def flush(self):
        nc = self.nc
        final = dict(self.cnt)
        with nc.Block() as block:
            def mk(engname):
                def body(eng):
                    for fn, w, s, inc in self.ops[engname]:
                        for ws, wv in w:
                            eng.wait_ge(self.sem[ws], wv)
                        fn(eng).then_inc(self.sem[s], inc)
                    for s, v in final.items():
                        if v > 0 and self.waited[engname].get(s, 0) < v:
                            eng.wait_ge(self.sem[s], v)
                            self.waited[engname][s] = v
                return body
            block.tensor(mk("pe"))
            block.scalar(mk("act"))
            block.vector(mk("dve"))
            block.gpsimd(mk("pool"))
            block.sync(mk("sp"))
        self.ops = {e: [] for e in self.ENGS}


def build(n_blocks=NB, do_attn=True, do_peer=True, n_chunks=8):
    nc = bass.Bass("TRN2", target_bir_lowering=False)
    dt_in = lambda name, shape: nc.dram_tensor(name, list(shape), F32, kind="ExternalInput").ap()
    x = dt_in("x", [S, D])
    c = dt_in("c", [8, 128])
    w_ada = dt_in("w_ada", [D, 6 * D])
    b_ada = dt_in("b_ada", [1, 6 * D])
    w_in = dt_in("w_in", [D, 1184])
    swa_sink = dt_in("swa_sink", [1, 8])
    mla_q_norm = dt_in("mla_q_norm", [1, 256])
    w_q_up = dt_in("w_q_up", [256, 768])
    mla_kv_norm = dt_in("mla_kv_norm", [1, 128])
    w_kv_up = dt_in("w_kv_up", [128, 1024])
    out_norm = dt_in("out_norm", [1, 1024])
    w_out = dt_in("w_out", [D, D])
    w_pq = dt_in("w_pq", [D, 2048])
    sub_keys = dt_in("sub_keys", [16, 128, 128])
    exp_u = dt_in("exp_u", [16384, D])
    exp_v = dt_in("exp_v", [16384, D])
    final_norm = dt_in("final_norm", [1, D])
    rope64 = dt_in("rope64", [2, 128, S])
    rope32 = dt_in("rope32", [2, 32, S])
    y = nc.dram_tensor("y", [S, D], F32, kind="ExternalOutput").ap()
    x1d = nc.dram_tensor("x1d", [S, D], F32, kind="Internal").ap()
    modd = nc.dram_tensor("modd", [1, 6 * D], F32, kind="Internal").ap()
    uvd = nc.dram_tensor("uvd", [16384, 2 * D], BF16, kind="Internal").ap()
    UVK = [f"uvd{i}" for i in range(64)]

    with ExitStack() as gs:
        P = Prog(nc, gs)
        sb = lambda st, name, shape, dt: st.enter_context(nc.sbuf_tensor(name, list(shape), dt))
        ps = lambda st, name, shape, dt: st.enter_context(nc.psum_tensor(name, list(shape), dt))

        ident_f = sb(gs, "ident_f", [128, 128], F32)
        ident_b = sb(gs, "ident_b", [128, 128], BF16)
        dif_i = sb(gs, "dif_i", [128, 128], I32)
        ones_f = sb(gs, "ones_f", [1, 128], F32)
        fm_a = sb(gs, "fm_a", [128, 16], F32)
        iota16 = sb(gs, "iota16", [128, 16], F32)

        with ExitStack() as st:
            c8 = sb(st, "c8", [8, 128], F32)
            modrow = sb(st, "modrow", [1, 6 * D], F32)
            cT = sb(st, "cT", [128, 8], F32)
            brow = sb(st, "brow", [1, 6 * D], F32)
            wst = [sb(st, f"wst{i}", [128, 8, 512], F32) for i in range(2)]
            iota_i = sb(st, "iota_i", [128, 16], I32)
            pA = ps(st, "pA", [128, 512], F32)
            pB = ps(st, "pB", [128, 512], F32)

            P.op("pool", lambda e: e.iota(dif_i[:, :], [[1, 128]], 0, -1), writes=["dif_i"])
            P.op("pool", lambda e: e.iota(iota_i[:, :], [[1, 16]], 0, 0), writes=["iota_i"])
            P.op("dve", lambda e: e.tensor_copy(iota16[:, :], iota_i[:, :]), reads=["iota_i"], writes=["iota16"])
            if do_peer:
                for i in range(32):
                    for ti_, (tbl, off) in enumerate(((exp_u, 0), (exp_v, D))):
                        P.op("pool", lambda e, i=i, tbl=tbl, off=off: e.dma_start(
                            out=uvd[i * 512:(i + 1) * 512, off:off + D], in_=tbl[i * 512:(i + 1) * 512, :]),
                            writes=[UVK[i * 2 + ti_]], dma_sem="d_uvd")
            P.op("dve", lambda e: e.tensor_single_scalar(ident_f[:, :], dif_i[:, :], 0.0, ALU.is_equal),
                 reads=["dif_i"], writes=["ident_f"])
            P.op("dve", lambda e: e.tensor_single_scalar(ident_b[:, :], dif_i[:, :], 0.0, ALU.is_equal),
                 reads=["dif_i"], writes=["ident_b"])
            P.op("dve", lambda e: e.memset(ones_f[:, :], 1.0), writes=["ones_f"])
            P.op("sp", lambda e: e.dma_start(out=c8[:, :], in_=c), writes=["c8"], dma_sem="d_c8")
            P.op("sp", lambda e: e.dma_start(out=brow[:, :], in_=b_ada), writes=["brow"], dma_sem="d_brow")
            P.op("pe", lambda e: e.transpose(pA[:, 0:8], c8[:, :], ident_f[0:8, 0:8]),
                 reads=["c8", "ident_f"], writes=["pA"])
            P.op("act", lambda e: e.activation(cT[:, :], pA[:, 0:8], AF.Silu), reads=["pA"], writes=["cT"])
            wv = w_ada.rearrange("(k p) n -> p k n", p=128)
            pbuf = [pA, pB]
            for nb in range(12):
                wt = wst[nb % 2]
                wk = f"wst{nb % 2}"
                pp = pbuf[nb % 2]
                pk = "pA" if nb % 2 == 0 else "pB"
                P.op("sp", lambda e, wt=wt, nb=nb: e.dma_start(out=wt[:, :, :], in_=wv[:, :, nb * 512:(nb + 1) * 512]),
                     writes=[wk], dma_sem="d_" + wk)

                def mm(e, wt=wt, pp=pp):
                    for kc in range(8):
                        i = e.matmul(pp[0:1, :], cT[:, kc:kc + 1], wt[:, kc, :], start=(kc == 0), stop=(kc == 7))
                    return i
                P.op("pe", mm, reads=[wk, "cT"], writes=[pk])
                P.op("dve", lambda e, pp=pp, nb=nb: e.tensor_tensor(
                    modrow[0:1, nb * 512:(nb + 1) * 512], pp[0:1, :], brow[0:1, nb * 512:(nb + 1) * 512], ALU.add),
                    reads=[pk, "brow"], writes=["modrow"])
            P.op("dve", lambda e: e.tensor_scalar_add(modrow[0:1, D:2 * D], modrow[0:1, D:2 * D], 1.0),
                 reads=["modrow"], writes=["modrow"])
            P.op("dve", lambda e: e.tensor_scalar_add(modrow[0:1, 4 * D:5 * D], modrow[0:1, 4 * D:5 * D], 1.0),
                 reads=["modrow"], writes=["modrow"])
            P.op("sp", lambda e: e.dma_start(out=modd, in_=modrow[0:1, :]), reads=["modrow"], writes=["modd"],
                 dma_sem="d_modd")
            def fmm(e):
                for j in range(16):
                    off = (D if j < 8 else 0) + (j % 8) * 128
                    i = e.matmul(pA[:, j:j + 1], modrow[0:1, off:off + 128], ones_f[0:1, 0:1], start=True, stop=True)
                return i
            P.op("pe", fmm, reads=["modrow", "ones_f"], writes=["pA"])
            P.op("act", lambda e: e.copy(fm_a[:, :], pA[:, 0:16]), reads=["pA"], writes=["fm_a"])
            P.flush()

        if do_attn:
          with ExitStack() as sa:
            kvnT = sb(sa, "kvnT", [128, S], BF16)
            krT = sb(sa, "krT", [32, S], BF16)
            mla_v = sb(sa, "mla_v", [128, NB, 8, 65], BF16)
            swa_kT = sb(sa, "swa_kT", [128, S], BF16)
            swa_v = sb(sa, "swa_v", [128, NB, 2, 65], BF16)
            bc_ga = sb(sa, "bc_ga", [128, D], F32)
            bc_on = sb(sa, "bc_on", [128, D], F32)
            bc_qn = sb(sa, "bc_qn", [128, 256], F32)
            bc_kvn = sb(sa, "bc_kvn", [128, 128], F32)
            esink = sb(sa, "esink", [128, 8], F32)
            mprev = sb(sa, "mprev", [128, 128], BF16)
            mnext = sb(sa, "mnext", [128, 128], BF16)
            WkupT = sb(sa, "WkupT", [128, 4, 128], BF16)
            wvup = sb(sa, "wvup", [128, 512], BF16)
            xts = [sb(sa, f"xa{i}", [128, D], F32) for i in range(2)]
            xnb = sb(sa, "xnb", [128, D], BF16)
            hTc = sb(sa, "hTc", [128, 8, 512], BF16)
            cs64 = sb(sa, "cs64", [128, 2, 512], F32)
            cs32 = sb(sa, "cs32", [32, 2, 512], F32)
            tmpA = sb(sa, "tmpA", [128, 1024], F32)
            tmpB = sb(sa, "tmpB", [128, 512], F32)
            stA = sb(sa, "stA", [128, 8], F32)
            stB = sb(sa, "stB", [128, 8], F32)
            pT = ps(sa, "pTa", [128, 8, 128], BF16)
            pXX = ps(sa, "pXX", [128, 1024], F32)
            pSS = ps(sa, "pSS", [128, 1024], F32)
            pX = [pXX[:, i * 512:(i + 1) * 512] for i in range(2)]
            pSs = [pSS[:, i * 512:(i + 1) * 512] for i in range(2)]
            pO = [ps(sa, f"pO{i}", [128, 4, 65], F32) for i in range(2)]

            def bload(dst, dk, src):
                P.op("sp", lambda e: e.dma_start(out=dst[:, :], in_=src.partition_broadcast(128)),
                     reads=["modd"], writes=[dk], dma_sem="d_" + dk)
            bload(bc_ga, "bc_ga", modd[0:1, 2 * D:3 * D])
            bload(bc_on, "bc_on", out_norm)
            bload(bc_qn, "bc_qn", mla_q_norm)
            bload(bc_kvn, "bc_kvn", mla_kv_norm)
            bload(esink, "esink", swa_sink)
            P.op("act", lambda e: e.activation(esink[:, :], esink[:, :], AF.Exp), reads=["esink"], writes=["esink"])
            P.op("dve", lambda e: e.tensor_single_scalar(mprev[:, :], dif_i[:, :], 0.0, ALU.is_le),
                 reads=["dif_i"], writes=["mprev"])
            P.op("dve", lambda e: e.tensor_single_scalar(mnext[:, :], dif_i[:, :], 0.0, ALU.is_ge),
                 reads=["dif_i"], writes=["mnext"])
            P.op("dve", lambda e: e.memset(mla_v[:, :, :, 64:65], 1.0), writes=["mla_v"])
            P.op("dve", lambda e: e.memset(swa_v[:, :, :, 64:65], 1.0), writes=["swa_v"])

            pxi = [0]

            def nextpx():
                i = pxi[0] % 2
                pxi[0] += 1
                return pX[i], f"pX{i}"

            def load_norm_T(n, t):
                xt, xk = xts[n % 2], f"xa{n % 2}"
                P.op("sp", lambda e: e.dma_start(out=xt[:, :], in_=x[n * 128:(n + 1) * 128, :]),
                     writes=[xk], dma_sem="d_" + xk)
                P.op("act", lambda e: e.activation(xnb[:, :], xt[:, :], AF.Square, accum_out=stA[:, 0:1]),
                     reads=[xk], writes=["xnb", "stA0"])
                P.op("act", lambda e: e.activation(stA[:, 1:2], stA[:, 0:1], AF.Sqrt, bias=EPS, scale=1.0 / D),
                     reads=["stA0"], writes=["stA1"])
                P.op("dve", lambda e: e.reciprocal(stA[:, 2:3], stA[:, 1:2]), reads=["stA1"], writes=["stA2"])
                P.op("act", lambda e: e.activation(xnb[:, :], xt[:, :], AF.Copy, scale=stA[:, 2:3]),
                     reads=[xk, "stA2"], writes=["xnb"])

                def tr(e):
                    for kc in range(8):
                        i = e.transpose(pT[:, kc, :], xnb[:, kc * 128:(kc + 1) * 128], ident_b[:, :])
                    return i
                P.op("pe", tr, reads=["xnb", "ident_b"], writes=["pT"])

                def ev(e):
                    for kc in range(8):
                        i = e.activation(hTc[:, kc, t * 128:(t + 1) * 128], pT[:, kc, :], AF.Identity,
                                         bias=fm_a[:, 8 + kc:9 + kc], scale=fm_a[:, kc:kc + 1])
                    return i
                P.op("act", ev, reads=["pT", "fm_a"], writes=["hTc"])

            def proj_fm(w, wk, c0, M, rhs, rhsk, nk=8):
                pp, pk = nextpx()

                def mm(e):
                    for kc in range(nk):
                        i = e.matmul(pp[0:M, :], w[:, kc, c0:c0 + M], rhs[:, kc, :], start=(kc == 0), stop=(kc == nk - 1))
                    return i
                P.op("pe", mm, reads=[wk, rhsk], writes=[pk])
                return pp, pk

            def rope(pr, prk, psw, pswk, cs, csk, M, dst, dstk):
                P.op("dve", lambda e: e.tensor_tensor(tmpA[0:M, 0:512], pr[0:M, :], cs[0:M, 0, :], ALU.mult),
                     reads=[prk, csk], writes=["tmpA"])
                P.op("dve", lambda e: e.tensor_tensor(tmpB[0:M, 0:512], psw[0:M, :], cs[0:M, 1, :], ALU.mult),
                     reads=[pswk, csk], writes=["tmpB"])
                P.op("dve", lambda e: e.tensor_tensor(dst, tmpA[0:M, 0:512], tmpB[0:M, 0:512], ALU.add),
                     reads=["tmpA", "tmpB"], writes=[dstk])

            def load_rope(tc):
                c0 = tc * 512
                P.op("sp", lambda e: e.dma_start(out=cs64[:, :, :], in_=rope64[:, :, c0:c0 + 512].rearrange("t p s -> p t s")),
                     writes=["cs64"], dma_sem="d_cs64")
                P.op("sp", lambda e: e.dma_start(out=cs32[:, :, :], in_=rope32[:, :, c0:c0 + 512].rearrange("t p s -> p t s")),
                     writes=["cs32"], dma_sem="d_cs32")

            w_in_v = w_in.rearrange("(k p) n -> p k n", p=128)

            with ExitStack() as sA:
                wA = sb(sA, "wA", [128, 8, 576], BF16)
                stg = [sb(sA, f"stgA{i}", [128, 1184], F32) for i in range(2)]
                kvst = sb(sA, "kvst", [128, 1024], F32)
                knope = sb(sA, "knope", [128, 512], F32)
                kvnb = sb(sA, "kvnb", [128, 128], BF16)
                for kc in range(8):
                    sg, sk = stg[kc % 2], f"stgA{kc % 2}"
                    P.op("sp", lambda e, sg=sg, kc=kc: e.dma_start(out=sg[:, :], in_=w_in_v[:, kc, :]),
                         writes=[sk], dma_sem="d_" + sk)
                    P.op("act", lambda e, sg=sg, kc=kc: e.copy(wA[:, kc, 0:128], sg[:, 512:640]), reads=[sk], writes=["wA"])
                    P.op("act", lambda e, sg=sg, kc=kc: e.copy(wA[:, kc, 256:384], sg[:, 640:768]), reads=[sk], writes=["wA"])
                    P.op("act", lambda e, sg=sg, kc=kc: e.copy(wA[:, kc, 384:512], sg[:, 1024:1152]), reads=[sk], writes=["wA"])
                    P.op("act", lambda e, sg=sg, kc=kc: e.copy(wA[:, kc, 512:544], sg[:, 1152:1184]), reads=[sk], writes=["wA"])
                    ksrc = sg[:, 512:640].rearrange("p (g t d) -> p g t d", g=2, t=2)
                    kdst = wA[:, kc, 128:256].rearrange("p (g t d) -> p g t d", g=2, t=2)
                    P.op("dve", lambda e, a=kdst, b=ksrc: e.tensor_copy(a[:, :, 0, :], b[:, :, 1, :]), reads=[sk], writes=["wA"])
                    P.op("dve", lambda e, a=kdst, b=ksrc: e.tensor_copy(a[:, :, 1, :], b[:, :, 0, :]), reads=[sk], writes=["wA"])
                    P.op("dve", lambda e, sg=sg, kc=kc: e.tensor_copy(wA[:, kc, 544:560], sg[:, 1168:1184]), reads=[sk], writes=["wA"])
                    P.op("dve", lambda e, sg=sg, kc=kc: e.tensor_copy(wA[:, kc, 560:576], sg[:, 1152:1168]), reads=[sk], writes=["wA"])
                P.op("sp", lambda e: e.dma_start(out=kvst[:, :], in_=w_kv_up), writes=["kvst"], dma_sem="d_kvst")
                kv4 = kvst[:, :].rearrange("p (h t d) -> p h t d", h=8, t=2)
                P.op("dve", lambda e: e.tensor_copy(knope[:, :].rearrange("p (h d) -> p h d", h=8), kv4[:, :, 0, :]),
                     reads=["kvst"], writes=["knope"])
                P.op("dve", lambda e: e.tensor_copy(wvup[:, :].rearrange("p (h d) -> p h d", h=8), kv4[:, :, 1, :]),
                     reads=["kvst"], writes=["wvup"])

                def trk(e):
                    for j in range(4):
                        i = e.transpose(pX[0][:, j * 128:(j + 1) * 128], knope[:, j * 128:(j + 1) * 128], ident_f[:, :])
                    return i
                P.op("pe", trk, reads=["knope", "ident_f"], writes=["pX0"])
                P.op("act", lambda e: e.copy(WkupT[:, :, :], pX[0][:, :].rearrange("p (j r) -> p j r", j=4)),
                     reads=["pX0"], writes=["WkupT"])

                for tc in range(8):
                    c0 = tc * 512
                    load_rope(tc)
                    for t in range(4):
                        load_norm_T(tc * 4 + t, t)
                    pr, prk = proj_fm(wA, "wA", 0, 128, hTc, "hTc")
                    psw, pswk = proj_fm(wA, "wA", 128, 128, hTc, "hTc")
                    rope(pr, prk, psw, pswk, cs64, "cs64", 128, swa_kT[:, c0:c0 + 512], "swa_kT")
                    pr, prk = proj_fm(wA, "wA", 512, 32, hTc, "hTc")
                    psw, pswk = proj_fm(wA, "wA", 544, 32, hTc, "hTc")
                    rope(pr, prk, psw, pswk, cs32, "cs32", 32, krT[0:32, c0:c0 + 512], "krT")
                    for t in range(4):
                        n = tc * 4 + t
                        pp, pk = nextpx()

                        def mm(e, pp=pp, t=t):
                            for kc in range(8):
                                i = e.matmul(pp[:, 0:256], hTc[:, kc, t * 128:(t + 1) * 128], wA[:, kc, 256:512],
                                             start=(kc == 0), stop=(kc == 7))
                            return i
                        P.op("pe", mm, reads=["hTc", "wA"], writes=[pk])
                        P.op("act", lambda e, pp=pp, n=n: e.copy(swa_v[:, n, :, 0:64], pp[:, 0:128].rearrange("p (g d) -> p g d", g=2)),
                             reads=[pk], writes=["swa_v"])
                        P.op("act", lambda e, pp=pp: e.activation(tmpB[:, 0:128], pp[:, 128:256], AF.Square, accum_out=stB[:, 0:1]),
                             reads=[pk], writes=["tmpB", "stB0"])
                        P.op("act", lambda e: e.activation(stB[:, 1:2], stB[:, 0:1], AF.Sqrt, bias=EPS, scale=1.0 / 128),
                             reads=["stB0"], writes=["stB1"])
                        P.op("dve", lambda e: e.reciprocal(stB[:, 2:3], stB[:, 1:2]), reads=["stB1"], writes=["stB2"])
                        P.op("dve", lambda e, pp=pp: e.scalar_tensor_tensor(kvnb[:, :], pp[:, 128:256], stB[:, 2:3], bc_kvn[:, :],
                                                                           ALU.mult, ALU.mult),
                             reads=[pk, "stB2", "bc_kvn"], writes=["kvnb"])
                        P.op("pe", lambda e: e.transpose(pT[:, 0, :], kvnb[:, :], ident_b[:, :]),
                             reads=["kvnb", "ident_b"], writes=["pT"])
                        P.op("act", lambda e, n=n: e.copy(kvnT[:, n * 128:(n + 1) * 128], pT[:, 0, :]), reads=["pT"], writes=["kvnT"])
                        pp2, pk2 = nextpx()
                        P.op("pe", lambda e, pp2=pp2, n=n: e.matmul(pp2[:, :], kvnT[:, n * 128:(n + 1) * 128], wvup[:, :],
                                                                   start=True, stop=True),
                             reads=["kvnT", "wvup"], writes=[pk2])
                        P.op("dve", lambda e, pp2=pp2, n=n: e.tensor_copy(mla_v[:, n, :, 0:64],
                                                                         pp2[:, :].rearrange("p (h d) -> p h d", h=8)),
                             reads=[pk2], writes=["mla_v"])
                P.flush()

            with ExitStack() as sB:
                wB = sb(sB, "wB", [128, 8, 1280], BF16)
                wqn = sb(sB, "wqn", [128, 2, 512], BF16)
                wqr = sb(sB, "wqr", [128, 2, 512], BF16)
                woutb = sb(sB, "woutb", [128, 8, 1024], BF16)
                with ExitStack() as sW:
                    stg = [sb(sW, f"stgB{i}", [128, 1184], F32) for i in range(2)]
                    for kc in range(8):
                        sg, sk = stg[kc % 2], f"stgB{kc % 2}"
                        P.op("sp", lambda e, sg=sg, kc=kc: e.dma_start(out=sg[:, :], in_=w_in_v[:, kc, :]),
                             writes=[sk], dma_sem="d_" + sk)
                        qsrc = sg[:, 0:512].rearrange("p (g j d) -> p g j d", g=2, j=4)
                        qdst = wB[:, kc, 0:512].rearrange("p (j g d) -> p g j d", j=4, g=2)
                        P.op("act", lambda e, a=qdst, b=qsrc: e.copy(a[:, 0, :, :], b[:, 0, :, :]), reads=[sk], writes=["wB"])
                        P.op("act", lambda e, a=qdst, b=qsrc: e.copy(a[:, 1, :, :], b[:, 1, :, :]), reads=[sk], writes=["wB"])
                        qsrc5 = sg[:, 0:512].rearrange("p (g j t d) -> p g j t d", g=2, j=4, t=2)
                        qdst5 = wB[:, kc, 512:1024].rearrange("p (j g t d) -> p g j t d", j=4, g=2, t=2)
                        for g_ in range(2):
                            for t_ in range(2):
                                P.op("dve", lambda e, a=qdst5, b=qsrc5, g_=g_, t_=t_: e.tensor_copy(a[:, g_, :, t_, :], b[:, g_, :, 1 - t_, :]),
                                     reads=[sk], writes=["wB"])
                        P.op("act", lambda e, sg=sg, kc=kc: e.copy(wB[:, kc, 1024:1280], sg[:, 768:1024]), reads=[sk], writes=["wB"])
                    for a_ in range(2):
                        sg, sk = stg[a_], f"stgB{a_}"
                        P.op("sp", lambda e, sg=sg, a_=a_: e.dma_start(out=sg[:, 0:768], in_=w_q_up[a_ * 128:(a_ + 1) * 128, :]),
                             writes=[sk], dma_sem="d_" + sk)
                        u3 = sg[:, 0:768].rearrange("p (h c) -> p h c", h=8)
                        P.op("act", lambda e, u3=u3, a_=a_: e.copy(wqn[:, a_, :].rearrange("p (h d) -> p h d", h=8), u3[:, :, 0:64]),
                             reads=[sk], writes=["wqn"])
                        P.op("act", lambda e, u3=u3, a_=a_: e.copy(wqr[:, a_, 0:256].rearrange("p (h d) -> p h d", h=8), u3[:, :, 64:96]),
                             reads=[sk], writes=["wqr"])
                        sw3 = wqr[:, a_, 256:512].rearrange("p (h d) -> p h d", h=8)
                        P.op("dve", lambda e, u3=u3, sw3=sw3: e.tensor_copy(sw3[:, :, 0:16], u3[:, :, 80:96]), reads=[sk], writes=["wqr"])
                        P.op("dve", lambda e, u3=u3, sw3=sw3: e.tensor_copy(sw3[:, :, 16:32], u3[:, :, 64:80]), reads=[sk], writes=["wqr"])
                    w_out_v = w_out.rearrange("(k p) n -> p k n", p=128)
                    for kc in range(8):
                        sg, sk = stg[kc % 2], f"stgB{kc % 2}"
                        P.op("sp", lambda e, sg=sg, kc=kc: e.dma_start(out=sg[:, 0:1024], in_=w_out_v[:, kc, :]),
                             writes=[sk], dma_sem="d_" + sk)
                        if kc % 2 == 0:
                            P.op("act", lambda e, sg=sg, kc=kc: e.copy(woutb[:, kc, :], sg[:, 0:1024]), reads=[sk], writes=["woutb"])
                        else:
                            P.op("dve", lambda e, sg=sg, kc=kc: e.tensor_copy(woutb[:, kc, :], sg[:, 0:1024]), reads=[sk], writes=["woutb"])

                    P.flush()
                qT_swa = sb(sB, "qT_swa", [128, 4, 512], BF16)
                qnb = sb(sB, "qnb", [128, 256], BF16)
                qnT = sb(sB, "qnT", [128, 2, 512], BF16)
                qnopeT = sb(sB, "qnopeT", [128, 4, 512], BF16)
                qabsT = sb(sB, "qabsT", [128, 8, 512], BF16)
                qropeT = sb(sB, "qropeT", [32, 8, 512], BF16)
                E3 = [sb(sB, f"E3_{i}", [128, 3, 512], BF16) for i in range(2)]
                Em = [sb(sB, f"Em{i}", [128, 1024], BF16) for i in range(2)]
                mixed = sb(sB, "mixed", [128, 4, 1024], BF16)
                mixn = sb(sB, "mixn", [128, 1024], BF16)
                mixT = sb(sB, "mixT", [128, 8, 128], BF16)
                ssq = sb(sB, "ssq", [128, 4, 16], F32)
                sqt = sb(sB, "sqt", [128, 4, 64], F32)
                zt = sb(sB, "zt", [128, 4], F32)
                rzz = sb(sB, "rzz", [128, 4], F32)
                ssab = sb(sB, "ssab", [128, 2, 4], F32)
                rsab = sb(sB, "rsab", [128, 2, 4], F32)

                SC_SWA = 64 ** -0.5
                SC_MLA = 96 ** -0.5
                for tc in range(n_chunks):
                    c0 = tc * 512
                    load_rope(tc)
                    for t in range(4):
                        load_norm_T(tc * 4 + t, t)
                    for j in range(4):
                        pr, prk = proj_fm(wB, "wB", j * 128, 128, hTc, "hTc")
                        psw, pswk = proj_fm(wB, "wB", 512 + j * 128, 128, hTc, "hTc")
                        rope(pr, prk, psw, pswk, cs64, "cs64", 128, qT_swa[:, j, :], "qT_swa")
                    for t in range(4):
                        pp, pk = nextpx()

                        def mm(e, pp=pp, t=t):
                            for kc in range(8):
                                i = e.matmul(pp[:, 0:256], hTc[:, kc, t * 128:(t + 1) * 128], wB[:, kc, 1024:1280],
                                             start=(kc == 0), stop=(kc == 7))
                            return i
                        P.op("pe", mm, reads=["hTc", "wB"], writes=[pk])
                        P.op("act", lambda e, pp=pp: e.activation(tmpB[:, 0:256], pp[:, 0:256], AF.Square, accum_out=stB[:, 0:1]),
                             reads=[pk], writes=["tmpB", "stB0"])
                        P.op("act", lambda e: e.activation(stB[:, 1:2], stB[:, 0:1], AF.Sqrt, bias=EPS, scale=1.0 / 256),
                             reads=["stB0"], writes=["stB1"])
                        P.op("dve", lambda e: e.reciprocal(stB[:, 2:3], stB[:, 1:2]), reads=["stB1"], writes=["stB2"])
                        P.op("dve", lambda e, pp=pp: e.scalar_tensor_tensor(qnb[:, :], pp[:, 0:256], stB[:, 2:3], bc_qn[:, :],
                                                                           ALU.mult, ALU.mult),
                             reads=[pk, "stB2", "bc_qn"], writes=["qnb"])

                        def tr2(e):
                            for a_ in range(2):
                                i = e.transpose(pT[:, a_, :], qnb[:, a_ * 128:(a_ + 1) * 128], ident_b[:, :])
                            return i
                        P.op("pe", tr2, reads=["qnb", "ident_b"], writes=["pT"])
                        P.op("act", lambda e, t=t: e.copy(qnT[:, :, t * 128:(t + 1) * 128], pT[:, 0:2, :]), reads=["pT"], writes=["qnT"])
                    for j in range(4):
                        pp, pk = proj_fm(wqn, "wqn", j * 128, 128, qnT, "qnT", nk=2)
                        P.op("act", lambda e, pp=pp, j=j: e.copy(qnopeT[:, j, :], pp[:, :]), reads=[pk], writes=["qnopeT"])
                    for h in range(8):
                        j, m = h // 2, h % 2
                        pp, pk = nextpx()
                        P.op("pe", lambda e, pp=pp, j=j, m=m: e.matmul(pp[:, :], WkupT[m * 64:(m + 1) * 64, j, :],
                                                                      qnopeT[m * 64:(m + 1) * 64, j, :], start=True, stop=True),
                             reads=["WkupT", "qnopeT"], writes=[pk])
                        if h % 2 == 0:
                            P.op("act", lambda e, pp=pp, h=h: e.copy(qabsT[:, h, :], pp[:, :]), reads=[pk], writes=["qabsT"])
                        else:
                            P.op("dve", lambda e, pp=pp, h=h: e.tensor_copy(qabsT[:, h, :], pp[:, :]), reads=[pk], writes=["qabsT"])
                    for h in range(8):
                        pr, prk = proj_fm(wqr, "wqr", h * 32, 32, qnT, "qnT", nk=2)
                        psw, pswk = proj_fm(wqr, "wqr", 256 + h * 32, 32, qnT, "qnT", nk=2)
                        rope(pr, prk, psw, pswk, cs32, "cs32", 32, qropeT[0:32, h, :], "qropeT")

                    for t in range(4):
                        n = tc * 4 + t
                        js = [j for j in (n - 1, n, n + 1) if 0 <= j < NB]
                        for g in range(2):
                            e3, e3k = E3[g], f"E3_{g}"
                            for idx, j in enumerate(js):
                                psc, psk = pSs[idx % 2], f"pS{idx % 2}"
                                P.op("pe", lambda e, psc=psc, g=g, j=j, t=t: e.matmul(
                                    psc[:, :], swa_kT[g * 64:(g + 1) * 64, j * 128:(j + 1) * 128],
                                    qT_swa[g * 64:(g + 1) * 64, :, t * 128:(t + 1) * 128], start=True, stop=True),
                                    reads=["swa_kT", "qT_swa"], writes=[psk])
                                P.op("act", lambda e, psc=psc, e3=e3, idx=idx: e.activation(e3[:, idx, :], psc[:, :], AF.Exp, scale=SC_SWA),
                                     reads=[psk], writes=[e3k])
                                if j != n:
                                    mk_, mkk = (mprev, "mprev") if j == n - 1 else (mnext, "mnext")
                                    P.op("dve", lambda e, e3=e3, idx=idx, mk_=mk_: e.tensor_tensor(
                                        e3[:, idx, :].rearrange("p (h q) -> p h q", h=4),
                                        e3[:, idx, :].rearrange("p (h q) -> p h q", h=4),
                                        mk_[:, :].rearrange("p (o q) -> p o q", o=1).to_broadcast([128, 4, 128]), ALU.mult),
                                        reads=[e3k, mkk], writes=[e3k])
                            po, pok = pO[g], f"pO{g}"

                            def pv(e, po=po, e3=e3, g=g, js=js):
                                for hh in range(4):
                                    for idx, j in enumerate(js):
                                        i = e.matmul(po[:, hh, :], e3[:, idx, hh * 128:(hh + 1) * 128], swa_v[:, j, g, :],
                                                     start=(idx == 0), stop=(idx == len(js) - 1))
                                return i
                            P.op("pe", pv, reads=[e3k, "swa_v"], writes=[pok])
                            P.op("dve", lambda e, po=po, g=g: e.tensor_tensor(zt[:, :], po[:, :, 64], esink[:, 4 * g:4 * g + 4], ALU.add),
                                 reads=[pok, "esink"], writes=["zt"])
                            P.op("dve", lambda e: e.reciprocal(rzz[:, :], zt[:, :]), reads=["zt"], writes=["rzz"])
                            P.op("dve", lambda e, po=po, g=g, t=t: e.tensor_tensor(
                                sqt[:, :, :], po[:, :, 0:64],
                                rzz[:, :].rearrange("p (h o) -> p h o", o=1).to_broadcast([128, 4, 64]), ALU.mult),
                                reads=[pok, "rzz"], writes=["sqt"])
                            P.op("act", lambda e, g=g, t=t: e.copy(mixed[:, t, g * 256:(g + 1) * 256].rearrange("p (h d) -> p h d", h=4),
                                                                  sqt[:, :, :]),
                                 reads=["sqt"], writes=["mixed"])
                            P.op("dve", lambda e: e.tensor_tensor(sqt[:, :, :], sqt[:, :, :], sqt[:, :, :], ALU.mult),
                                 reads=["sqt"], writes=["sqt"])
                            P.op("dve", lambda e, g=g, t=t: e.tensor_reduce(ssq[:, t, g:g + 1], sqt[:, :, :], AX.XY, ALU.add),
                                 reads=["sqt"], writes=["ssq"])

                    sbufs = [(pSS, ["pS0", "pS1"]), (pXX, ["pX0", "pX1"])]
                    for h in range(8):
                        po, pok = pO[h % 2], f"pO{h % 2}"

                        def score(kp, h=h):
                            pst, psk = sbufs[kp % 2]

                            def mm(e):
                                for u_ in range(2):
                                    kb = kp * 2 + u_
                                    e.matmul(pst[:, u_ * 512:(u_ + 1) * 512], kvnT[:, kb * 128:(kb + 1) * 128], qabsT[:, h, :],
                                             start=True, stop=False)
                                    i = e.matmul(pst[:, u_ * 512:(u_ + 1) * 512], krT[0:32, kb * 128:(kb + 1) * 128],
                                                 qropeT[0:32, h, :], start=False, stop=True)
                                return i
                            P.op("pe", mm, reads=["kvnT", "krT", "qabsT", "qropeT"], writes=psk)
                        score(0)
                        for kp in range(NB // 2):
                            pst, psk = sbufs[kp % 2]
                            em, emk = Em[kp % 2], f"Em{kp % 2}"
                            P.op("act", lambda e, pst=pst, em=em: e.activation(em[:, :], pst[:, :], AF.Exp, scale=SC_MLA),
                                 reads=psk, writes=[emk])
                            if kp + 1 < NB // 2:
                                score(kp + 1)

                            def pv(e, em=em, kp=kp, po=po, h=h):
                                for u_ in range(2):
                                    kb = kp * 2 + u_
                                    for t in range(4):
                                        i = e.matmul(po[:, t, :], em[:, u_ * 512 + t * 128:u_ * 512 + (t + 1) * 128], mla_v[:, kb, h, :],
                                                     start=(kb == 0 and t == 0), stop=(kb == NB - 1), skip_group_check=True)
                                return i
                            P.op("pe", pv, reads=[emk, "mla_v"], writes=[pok])
                        P.op("dve", lambda e, po=po: e.reciprocal(rzz[:, :], po[:, :, 64]), reads=[pok], writes=["rzz"])
                        P.op("dve", lambda e, po=po: e.tensor_tensor(
                            sqt[:, :, :], po[:, :, 0:64],
                            rzz[:, :].rearrange("p (h o) -> p h o", o=1).to_broadcast([128, 4, 64]), ALU.mult),
                            reads=[pok, "rzz"], writes=["sqt"])
                        P.op("act", lambda e, h=h: e.copy(mixed[:, :, 512 + h * 64:512 + (h + 1) * 64], sqt[:, :, :]),
                             reads=["sqt"], writes=["mixed"])
                        P.op("dve", lambda e: e.tensor_tensor(sqt[:, :, :], sqt[:, :, :], sqt[:, :, :], ALU.mult),
                             reads=["sqt"], writes=["sqt"])
                        P.op("dve", lambda e, h=h: e.tensor_reduce(ssq[:, :, 2 + h], sqt[:, :, :], AX.X, ALU.add),
                             reads=["sqt"], writes=["ssq"])

                    P.op("dve", lambda e: e.tensor_reduce(ssab[:, 0, :], ssq[:, :, 0:2], AX.X, ALU.add), reads=["ssq"], writes=["ssab"])
                    P.op("dve", lambda e: e.tensor_reduce(ssab[:, 1, :], ssq[:, :, 2:10], AX.X, ALU.add), reads=["ssab", "ssq"], writes=["ssab"])
                    P.op("act", lambda e: e.activation(ssab[:, :, :], ssab[:, :, :], AF.Sqrt, bias=EPS, scale=1.0 / 512),
                         reads=["ssab"], writes=["ssab"])
                    P.op("dve", lambda e: e.reciprocal(rsab[:, :, :], ssab[:, :, :]), reads=["ssab"], writes=["rsab"])
                    for t in range(4):
                        n = tc * 4 + t
                        for gi in range(2):
                            P.op("dve", lambda e, t=t, gi=gi: e.scalar_tensor_tensor(
                                mixn[:, gi * 512:(gi + 1) * 512], mixed[:, t, gi * 512:(gi + 1) * 512], rsab[:, gi, t:t + 1],
                                bc_on[:, gi * 512:(gi + 1) * 512], ALU.mult, ALU.mult),
                                reads=["mixed", "rsab", "bc_on"], writes=["mixn"])

                        def trm(e):
                            for kc in range(8):
                                i = e.transpose(pT[:, kc, :], mixn[:, kc * 128:(kc + 1) * 128], ident_b[:, :])
                            return i
                        P.op("pe", trm, reads=["mixn", "ident_b"], writes=["pT"])
                        P.op("act", lambda e: e.copy(mixT[:, :, :], pT[:, :, :]), reads=["pT"], writes=["mixT"])
                        xt, xk = xts[n % 2], f"xa{n % 2}"
                        P.op("sp", lambda e, xt=xt, n=n: e.dma_start(out=xt[:, :], in_=x[n * 128:(n + 1) * 128, :]),
                             writes=[xk], dma_sem="d_" + xk)
                        for hf in range(2):
                            pp, pk = nextpx()

                            def mmo(e, pp=pp, hf=hf):
                                for kc in range(8):
                                    i = e.matmul(pp[:, :], mixT[:, kc, :], woutb[:, kc, hf * 512:(hf + 1) * 512],
                                                 start=(kc == 0), stop=(kc == 7))
                                return i
                            P.op("pe", mmo, reads=["mixT", "woutb"], writes=[pk])
                            P.op("dve", lambda e, pp=pp, hf=hf: e.tensor_tensor(tmpA[:, hf * 512:(hf + 1) * 512], pp[:, :],
                                                                               bc_ga[:, hf * 512:(hf + 1) * 512], ALU.mult),
                                 reads=[pk, "bc_ga"], writes=["tmpA"])
                        P.op("dve", lambda e, xt=xt: e.tensor_tensor(tmpA[:, :], tmpA[:, :], xt[:, :], ALU.add),
                             reads=["tmpA", xk], writes=["tmpA"])
                        P.op("sp", lambda e, n=n: e.dma_start(out=x1d[n * 128:(n + 1) * 128, :], in_=tmpA[:, :]),
                             reads=["tmpA"], writes=["x1d"], dma_sem="d_x1d")
                P.flush()
        x1src = x1d if do_attn else x

        if do_peer:
          with ExitStack() as st:
            wq = sb(st, "wq", [128, 8, 2048], BF16)
            bc_sc1f = sb(st, "bc_sc1f", [128, D], F32)
            bc_shf = sb(st, "bc_shf", [128, D], F32)
            bc_gf = sb(st, "bc_gf", [128, D], F32)
            bc_fin = sb(st, "bc_fin", [128, D], F32)
            for dst, dk, off in ((bc_shf, "bc_shf", 3 * D), (bc_sc1f, "bc_sc1f", 4 * D), (bc_gf, "bc_gf", 5 * D)):
                P.op("sp", lambda e, dst=dst, off=off: e.dma_start(
                    out=dst[:, :], in_=modd[0:1, off:off + D].partition_broadcast(128)),
                    reads=["modd"], writes=[dk], dma_sem="d_" + dk)
            P.op("sp", lambda e: e.dma_start(out=bc_fin[:, :], in_=final_norm.partition_broadcast(128)),
                 writes=["bc_fin"], dma_sem="d_fin")
            keysT = sb(st, "keysT", [128, 16, 128], BF16)
            NS = 16
            G = 4
            ug = [sb(st, f"ug{i}", [128, 2 * D], BF16) for i in range(NS)]
            dg = [sb(st, f"dg{i}", [128, 128], BF16) for i in range(4)]
            x1t = [sb(st, f"x1t{i}", [128, D], F32) for i in range(2)]
            hh = [sb(st, f"hh{i}", [128, D], F32) for i in range(2)]
            junkb = sb(st, "junkb", [128, D], BF16)
            junkf = sb(st, "junkf", [128, D], BF16)
            hb = sb(st, "hb", [128, D], BF16)
            hT = sb(st, "hT", [128, 8, 128], BF16)
            qT = sb(st, "qT", [128, 16, 128], BF16)
            bufS = sb(st, "bufS", [128, 2048], F32)
            bufS2 = sb(st, "bufS2", [128, 2048], F32)
            tv = sb(st, "tv", [128, 16, 16], F32)
            ti = sb(st, "ti", [128, 16, 16], U32)
            tif = sb(st, "tif", [128, 16, 16], F32)
            best = sb(st, "best", [128, 8, 16], F32)
            pos = sb(st, "pos", [128, 8, 16], U32)
            posa = sb(st, "posa", [128, 8, 16], U32)
            posb = sb(st, "posb", [128, 8, 16], U32)
            paf = sb(st, "paf", [128, 8, 16], F32)
            pbf = sb(st, "pbf", [128, 8, 16], F32)
            If = sb(st, "If", [128, 8, 16], F32)
            Jf = sb(st, "Jf", [128, 8, 16], F32)
            ef = sb(st, "ef", [128, 128], F32)
            ei = [sb(st, f"ei{i}", [128, 128], I32) for i in range(2)]
            gg = [sb(st, f"gg{i}", [128, 8, 16], F32) for i in range(2)]
            nmx = sb(st, "nmx", [128, 8], F32)
            zs = sb(st, "zs", [128, 8], F32)
            rz = sb(st, "rz", [128, 8], F32)
            Aa = sb(st, "Aa", [128, 128], F32)
            ga = sb(st, "ga", [128, 128], F32)
            ww = sb(st, "ww", [128, 128], F32)
            acc = sb(st, "acc", [128, D], F32)
            st8 = sb(st, "st8", [128, 8], F32)
            yt = sb(st, "yt", [128, D], F32)
            kst = sb(st, "kst", [128, 16, 128], F32)
            wstg = [sb(st, f"wstg{i}", [128, 2048], F32) for i in range(2)]
            pS = ps(st, "pS", [128, 2048], F32)
            pQ = ps(st, "pQ", [128, 4, 128], F32)
            pAcc = ps(st, "pAcc", [128, D], F32)
            pT = ps(st, "pT", [128, 8, 128], BF16)

            wqv = w_pq.rearrange("(k p) n -> p k n", p=128)
            for kc in range(8):
                wt = wstg[kc % 2]
                wk = f"wstg{kc % 2}"
                P.op("sp", lambda e, wt=wt, kc=kc: e.dma_start(out=wt[:, :], in_=wqv[:, kc, :]),
                     writes=[wk], dma_sem="d_" + wk)
                if kc % 2 == 0:
                    P.op("dve", lambda e, wt=wt, kc=kc: e.tensor_copy(wq[:, kc, :], wt[:, :]), reads=[wk], writes=["wq"])
                else:
                    P.op("act", lambda e, wt=wt, kc=kc: e.copy(wq[:, kc, :], wt[:, :]), reads=[wk], writes=["wq"])
            P.op("sp", lambda e: e.dma_start(out=kst[:, :, :], in_=sub_keys.rearrange("g n d -> n g d")),
                 writes=["kst"], dma_sem="d_kst")
            for g4 in range(4):
                def tr(e, g4=g4):
                    for j in range(4):
                        g = g4 * 4 + j
                        i = e.transpose(pS[:, j * 128:(j + 1) * 128], kst[:, g, :], ident_f[:, :])
                    return i
                P.op("pe", tr, reads=["kst", "ident_f"], writes=["pS"])
                P.op("act", lambda e, g4=g4: e.copy(keysT[:, g4 * 4:(g4 + 1) * 4, :],
                                                  pS[:, 0:512].rearrange("p (g n) -> p g n", g=4)),
                     reads=["pS"], writes=["keysT"])

            def top16(src, srck, dst2, dst2k, nseg, seglen, tvv, tvk, tii, tik):
                def r1(e):
                    for g in range(nseg):
                        i = e.max(tvv[:, g, 0:8], src[:, g * seglen:(g + 1) * seglen])
                    return i
                P.op("dve", r1, reads=[srck], writes=[tvk])

                def r2(e):
                    for g in range(nseg):
                        i = e.match_replace(dst2[:, g * seglen:(g + 1) * seglen], tvv[:, g, 0:8],
                                            src[:, g * seglen:(g + 1) * seglen], NEG)
                    return i
                P.op("dve", r2, reads=[srck, tvk], writes=[dst2k])

                def r3(e):
                    for g in range(nseg):
                        i = e.max(tvv[:, g, 8:16], dst2[:, g * seglen:(g + 1) * seglen])
                    return i
                P.op("dve", r3, reads=[dst2k], writes=[tvk])

                def r4(e):
                    for g in range(nseg):
                        e.max_index(tii[:, g, 0:8], tvv[:, g, 0:8], src[:, g * seglen:(g + 1) * seglen])
                        i = e.max_index(tii[:, g, 8:16], tvv[:, g, 8:16], dst2[:, g * seglen:(g + 1) * seglen])
                    return i
                P.op("dve", r4, reads=[srck, dst2k, tvk], writes=[tik])

            def sel(n):
                b = n % 2
                xt, xk = x1t[b], f"x1t{b}"
                h, hk = hh[b], f"hh{b}"
                P.op("sp", lambda e: e.dma_start(out=xt[:, :], in_=x1src[n * 128:(n + 1) * 128, :]),
                     reads=["x1d"], writes=[xk], dma_sem="d_" + xk)
                P.op("act", lambda e: e.activation(junkb[:, :], xt[:, :], AF.Square, accum_out=st8[:, 0:1]),
                     reads=[xk], writes=["junkb", "st8a"])
                P.op("act", lambda e: e.activation(st8[:, 1:2], st8[:, 0:1], AF.Sqrt, bias=EPS, scale=1.0 / D),
                     reads=["st8a"], writes=["st8b"])
                P.op("dve", lambda e: e.reciprocal(st8[:, 2:3], st8[:, 1:2]), reads=["st8b"], writes=["st8c"])
                P.op("dve", lambda e: e.scalar_tensor_tensor(h[:, :], xt[:, :], st8[:, 2:3], bc_sc1f[:, :], ALU.mult, ALU.mult),
                     reads=[xk, "st8c", "bc_sc1f"], writes=[hk])
                P.op("dve", lambda e: e.tensor_tensor(h[:, :], h[:, :], bc_shf[:, :], ALU.add),
                     reads=[hk, "bc_shf"], writes=[hk])
                P.op("act", lambda e: e.copy(hb[:, :], h[:, :]), reads=[hk], writes=["hb"])

                def tr(e):
                    for kc in range(8):
                        i = e.transpose(pT[:, kc, :], hb[:, kc * 128:(kc + 1) * 128], ident_b[:, :])
                    return i
                P.op("pe", tr, reads=["hb", "ident_b"], writes=["pT"])
                P.op("act", lambda e: e.copy(hT[:, :, :], pT[:, :, :]), reads=["pT"], writes=["hT"])
                for r in range(4):
                    def qmm(e, r=r):
                        for j in range(4):
                            g = r * 4 + j
                            for kc in range(8):
                                i = e.matmul(pQ[:, j, :], wq[:, kc, g * 128:(g + 1) * 128], hT[:, kc, :],
                                             start=(kc == 0), stop=(kc == 7))
                        return i
                    P.op("pe", qmm, reads=["wq", "hT"], writes=["pQ"])
                    P.op("act", lambda e, r=r: e.copy(qT[:, r * 4:(r + 1) * 4, :], pQ[:, :, :]), reads=["pQ"], writes=["qT"])

                def smm(e):
                    for g in range(16):
                        i = e.matmul(pS[:, g * 128:(g + 1) * 128], qT[:, g, :], keysT[:, g, :], start=True, stop=True)
                    return i
                P.op("pe", smm, reads=["qT", "keysT"], writes=["pS"])
                P.op("act", lambda e: e.copy(bufS[:, :], pS[:, :]), reads=["pS"], writes=["bufS"])
                top16(bufS, "bufS", bufS2, "bufS2", 16, 128, tv, "tv", ti, "ti")
                P.op("dve", lambda e: e.tensor_copy(tif[:, :, :], ti[:, :, :]), reads=["ti"], writes=["tif"])
                tv4 = tv[:, :, :].rearrange("p (h t) k -> p h t k", t=2)
                tif4 = tif[:, :, :].rearrange("p (h t) k -> p h t k", t=2)
                cand = bufS[:, :].rearrange("p (h a b) -> p h a b", h=8, a=16)
                P.op("dve", lambda e: e.tensor_tensor(
                    cand, tv4[:, :, 0, :].rearrange("p h (a o) -> p h a o", o=1).to_broadcast([128, 8, 16, 16]),
                    tv4[:, :, 1:2, :].to_broadcast([128, 8, 16, 16]), ALU.add),
                    reads=["tv"], writes=["bufS"])
                top16(bufS, "bufS", bufS2, "bufS2", 8, 256, best, "best", pos, "pos")
                P.op("dve", lambda e: e.tensor_scalar_mul(nmx[:, :], best[:, :, 0], -1.0), reads=["best"], writes=["nmx"])
                g_ = gg[b]
                gk = f"gg{b}"

                def ex(e):
                    for hd in range(8):
                        i = e.activation(g_[:, hd, :], best[:, hd, :], AF.Exp, bias=nmx[:, hd:hd + 1],
                                         accum_out=zs[:, hd:hd + 1])
                    return i
                P.op("act", ex, reads=["best", "nmx"], writes=[gk, "zs"])
                P.op("dve", lambda e: e.reciprocal(rz[:, :], zs[:, :]), reads=["zs"], writes=["rz"])
                P.op("dve", lambda e: e.tensor_tensor(
                    g_[:, :, :], g_[:, :, :], rz[:, :].rearrange("p (h o) -> p h o", o=1).to_broadcast([128, 8, 16]), ALU.mult),
                    reads=[gk, "rz"], writes=[gk])
                P.op("dve", lambda e: e.tensor_single_scalar(posa[:, :, :], pos[:, :, :], 4, ALU.logical_shift_right),
                     reads=["pos"], writes=["posa"])
                P.op("dve", lambda e: e.tensor_single_scalar(posb[:, :, :], pos[:, :, :], 15, ALU.bitwise_and),
                     reads=["pos"], writes=["posb"])
                P.op("dve", lambda e: e.tensor_copy(paf[:, :, :], posa[:, :, :]), reads=["posa"], writes=["paf"])
                P.op("dve", lambda e: e.tensor_copy(pbf[:, :, :], posb[:, :, :]), reads=["posb"], writes=["pbf"])
                oh = bufS[:, :].rearrange("p (h k a) -> p h k a", h=8, k=16)
                oh2 = bufS2[:, :].rearrange("p (h k a) -> p h k a", h=8, k=16)
                io4 = iota16[:, :].rearrange("p (h k a) -> p h k a", h=1, k=1).to_broadcast([128, 8, 16, 16])
                for (pf, pfk, t, dst, dstk) in ((paf, "paf", 0, If, "If"), (pbf, "pbf", 1, Jf, "Jf")):
                    P.op("dve", lambda e, pf=pf: e.tensor_tensor(
                        oh, io4, pf[:, :, :].rearrange("p h (k o) -> p h k o", o=1).to_broadcast([128, 8, 16, 16]),
                        ALU.is_equal), reads=[pfk, "iota16"], writes=["bufS"])
                    P.op("dve", lambda e, t=t: e.tensor_tensor(
                        oh2, oh, tif4[:, :, t:t + 1, :].to_broadcast([128, 8, 16, 16]), ALU.mult),
                        reads=["bufS", "tif"], writes=["bufS2"])
                    P.op("dve", lambda e, dst=dst: e.tensor_reduce(dst[:, :, :], oh2, AX.X, ALU.add),
                         reads=["bufS2"], writes=[dstk])
                P.op("dve", lambda e: e.scalar_tensor_tensor(
                    ef[:, :], If[:, :, :].rearrange("p h k -> p (h k)"), 128.0,
                    Jf[:, :, :].rearrange("p h k -> p (h k)"), ALU.mult, ALU.add),
                    reads=["If", "Jf"], writes=["ef"])
                P.op("dve", lambda e: e.tensor_copy(ei[b][:, :], ef[:, :]), reads=["ef"], writes=[f"ei{b}"])

            slot = [0]

            def gather(n_b, j):
                s_ = slot[0] % NS
                slot[0] += 1
                P.op("pool", lambda e: e.indirect_dma_start(
                    out=ug[s_][:, :], out_offset=None, in_=uvd,
                    in_offset=bass.IndirectOffsetOnAxis(ap=ei[n_b][:, j:j + 1], axis=0)),
                    reads=[f"ei{n_b}"] + UVK, writes=[f"ug{s_}"], dma_sem=f"d_ug{s_}")
                return s_

            dgi = [0]

            def experts(n):
                b = n % 2
                xt, xk = x1t[b], f"x1t{b}"
                h, hk = hh[b], f"hh{b}"
                gflat = gg[b][:, :, :].rearrange("p h k -> p (h k)")
                ngrp = 128 // G
                pend = None

                def vside(grp, slots):
                    cs = slice(grp * G, (grp + 1) * G)
                    kq = grp % 4
                    P.op("dve", lambda e: e.tensor_tensor(ww[:, cs], ga[:, cs], gflat[:, cs], ALU.mult),
                         reads=[f"ga{kq}", f"gg{b}"], writes=[f"ww{kq}"])
                    for jj, s_ in enumerate(slots):
                        j = grp * G + jj
                        di = dgi[0] % 4
                        dgi[0] += 1
                        P.op("act", lambda e, di=di, j=j: e.activation(dg[di][:, :], ident_b[:, :], AF.Copy, scale=ww[:, j:j + 1]),
                             reads=[f"ww{kq}", "ident_b"], writes=[f"dg{di}"])

                        def mm(e, di=di, s_=s_, j=j):
                            e.matmul(pAcc[:, 0:512], dg[di][:, :], ug[s_][:, D:D + 512], start=(j == 0), stop=(j == 127))
                            return e.matmul(pAcc[:, 512:1024], dg[di][:, :], ug[s_][:, D + 512:2 * D], start=(j == 0), stop=(j == 127))
                        P.op("pe", mm, reads=[f"dg{di}", f"ug{s_}"], writes=["pAcc"])

                for grp in range(ngrp):
                    cs = slice(grp * G, (grp + 1) * G)
                    kq = grp % 4
                    slots = []
                    for jj in range(G):
                        j = grp * G + jj
                        s_ = gather(b, j)
                        slots.append(s_)
                        P.op("dve", lambda e, s_=s_, j=j: e.scalar_tensor_tensor(
                            junkf[:, :], ug[s_][:, 0:D], 1.0, h[:, :], ALU.mult, ALU.mult, accum_out=Aa[:, j:j + 1]),
                            reads=[f"ug{s_}", hk], writes=["junkf", f"Aa{kq}"])
                    P.op("act", lambda e, cs=cs: e.activation(ga[:, cs], Aa[:, cs], AF.Gelu), reads=[f"Aa{kq}"], writes=[f"ga{kq}"])
                    if pend is not None:
                        vside(*pend)
                    pend = (grp, slots)
                    if grp >= 1:
                        k_ = (len(pending_sel) + (ngrp - 1 - grp) - 1) // max(ngrp - 1 - grp, 1) if grp < ngrp - 1 else len(pending_sel)
                        P.replay(pending_sel[:k_])
                        del pending_sel[:k_]
                vside(*pend)
                P.op("dve", lambda e: e.tensor_tensor(acc[:, :], pAcc[:, :], bc_gf[:, :], ALU.mult),
                     reads=["pAcc", "bc_gf"], writes=["acc"])
                P.op("dve", lambda e: e.tensor_tensor(acc[:, :], acc[:, :], xt[:, :], ALU.add),
                     reads=["acc", xk], writes=["acc"])
                P.op("act", lambda e: e.activation(junkb[:, :], acc[:, :], AF.Square, accum_out=st8[:, 3:4]),
                     reads=["acc"], writes=["junkb", "st8d"])
                P.op("act", lambda e: e.activation(st8[:, 4:5], st8[:, 3:4], AF.Sqrt, bias=EPS, scale=1.0 / D),
                     reads=["st8d"], writes=["st8e"])
                P.op("dve", lambda e: e.reciprocal(st8[:, 5:6], st8[:, 4:5]), reads=["st8e"], writes=["st8f"])
                P.op("dve", lambda e: e.scalar_tensor_tensor(yt[:, :], acc[:, :], st8[:, 5:6], bc_fin[:, :], ALU.mult, ALU.mult),
                     reads=["acc", "st8f", "bc_fin"], writes=["yt"])
                P.op("sp", lambda e: e.dma_start(out=y[n * 128:(n + 1) * 128, :], in_=yt[:, :]),
                     reads=["yt"], writes=["y_out"], dma_sem="d_y")

            pending_sel = []
            sel(0)
            for n in range(n_blocks):
                if n + 1 < n_blocks:
                    pending_sel.extend(P.capture(lambda: sel(n + 1)))
                experts(n)
                P.replay(pending_sel)
                del pending_sel[:]
            P.flush()
    return nc


def _rope_tables():
    pos = np.arange(S, dtype=np.float32)
    out = {}
    for dim, name in ((64, "rope64"), (32, "rope32")):
        inv = (1.0 / (10000.0 ** (np.arange(0, dim, 2, dtype=np.float32) / dim))).astype(np.float32)
        ang = pos[:, None] * inv[None, :]
        cos = np.cos(ang).astype(np.float32).T
        sin = np.sin(ang).astype(np.float32).T
        cosf = np.concatenate([cos, cos], axis=0)
        sinf = np.concatenate([-sin, sin], axis=0)
        if dim == 64:
            cosf = np.concatenate([cosf, cosf], axis=0)
            sinf = np.concatenate([sinf, sinf], axis=0)
        out[name] = np.ascontiguousarray(np.stack([cosf, sinf], axis=0))
    return out


def make_in_maps(inputs, n_cores=8):
    g = lambda k: np.ascontiguousarray(np.asarray(inputs[k], dtype=np.float32))
    rt = _rope_tables()
    shared = {
        "w_ada": g("w_ada")[0], "b_ada": g("b_ada")[0][None, :], "w_in": g("w_in")[0],
        "swa_sink": g("swa_sink")[0][None, :], "mla_q_norm": g("mla_q_norm")[0][None, :],
        "w_q_up": g("w_mla_q_up")[0], "mla_kv_norm": g("mla_kv_norm")[0][None, :],
        "w_kv_up": g("w_mla_kv_up")[0],
        "out_norm": np.ascontiguousarray(np.concatenate([g("out_norm_swa")[0], g("out_norm_mla")[0]])[None, :]),
        "w_out": g("w_out")[0], "w_pq": g("w_peer_query")[0],
        "sub_keys": np.ascontiguousarray(g("peer_sub_keys")[0].reshape(16, 128, 128)),
        "exp_u": g("peer_expert_u")[0], "exp_v": g("peer_expert_v")[0],
        "final_norm": g("final_norm")[None, :], "rope64": rt["rope64"], "rope32": rt["rope32"],
    }
    xs = g("x")
    cs = g("c")
    maps = []
    for i in range(n_cores):
        m = dict(shared)
        m["x"] = np.ascontiguousarray(xs[i])
        m["c"] = np.ascontiguousarray(cs[i].reshape(8, 128))
        maps.append(m)
    return maps


def kernel(**inputs):
    nc = build()
    in_maps = make_in_maps(inputs, 8)
    res = run_bass_kernel_spmd(nc, in_maps, core_ids=list(range(8)))
    return np.stack([np.asarray(r["y"]).reshape(S, D) for r in res.results], axis=0).astype(np.float32)
```

```python
from contextlib import ExitStack
import numpy as np
import concourse.bass as bass
import concourse.mybir as mybir
from concourse.bass_utils import run_bass_kernel_spmd

F32 = mybir.dt.float32
BF16 = mybir.dt.bfloat16
I32 = mybir.dt.int32
U32 = mybir.dt.uint32
AF = mybir.ActivationFunctionType
ALU = mybir.AluOpType
AX = mybir.AxisListType

S = 4096
D = 1024
NB = S // 128
EPS = 1e-6
NEG = -1e30


class Prog:
    ENGS = ("pe", "act", "dve", "pool", "sp")

    def __init__(self, nc, stack):
        self.nc = nc
        self.stack = stack
        self.ops = {e: [] for e in self.ENGS}
        self.sem = {}
        self.cnt = {}
        self.waited = {e: {} for e in self.ENGS}
        self.lastw = {}
        self.reads = {}
        self.defer = None
        for e in self.ENGS:
            self.newsem("c_" + e)

    def capture(self, fn):
        self.defer = []
        fn()
        lst, self.defer = self.defer, None
        return lst

    def replay(self, thunks):
        for t in thunks:
            self.op(*t)

    def newsem(self, name):
        if name not in self.sem:
            self.sem[name] = self.stack.enter_context(self.nc.semaphore(name))
            self.cnt[name] = 0
        return name

    def op(self, eng, fn, reads=(), writes=(), dma_sem=None):
        if self.defer is not None:
            self.defer.append((eng, fn, tuple(reads), tuple(writes), dma_sem))
            return None
        waits = {}

        def need(tok):
            if tok is None:
                return
            s, v = tok
            if waits.get(s, 0) < v:
                waits[s] = v

        for k in reads:
            need(self.lastw.get(k))
        for k in writes:
            need(self.lastw.get(k))
            for s, v in self.reads.get(k, {}).items():
                need((s, v))
        w = []
        for s, v in waits.items():
            if self.waited[eng].get(s, 0) < v:
                self.waited[eng][s] = v
                w.append((s, v))
        if dma_sem is None:
            s = "c_" + eng
            inc = 1
        else:
            s = self.newsem(dma_sem)
            inc = 16
        self.cnt[s] += inc
        tok = (s, self.cnt[s])
        for k in writes:
            self.lastw[k] = tok
            self.reads[k] = {}
        for k in reads:
            self.reads.setdefault(k, {})[s] = self.cnt[s]
        self.ops[eng].append((fn, w, s, inc))
        return tok

    def flush(self):
        nc = self.nc
        final = dict(self.cnt)
        with nc.Block() as block:
            def mk(engname):
                def body(eng):
                    for fn, w, s, inc in self.ops[engname]:
                        for ws, wv in w:
                            eng.wait_ge(self.sem[ws], wv)
                        fn(eng).then_inc(self.sem[s], inc)
                    for s, v in final.items():
                        if v > 0 and self.waited[engname].get(s, 0) < v:
                            eng.wait_ge(self.sem[s], v)
                            self.waited[engname][s] = v
                return body
            block.tensor(mk("pe"))
            block.scalar(mk("act"))
            block.vector(mk("dve"))
            block.gpsimd(mk("pool"))
            block.sync(mk("sp"))
        self.ops = {e: [] for e in self.ENGS}


def build(n_blocks=NB, do_attn=True, do_peer=True, n_chunks=8):
    nc = bass.Bass("TRN2", target_bir_lowering=False)
    dt_in = lambda name, shape: nc.dram_tensor(name, list(shape), F32, kind="ExternalInput").ap()
    x = dt_in("x", [S, D])
    c = dt_in("c", [8, 128])
    w_ada = dt_in("w_ada", [D, 6 * D])
    b_ada = dt_in("b_ada", [1, 6 * D])
    w_in = dt_in("w_in", [D, 1184])
    swa_sink = dt_in("swa_sink", [1, 8])
    mla_q_norm = dt_in("mla_q_norm", [1, 256])
    w_q_up = dt_in("w_q_up", [256, 768])
    mla_kv_norm = dt_in("mla_kv_norm", [1, 128])
    w_kv_up = dt_in("w_kv_up", [128, 1024])
    out_norm = dt_in("out_norm", [1, 1024])
    w_out = dt_in("w_out", [D, D])
    w_pq = dt_in("w_pq", [D, 2048])
    sub_keys = dt_in("sub_keys", [16, 128, 128])
    exp_u = dt_in("exp_u", [16384, D])
    exp_v = dt_in("exp_v", [16384, D])
    final_norm = dt_in("final_norm", [1, D])
    rope64 = dt_in("rope64", [2, 128, S])
    rope32 = dt_in("rope32", [2, 64, S])
    y = nc.dram_tensor("y", [S, D], F32, kind="ExternalOutput").ap()
    x1d = nc.dram_tensor("x1d", [S, D], F32, kind="Internal").ap()
    modd = nc.dram_tensor("modd", [1, 6 * D], F32, kind="Internal").ap()
    uvd = nc.dram_tensor("uvd", [16384, 2 * D], BF16, kind="Internal").ap()
    UVK = [f"uvd{i}" for i in range(64)]

    with ExitStack() as gs:
        P = Prog(nc, gs)
        sb = lambda st, name, shape, dt: st.enter_context(nc.sbuf_tensor(name, list(shape), dt))
        ps = lambda st, name, shape, dt: st.enter_context(nc.psum_tensor(name, list(shape), dt))

        ident_f = sb(gs, "ident_f", [128, 128], F32)
        ident_b = sb(gs, "ident_b", [128, 128], BF16)
        dif_i = sb(gs, "dif_i", [128, 128], I32)
        ones_f = sb(gs, "ones_f", [1, 128], F32)
        fm_a = sb(gs, "fm_a", [128, 16], F32)
        iota16 = sb(gs, "iota16", [128, 16], F32)

        with ExitStack() as st:
            c8 = sb(st, "c8", [8, 128], F32)
            modrow = sb(st, "modrow", [1, 6 * D], F32)
            cT = sb(st, "cT", [128, 8], F32)
            brow = sb(st, "brow", [1, 6 * D], F32)
            wst = [sb(st, f"wst{i}", [128, 8, 512], F32) for i in range(2)]
            iota_i = sb(st, "iota_i", [128, 16], I32)
            pA = ps(st, "pA", [128, 512], F32)
            pB = ps(st, "pB", [128, 512], F32)

            P.op("pool", lambda e: e.iota(dif_i[:, :], [[1, 128]], 0, -1), writes=["dif_i"])
            P.op("pool", lambda e: e.iota(iota_i[:, :], [[1, 16]], 0, 0), writes=["iota_i"])
            P.op("dve", lambda e: e.tensor_copy(iota16[:, :], iota_i[:, :]), reads=["iota_i"], writes=["iota16"])
            P.op("dve", lambda e: e.tensor_single_scalar(ident_f[:, :], dif_i[:, :], 0.0, ALU.is_equal),
                 reads=["dif_i"], writes=["ident_f"])
            P.op("dve", lambda e: e.tensor_single_scalar(ident_b[:, :], dif_i[:, :], 0.0, ALU.is_equal),
                 reads=["dif_i"], writes=["ident_b"])
            P.op("dve", lambda e: e.memset(ones_f[:, :], 1.0), writes=["ones_f"])
            P.op("sp", lambda e: e.dma_start(out=c8[:, :], in_=c), writes=["c8"], dma_sem="d_c8")
            P.op("sp", lambda e: e.dma_start(out=brow[:, :], in_=b_ada), writes=["brow"], dma_sem="d_brow")
            P.op("pe", lambda e: e.transpose(pA[:, 0:8], c8[:, :], ident_f[0:8, 0:8]),
                 reads=["c8", "ident_f"], writes=["pA"])
            P.op("act", lambda e: e.activation(cT[:, :], pA[:, 0:8], AF.Silu), reads=["pA"], writes=["cT"])
            wv = w_ada.rearrange("(k p) n -> p k n", p=128)
            pbuf = [pA, pB]
            for nb in range(12):
                wt = wst[nb % 2]
                wk = f"wst{nb % 2}"
                pp = pbuf[nb % 2]
                pk = "pA" if nb % 2 == 0 else "pB"
                P.op("sp", lambda e, wt=wt, nb=nb: e.dma_start(out=wt[:, :, :], in_=wv[:, :, nb * 512:(nb + 1) * 512]),
                     writes=[wk], dma_sem="d_" + wk)

                def mm(e, wt=wt, pp=pp):
                    for kc in range(8):
                        i = e.matmul(pp[0:1, :], cT[:, kc:kc + 1], wt[:, kc, :], start=(kc == 0), stop=(kc == 7))
                    return i
                P.op("pe", mm, reads=[wk, "cT"], writes=[pk])
                P.op("dve", lambda e, pp=pp, nb=nb: e.tensor_tensor(
                    modrow[0:1, nb * 512:(nb + 1) * 512], pp[0:1, :], brow[0:1, nb * 512:(nb + 1) * 512], ALU.add),
                    reads=[pk, "brow"], writes=["modrow"])
            P.op("dve", lambda e: e.tensor_scalar_add(modrow[0:1, D:2 * D], modrow[0:1, D:2 * D], 1.0),
                 reads=["modrow"], writes=["modrow"])
            P.op("dve", lambda e: e.tensor_scalar_add(modrow[0:1, 4 * D:5 * D], modrow[0:1, 4 * D:5 * D], 1.0),
                 reads=["modrow"], writes=["modrow"])
            P.op("sp", lambda e: e.dma_start(out=modd, in_=modrow[0:1, :]), reads=["modrow"], writes=["modd"],
                 dma_sem="d_modd")
            def fmm(e):
                for j in range(16):
                    off = (D if j < 8 else 0) + (j % 8) * 128
                    i = e.matmul(pA[:, j:j + 1], modrow[0:1, off:off + 128], ones_f[0:1, 0:1], start=True, stop=True)
                return i
            P.op("pe", fmm, reads=["modrow", "ones_f"], writes=["pA"])
            P.op("act", lambda e: e.copy(fm_a[:, :], pA[:, 0:16]), reads=["pA"], writes=["fm_a", "uv_go"])
            if do_peer:
                for i in range(32):
                    for ti_, (tbl, off) in enumerate(((exp_u, 0), (exp_v, D))):
                        P.op("pool", lambda e, i=i, tbl=tbl, off=off: e.dma_start(
                            out=uvd[i * 512:(i + 1) * 512, off:off + D], in_=tbl[i * 512:(i + 1) * 512, :]),
                            reads=["uv_go"], writes=[UVK[i * 2 + ti_]], dma_sem="d_uvd")
            P.flush()

        if do_attn:
          with ExitStack() as sa:
            kvnT = sb(sa, "kvnT", [128, S], BF16)
            krT = sb(sa, "krT", [64, S], BF16)
            mla_v = sb(sa, "mla_v", [128, NB, 8, 65], BF16)
            swa_kT = sb(sa, "swa_kT", [128, S], BF16)
            swa_v = sb(sa, "swa_v", [128, NB, 2, 65], BF16)
            bc_ga = sb(sa, "bc_ga", [128, D], F32)
            bc_on = sb(sa, "bc_on", [128, D], F32)
            bc_qn = sb(sa, "bc_qn", [128, 256], F32)
            bc_kvn = sb(sa, "bc_kvn", [128, 128], F32)
            esink = sb(sa, "esink", [128, 8], F32)
            mprev = sb(sa, "mprev", [128, 128], BF16)
            mnext = sb(sa, "mnext", [128, 128], BF16)
            WkupT = sb(sa, "WkupT", [128, 4, 128], BF16)
            wvup = sb(sa, "wvup", [128, 512], BF16)
            xts = [sb(sa, f"xa{i}", [128, D], F32) for i in range(2)]
            xnb = sb(sa, "xnb", [128, D], BF16)
            hTc = sb(sa, "hTc", [128, 8, 512], BF16)
            cs64 = sb(sa, "cs64", [128, 2, 512], F32)
            cs32 = sb(sa, "cs32", [64, 2, 512], F32)
            tmpA = sb(sa, "tmpA", [128, 1024], F32)
            tmpB = sb(sa, "tmpB", [128, 512], F32)
            stA = sb(sa, "stA", [128, 8], F32)
            stB = sb(sa, "stB", [128, 8], F32)
            pT = ps(sa, "pTa", [128, 8, 128], BF16)
            pXX = ps(sa, "pXX", [128, 1024], F32)
            pSS = ps(sa, "pSS", [128, 1024], F32)
            pX = [pXX[:, i * 512:(i + 1) * 512] for i in range(2)]
            pSs = [pSS[:, i * 512:(i + 1) * 512] for i in range(2)]
            pO = [ps(sa, f"pO{i}", [128, 4, 65], F32) for i in range(2)]

            def bload(dst, dk, src):
                P.op("sp", lambda e: e.dma_start(out=dst[:, :], in_=src.partition_broadcast(128)),
                     reads=["modd"], writes=[dk], dma_sem="d_" + dk)
            bload(bc_ga, "bc_ga", modd[0:1, 2 * D:3 * D])
            bload(bc_on, "bc_on", out_norm)
            bload(bc_qn, "bc_qn", mla_q_norm)
            bload(bc_kvn, "bc_kvn", mla_kv_norm)
            bload(esink, "esink", swa_sink)
            P.op("act", lambda e: e.activation(esink[:, :], esink[:, :], AF.Exp), reads=["esink"], writes=["esink"])
            P.op("dve", lambda e: e.tensor_single_scalar(mprev[:, :], dif_i[:, :], 0.0, ALU.is_le),
                 reads=["dif_i"], writes=["mprev"])
            P.op("dve", lambda e: e.tensor_single_scalar(mnext[:, :], dif_i[:, :], 0.0, ALU.is_ge),
                 reads=["dif_i"], writes=["mnext"])
            P.op("dve", lambda e: e.memset(mla_v[:, :, :, 64:65], 1.0), writes=["mla_v"])
            P.op("dve", lambda e: e.memset(swa_v[:, :, :, 64:65], 1.0), writes=["swa_v"])

            pxi = [0]

            def nextpx():
                i = pxi[0] % 2
                pxi[0] += 1
                return pX[i], f"pX{i}"

            def load_norm_T(n, t):
                xt, xk = xts[n % 2], f"xa{n % 2}"
                P.op("sp", lambda e: e.dma_start(out=xt[:, :], in_=x[n * 128:(n + 1) * 128, :]),
                     writes=[xk], dma_sem="d_" + xk)
                P.op("act", lambda e: e.activation(xnb[:, :], xt[:, :], AF.Square, accum_out=stA[:, 0:1]),
                     reads=[xk], writes=["xnb", "stA0"])
                P.op("act", lambda e: e.activation(stA[:, 1:2], stA[:, 0:1], AF.Sqrt, bias=EPS, scale=1.0 / D),
                     reads=["stA0"], writes=["stA1"])
                P.op("dve", lambda e: e.reciprocal(stA[:, 2:3], stA[:, 1:2]), reads=["stA1"], writes=["stA2"])
                P.op("act", lambda e: e.activation(xnb[:, :], xt[:, :], AF.Copy, scale=stA[:, 2:3]),
                     reads=[xk, "stA2"], writes=["xnb"])

                def tr(e):
                    for kc in range(8):
                        i = e.transpose(pT[:, kc, :], xnb[:, kc * 128:(kc + 1) * 128], ident_b[:, :])
                    return i
                P.op("pe", tr, reads=["xnb", "ident_b"], writes=["pT"])

                def ev(e):
                    for kc in range(8):
                        i = e.activation(hTc[:, kc, t * 128:(t + 1) * 128], pT[:, kc, :], AF.Identity,
                                         bias=fm_a[:, 8 + kc:9 + kc], scale=fm_a[:, kc:kc + 1])
                    return i
                P.op("act", ev, reads=["pT", "fm_a"], writes=["hTc"])

            def proj_fm(w, wk, c0, M, rhs, rhsk, nk=8):
                pp, pk = nextpx()

                def mm(e):
                    for kc in range(nk):
                        i = e.matmul(pp[0:M, :], w[:, kc, c0:c0 + M], rhs[:, kc, :], start=(kc == 0), stop=(kc == nk - 1))
                    return i
                P.op("pe", mm, reads=[wk, rhsk], writes=[pk])
                return pp, pk

            def rope(pr, prk, psw, pswk, cs, csk, M, dst, dstk):
                P.op("dve", lambda e: e.tensor_tensor(tmpA[0:M, 0:512], pr[0:M, :], cs[0:M, 0, :], ALU.mult),
                     reads=[prk, csk], writes=["tmpA"])
                P.op("dve", lambda e: e.tensor_tensor(tmpB[0:M, 0:512], psw[0:M, :], cs[0:M, 1, :], ALU.mult),
                     reads=[pswk, csk], writes=["tmpB"])
                P.op("dve", lambda e: e.tensor_tensor(dst, tmpA[0:M, 0:512], tmpB[0:M, 0:512], ALU.add),
                     reads=["tmpA", "tmpB"], writes=[dstk])

            def load_rope(tc):
                c0 = tc * 512
                P.op("sp", lambda e: e.dma_start(out=cs64[:, :, :], in_=rope64[:, :, c0:c0 + 512].rearrange("t p s -> p t s")),
                     writes=["cs64"], dma_sem="d_cs64")
                P.op("sp", lambda e: e.dma_start(out=cs32[:, :, :], in_=rope32[:, :, c0:c0 + 512].rearrange("t p s -> p t s")),
                     writes=["cs32"], dma_sem="d_cs32")

            w_in_v = w_in.rearrange("(k p) n -> p k n", p=128)

            with ExitStack() as sA:
                wA = sb(sA, "wA", [128, 8, 640], BF16)
                stg = [sb(sA, f"stgA{i}", [128, 1184], F32) for i in range(2)]
                kvst = sb(sA, "kvst", [128, 1024], F32)
                knope = sb(sA, "knope", [128, 512], F32)
                kvnb = sb(sA, "kvnb", [128, 128], BF16)
                for kc in range(8):
                    sg, sk = stg[kc % 2], f"stgA{kc % 2}"
                    P.op("sp", lambda e, sg=sg, kc=kc: e.dma_start(out=sg[:, :], in_=w_in_v[:, kc, :]),
                         writes=[sk], dma_sem="d_" + sk)
                    P.op("act", lambda e, sg=sg, kc=kc: e.copy(wA[:, kc, 0:128], sg[:, 512:640]), reads=[sk], writes=["wA"])
                    P.op("act", lambda e, sg=sg, kc=kc: e.copy(wA[:, kc, 256:384], sg[:, 640:768]), reads=[sk], writes=["wA"])
                    P.op("act", lambda e, sg=sg, kc=kc: e.copy(wA[:, kc, 384:512], sg[:, 1024:1152]), reads=[sk], writes=["wA"])
                    P.op("act", lambda e, sg=sg, kc=kc: e.copy(wA[:, kc, 512:544], sg[:, 1152:1184]), reads=[sk], writes=["wA"])
                    P.op("act", lambda e, sg=sg, kc=kc: e.copy(wA[:, kc, 544:576], sg[:, 1152:1184]), reads=[sk], writes=["wA"])
                    ksrc = sg[:, 512:640].rearrange("p (g t d) -> p g t d", g=2, t=2)
                    kdst = wA[:, kc, 128:256].rearrange("p (g t d) -> p g t d", g=2, t=2)
                    P.op("dve", lambda e, a=kdst, b=ksrc: e.tensor_copy(a[:, :, 0, :], b[:, :, 1, :]), reads=[sk], writes=["wA"])
                    P.op("dve", lambda e, a=kdst, b=ksrc: e.tensor_copy(a[:, :, 1, :], b[:, :, 0, :]), reads=[sk], writes=["wA"])
                    for o_ in (576, 608):
                        P.op("dve", lambda e, sg=sg, kc=kc, o_=o_: e.tensor_copy(wA[:, kc, o_:o_ + 16], sg[:, 1168:1184]), reads=[sk], writes=["wA"])
                        P.op("dve", lambda e, sg=sg, kc=kc, o_=o_: e.tensor_copy(wA[:, kc, o_ + 16:o_ + 32], sg[:, 1152:1168]), reads=[sk], writes=["wA"])
                P.op("sp", lambda e: e.dma_start(out=kvst[:, :], in_=w_kv_up), writes=["kvst"], dma_sem="d_kvst")
                kv4 = kvst[:, :].rearrange("p (h t d) -> p h t d", h=8, t=2)
                P.op("dve", lambda e: e.tensor_copy(knope[:, :].rearrange("p (h d) -> p h d", h=8), kv4[:, :, 0, :]),
                     reads=["kvst"], writes=["knope"])
                P.op("dve", lambda e: e.tensor_copy(wvup[:, :].rearrange("p (h d) -> p h d", h=8), kv4[:, :, 1, :]),
                     reads=["kvst"], writes=["wvup"])

                def trk(e):
                    for j in range(4):
                        i = e.transpose(pX[0][:, j * 128:(j + 1) * 128], knope[:, j * 128:(j + 1) * 128], ident_f[:, :])
                    return i
                P.op("pe", trk, reads=["knope", "ident_f"], writes=["pX0"])
                P.op("act", lambda e: e.copy(WkupT[:, :, :], pX[0][:, :].rearrange("p (j r) -> p j r", j=4)),
                     reads=["pX0"], writes=["WkupT"])

                for tc in range(8):
                    c0 = tc * 512
                    load_rope(tc)
                    for t in range(4):
                        load_norm_T(tc * 4 + t, t)
                    pr, prk = proj_fm(wA, "wA", 0, 128, hTc, "hTc")
                    psw, pswk = proj_fm(wA, "wA", 128, 128, hTc, "hTc")
                    rope(pr, prk, psw, pswk, cs64, "cs64", 128, swa_kT[:, c0:c0 + 512], "swa_kT")
                    pr, prk = proj_fm(wA, "wA", 512, 64, hTc, "hTc")
                    psw, pswk = proj_fm(wA, "wA", 576, 64, hTc, "hTc")
                    rope(pr, prk, psw, pswk, cs32, "cs32", 64, krT[0:64, c0:c0 + 512], "krT")
                    for t in range(4):
                        n = tc * 4 + t
                        pp, pk = nextpx()

                        def mm(e, pp=pp, t=t):
                            for kc in range(8):
                                i = e.matmul(pp[:, 0:256], hTc[:, kc, t * 128:(t + 1) * 128], wA[:, kc, 256:512],
                                             start=(kc == 0), stop=(kc == 7))
                            return i
                        P.op("pe", mm, reads=["hTc", "wA"], writes=[pk])
                        P.op("act", lambda e, pp=pp, n=n: e.copy(swa_v[:, n, :, 0:64], pp[:, 0:128].rearrange("p (g d) -> p g d", g=2)),
                             reads=[pk], writes=["swa_v"])
                        P.op("act", lambda e, pp=pp: e.activation(tmpB[:, 0:128], pp[:, 128:256], AF.Square, accum_out=stB[:, 0:1]),
                             reads=[pk], writes=["tmpB", "stB0"])
                        P.op("act", lambda e: e.activation(stB[:, 1:2], stB[:, 0:1], AF.Sqrt, bias=EPS, scale=1.0 / 128),
                             reads=["stB0"], writes=["stB1"])
                        P.op("dve", lambda e: e.reciprocal(stB[:, 2:3], stB[:, 1:2]), reads=["stB1"], writes=["stB2"])
                        P.op("dve", lambda e, pp=pp: e.scalar_tensor_tensor(kvnb[:, :], pp[:, 128:256], stB[:, 2:3], bc_kvn[:, :],
                                                                           ALU.mult, ALU.mult),
                             reads=[pk, "stB2", "bc_kvn"], writes=["kvnb"])
                        P.op("pe", lambda e: e.transpose(pT[:, 0, :], kvnb[:, :], ident_b[:, :]),
                             reads=["kvnb", "ident_b"], writes=["pT"])
                        P.op("act", lambda e, n=n: e.copy(kvnT[:, n * 128:(n + 1) * 128], pT[:, 0, :]), reads=["pT"], writes=["kvnT"])
                        pp2, pk2 = nextpx()
                        P.op("pe", lambda e, pp2=pp2, n=n: e.matmul(pp2[:, :], kvnT[:, n * 128:(n + 1) * 128], wvup[:, :],
                                                                   start=True, stop=True),
                             reads=["kvnT", "wvup"], writes=[pk2])
                        P.op("dve", lambda e, pp2=pp2, n=n: e.tensor_copy(mla_v[:, n, :, 0:64],
                                                                         pp2[:, :].rearrange("p (h d) -> p h d", h=8)),
                             reads=[pk2], writes=["mla_v"])
                P.flush()

            with ExitStack() as sB:
                wB = sb(sB, "wB", [128, 8, 1280], BF16)
                wqn = sb(sB, "wqn", [128, 2, 512], BF16)
                wqr = sb(sB, "wqr", [128, 2, 1024], BF16)
                woutb = sb(sB, "woutb", [128, 8, 1024], BF16)
                with ExitStack() as sW:
                    stg = [sb(sW, f"stgB{i}", [128, 1184], F32) for i in range(2)]
                    for kc in range(8):
                        sg, sk = stg[kc % 2], f"stgB{kc % 2}"
                        P.op("sp", lambda e, sg=sg, kc=kc: e.dma_start(out=sg[:, :], in_=w_in_v[:, kc, :]),
                             writes=[sk], dma_sem="d_" + sk)
                        qsrc = sg[:, 0:512].rearrange("p (g j d) -> p g j d", g=2, j=4)
                        qdst = wB[:, kc, 0:512].rearrange("p (j g d) -> p g j d", j=4, g=2)
                        P.op("act", lambda e, a=qdst, b=qsrc: e.copy(a[:, 0, :, :], b[:, 0, :, :]), reads=[sk], writes=["wB"])
                        P.op("act", lambda e, a=qdst, b=qsrc: e.copy(a[:, 1, :, :], b[:, 1, :, :]), reads=[sk], writes=["wB"])
                        qsrc5 = sg[:, 0:512].rearrange("p (g j t d) -> p g j t d", g=2, j=4, t=2)
                        qdst5 = wB[:, kc, 512:1024].rearrange("p (j g t d) -> p g j t d", j=4, g=2, t=2)
                        for g_ in range(2):
                            for t_ in range(2):
                                P.op("dve", lambda e, a=qdst5, b=qsrc5, g_=g_, t_=t_: e.tensor_copy(a[:, g_, :, t_, :], b[:, g_, :, 1 - t_, :]),
                                     reads=[sk], writes=["wB"])
                        P.op("act", lambda e, sg=sg, kc=kc: e.copy(wB[:, kc, 1024:1280], sg[:, 768:1024]), reads=[sk], writes=["wB"])
                    for a_ in range(2):
                        sg, sk = stg[a_], f"stgB{a_}"
                        P.op("sp", lambda e, sg=sg, a_=a_: e.dma_start(out=sg[:, 0:768], in_=w_q_up[a_ * 128:(a_ + 1) * 128, :]),
                             writes=[sk], dma_sem="d_" + sk)
                        u3 = sg[:, 0:768].rearrange("p (h c) -> p h c", h=8)
                        P.op("act", lambda e, u3=u3, a_=a_: e.copy(wqn[:, a_, :].rearrange("p (h d) -> p h d", h=8), u3[:, :, 0:64]),
                             reads=[sk], writes=["wqn"])
                        rw4 = wqr[:, a_, 0:512].rearrange("p (h r d) -> p h r d", h=8, r=2)
                        sw4 = wqr[:, a_, 512:1024].rearrange("p (h r d) -> p h r d", h=8, r=2)
                        for r_ in range(2):
                            P.op("act", lambda e, u3=u3, rw4=rw4, r_=r_: e.copy(rw4[:, :, r_, :], u3[:, :, 64:96]), reads=[sk], writes=["wqr"])
                            P.op("dve", lambda e, u3=u3, sw4=sw4, r_=r_: e.tensor_copy(sw4[:, :, r_, 0:16], u3[:, :, 80:96]), reads=[sk], writes=["wqr"])
                            P.op("dve", lambda e, u3=u3, sw4=sw4, r_=r_: e.tensor_copy(sw4[:, :, r_, 16:32], u3[:, :, 64:80]), reads=[sk], writes=["wqr"])
                    w_out_v = w_out.rearrange("(k p) n -> p k n", p=128)
                    for kc in range(8):
                        sg, sk = stg[kc % 2], f"stgB{kc % 2}"
                        P.op("sp", lambda e, sg=sg, kc=kc: e.dma_start(out=sg[:, 0:1024], in_=w_out_v[:, kc, :]),
                             writes=[sk], dma_sem="d_" + sk)
                        if kc % 2 == 0:
                            P.op("act", lambda e, sg=sg, kc=kc: e.copy(woutb[:, kc, :], sg[:, 0:1024]), reads=[sk], writes=["woutb"])
                        else:
                            P.op("dve", lambda e, sg=sg, kc=kc: e.tensor_copy(woutb[:, kc, :], sg[:, 0:1024]), reads=[sk], writes=["woutb"])

                    P.flush()
                qT_swa = sb(sB, "qT_swa", [128, 4, 512], BF16)
                qnb = sb(sB, "qnb", [128, 256], BF16)
                qnT = sb(sB, "qnT", [128, 2, 512], BF16)
                qnopeT = sb(sB, "qnopeT", [128, 4, 512], BF16)
                qabsT = sb(sB, "qabsT", [128, 8, 512], BF16)
                qropeT = sb(sB, "qropeT", [64, 8, 512], BF16)
                E3 = [sb(sB, f"E3_{i}", [128, 3, 512], BF16) for i in range(2)]
                Em = [sb(sB, f"Em{i}", [128, 1024], BF16) for i in range(2)]
                mixed = sb(sB, "mixed", [128, 4, 1024], BF16)
                mixn = sb(sB, "mixn", [128, 1024], BF16)
                mixT = sb(sB, "mixT", [128, 8, 128], BF16)
                ssq = sb(sB, "ssq", [128, 4, 16], F32)
                sqt = sb(sB, "sqt", [128, 4, 64], F32)
                zt = sb(sB, "zt", [128, 4], F32)
                rzz = sb(sB, "rzz", [128, 4], F32)
                ssab = sb(sB, "ssab", [128, 2, 4], F32)
                rsab = sb(sB, "rsab", [128, 2, 4], F32)

                SC_SWA = 64 ** -0.5
                SC_MLA = 96 ** -0.5
                for tc in range(n_chunks):
                    c0 = tc * 512
                    load_rope(tc)
                    for t in range(4):
                        load_norm_T(tc * 4 + t, t)
                    for j in range(4):
                        pr, prk = proj_fm(wB, "wB", j * 128, 128, hTc, "hTc")
                        psw, pswk = proj_fm(wB, "wB", 512 + j * 128, 128, hTc, "hTc")
                        rope(pr, prk, psw, pswk, cs64, "cs64", 128, qT_swa[:, j, :], "qT_swa")
                    for t in range(4):
                        pp, pk = nextpx()

                        def mm(e, pp=pp, t=t):
                            for kc in range(8):
                                i = e.matmul(pp[:, 0:256], hTc[:, kc, t * 128:(t + 1) * 128], wB[:, kc, 1024:1280],
                                             start=(kc == 0), stop=(kc == 7))
                            return i
                        P.op("pe", mm, reads=["hTc", "wB"], writes=[pk])
                        P.op("act", lambda e, pp=pp: e.activation(tmpB[:, 0:256], pp[:, 0:256], AF.Square, accum_out=stB[:, 0:1]),
                             reads=[pk], writes=["tmpB", "stB0"])
                        P.op("act", lambda e: e.activation(stB[:, 1:2], stB[:, 0:1], AF.Sqrt, bias=EPS, scale=1.0 / 256),
                             reads=["stB0"], writes=["stB1"])
                        P.op("dve", lambda e: e.reciprocal(stB[:, 2:3], stB[:, 1:2]), reads=["stB1"], writes=["stB2"])
                        P.op("dve", lambda e, pp=pp: e.scalar_tensor_tensor(qnb[:, :], pp[:, 0:256], stB[:, 2:3], bc_qn[:, :],
                                                                           ALU.mult, ALU.mult),
                             reads=[pk, "stB2", "bc_qn"], writes=["qnb"])

                        def tr2(e):
                            for a_ in range(2):
                                i = e.transpose(pT[:, a_, :], qnb[:, a_ * 128:(a_ + 1) * 128], ident_b[:, :])
                            return i
                        P.op("pe", tr2, reads=["qnb", "ident_b"], writes=["pT"])
                        P.op("act", lambda e, t=t: e.copy(qnT[:, :, t * 128:(t + 1) * 128], pT[:, 0:2, :]), reads=["pT"], writes=["qnT"])
                    for j in range(4):
                        pp, pk = proj_fm(wqn, "wqn", j * 128, 128, qnT, "qnT", nk=2)
                        P.op("act", lambda e, pp=pp, j=j: e.copy(qnopeT[:, j, :], pp[:, :]), reads=[pk], writes=["qnopeT"])
                    for h in range(8):
                        j, m = h // 2, h % 2
                        pp, pk = nextpx()
                        P.op("pe", lambda e, pp=pp, j=j, m=m: e.matmul(pp[:, :], WkupT[m * 64:(m + 1) * 64, j, :],
                                                                      qnopeT[m * 64:(m + 1) * 64, j, :], start=True, stop=True),
                             reads=["WkupT", "qnopeT"], writes=[pk])
                        if h % 2 == 0:
                            P.op("act", lambda e, pp=pp, h=h: e.copy(qabsT[:, h, :], pp[:, :]), reads=[pk], writes=["qabsT"])
                        else:
                            P.op("dve", lambda e, pp=pp, h=h: e.tensor_copy(qabsT[:, h, :], pp[:, :]), reads=[pk], writes=["qabsT"])
                    for h in range(8):
                        pr, prk = proj_fm(wqr, "wqr", h * 64, 64, qnT, "qnT", nk=2)
                        psw, pswk = proj_fm(wqr, "wqr", 512 + h * 64, 64, qnT, "qnT", nk=2)
                        rope(pr, prk, psw, pswk, cs32, "cs32", 64, qropeT[0:64, h, :], "qropeT")

                    for t in range(4):
                        n = tc * 4 + t
                        js = [j for j in (n - 1, n, n + 1) if 0 <= j < NB]
                        for g in range(2):
                            e3, e3k = E3[g], f"E3_{g}"
                            for idx, j in enumerate(js):
                                psc, psk = pSs[idx % 2], f"pS{idx % 2}"
                                P.op("pe", lambda e, psc=psc, g=g, j=j, t=t: e.matmul(
                                    psc[:, :], swa_kT[g * 64:(g + 1) * 64, j * 128:(j + 1) * 128],
                                    qT_swa[g * 64:(g + 1) * 64, :, t * 128:(t + 1) * 128], start=True, stop=True),
                                    reads=["swa_kT", "qT_swa"], writes=[psk])
                                P.op("act", lambda e, psc=psc, e3=e3, idx=idx: e.activation(e3[:, idx, :], psc[:, :], AF.Exp, scale=SC_SWA),
                                     reads=[psk], writes=[e3k])
                                if j != n:
                                    mk_, mkk = (mprev, "mprev") if j == n - 1 else (mnext, "mnext")
                                    P.op("dve", lambda e, e3=e3, idx=idx, mk_=mk_: e.tensor_tensor(
                                        e3[:, idx, :].rearrange("p (h q) -> p h q", h=4),
                                        e3[:, idx, :].rearrange("p (h q) -> p h q", h=4),
                                        mk_[:, :].rearrange("p (o q) -> p o q", o=1).to_broadcast([128, 4, 128]), ALU.mult),
                                        reads=[e3k, mkk], writes=[e3k])
                            po, pok = pO[g], f"pO{g}"

                            def pv(e, po=po, e3=e3, g=g, js=js):
                                for hh in range(4):
                                    for idx, j in enumerate(js):
                                        i = e.matmul(po[:, hh, :], e3[:, idx, hh * 128:(hh + 1) * 128], swa_v[:, j, g, :],
                                                     start=(idx == 0), stop=(idx == len(js) - 1))
                                return i
                            P.op("pe", pv, reads=[e3k, "swa_v"], writes=[pok])
                            P.op("dve", lambda e, po=po, g=g: e.tensor_tensor(zt[:, :], po[:, :, 64], esink[:, 4 * g:4 * g + 4], ALU.add),
                                 reads=[pok, "esink"], writes=["zt"])
                            P.op("dve", lambda e: e.reciprocal(rzz[:, :], zt[:, :]), reads=["zt"], writes=["rzz"])
                            P.op("dve", lambda e, po=po, g=g, t=t: e.tensor_tensor(
                                sqt[:, :, :], po[:, :, 0:64],
                                rzz[:, :].rearrange("p (h o) -> p h o", o=1).to_broadcast([128, 4, 64]), ALU.mult),
                                reads=[pok, "rzz"], writes=["sqt"])
                            P.op("act", lambda e, g=g, t=t: e.copy(mixed[:, t, g * 256:(g + 1) * 256].rearrange("p (h d) -> p h d", h=4),
                                                                  sqt[:, :, :]),
                                 reads=["sqt"], writes=["mixed"])
                            P.op("dve", lambda e: e.tensor_tensor(sqt[:, :, :], sqt[:, :, :], sqt[:, :, :], ALU.mult),
                                 reads=["sqt"], writes=["sqt"])
                            P.op("dve", lambda e, g=g, t=t: e.tensor_reduce(ssq[:, t, g:g + 1], sqt[:, :, :], AX.XY, ALU.add),
                                 reads=["sqt"], writes=["ssq"])

                    sbufs = [(pSS, ["pS0", "pS1"]), (pXX, ["pX0", "pX1"])]
                    for h in range(8):
                        po, pok = pO[h % 2], f"pO{h % 2}"

                        def score(kp, h=h):
                            pst, psk = sbufs[kp % 2]

                            def mm(e):
                                for u_ in range(2):
                                    kb = kp * 2 + u_
                                    e.matmul(pst[:, u_ * 512:(u_ + 1) * 512], kvnT[:, kb * 128:(kb + 1) * 128], qabsT[:, h, :],
                                             start=True, stop=False)
                                for u_ in range(2):
                                    kb = kp * 2 + u_
                                    i = e.matmul(pst[:, u_ * 512:(u_ + 1) * 512], krT[u_ * 32:(u_ + 1) * 32, kb * 128:(kb + 1) * 128],
                                                 qropeT[u_ * 32:(u_ + 1) * 32, h, :], start=False, stop=True)
                                return i
                            P.op("pe", mm, reads=["kvnT", "krT", "qabsT", "qropeT"], writes=psk)
                        score(0)
                        for kp in range(NB // 2):
                            pst, psk = sbufs[kp % 2]
                            em, emk = Em[kp % 2], f"Em{kp % 2}"
                            P.op("act", lambda e, pst=pst, em=em: e.activation(em[:, :], pst[:, :], AF.Exp, scale=SC_MLA),
                                 reads=psk, writes=[emk])
                            if kp + 1 < NB // 2:
                                score(kp + 1)

                            def pv(e, em=em, kp=kp, po=po, h=h):
                                for u_ in range(2):
                                    kb = kp * 2 + u_
                                    for t in range(4):
                                        i = e.matmul(po[:, t, :], em[:, u_ * 512 + t * 128:u_ * 512 + (t + 1) * 128], mla_v[:, kb, h, :],
                                                     start=(kb == 0 and t == 0), stop=(kb == NB - 1), skip_group_check=True)
                                return i
                            P.op("pe", pv, reads=[emk, "mla_v"], writes=[pok])
                        P.op("dve", lambda e, po=po: e.reciprocal(rzz[:, :], po[:, :, 64]), reads=[pok], writes=["rzz"])
                        P.op("dve", lambda e, po=po: e.tensor_tensor(
                            sqt[:, :, :], po[:, :, 0:64],
                            rzz[:, :].rearrange("p (h o) -> p h o", o=1).to_broadcast([128, 4, 64]), ALU.mult),
                            reads=[pok, "rzz"], writes=["sqt"])
                        P.op("act", lambda e, h=h: e.copy(mixed[:, :, 512 + h * 64:512 + (h + 1) * 64], sqt[:, :, :]),
                             reads=["sqt"], writes=["mixed"])
                        P.op("dve", lambda e: e.tensor_tensor(sqt[:, :, :], sqt[:, :, :], sqt[:, :, :], ALU.mult),
                             reads=["sqt"], writes=["sqt"])
                        P.op("dve", lambda e, h=h: e.tensor_reduce(ssq[:, :, 2 + h], sqt[:, :, :], AX.X, ALU.add),
                             reads=["sqt"], writes=["ssq"])

                    P.op("dve", lambda e: e.tensor_reduce(ssab[:, 0, :], ssq[:, :, 0:2], AX.X, ALU.add), reads=["ssq"], writes=["ssab"])
                    P.op("dve", lambda e: e.tensor_reduce(ssab[:, 1, :], ssq[:, :, 2:10], AX.X, ALU.add), reads=["ssab", "ssq"], writes=["ssab"])
                    P.op("act", lambda e: e.activation(ssab[:, :, :], ssab[:, :, :], AF.Sqrt, bias=EPS, scale=1.0 / 512),
                         reads=["ssab"], writes=["ssab"])
                    P.op("dve", lambda e: e.reciprocal(rsab[:, :, :], ssab[:, :, :]), reads=["ssab"], writes=["rsab"])
                    for t in range(4):
                        n = tc * 4 + t
                        for gi in range(2):
                            P.op("dve", lambda e, t=t, gi=gi: e.scalar_tensor_tensor(
                                mixn[:, gi * 512:(gi + 1) * 512], mixed[:, t, gi * 512:(gi + 1) * 512], rsab[:, gi, t:t + 1],
                                bc_on[:, gi * 512:(gi + 1) * 512], ALU.mult, ALU.mult),
                                reads=["mixed", "rsab", "bc_on"], writes=["mixn"])

                        def trm(e):
                            for kc in range(8):
                                i = e.transpose(pT[:, kc, :], mixn[:, kc * 128:(kc + 1) * 128], ident_b[:, :])
                            return i
                        P.op("pe", trm, reads=["mixn", "ident_b"], writes=["pT"])
                        P.op("act", lambda e: e.copy(mixT[:, :, :], pT[:, :, :]), reads=["pT"], writes=["mixT"])
                        xt, xk = xts[n % 2], f"xa{n % 2}"
                        P.op("sp", lambda e, xt=xt, n=n: e.dma_start(out=xt[:, :], in_=x[n * 128:(n + 1) * 128, :]),
                             writes=[xk], dma_sem="d_" + xk)
                        for hf in range(2):
                            pp, pk = nextpx()

                            def mmo(e, pp=pp, hf=hf):
                                for kc in range(8):
                                    i = e.matmul(pp[:, :], mixT[:, kc, :], woutb[:, kc, hf * 512:(hf + 1) * 512],
                                                 start=(kc == 0), stop=(kc == 7))
                                return i
                            P.op("pe", mmo, reads=["mixT", "woutb"], writes=[pk])
                            P.op("dve", lambda e, pp=pp, hf=hf: e.tensor_tensor(tmpA[:, hf * 512:(hf + 1) * 512], pp[:, :],
                                                                               bc_ga[:, hf * 512:(hf + 1) * 512], ALU.mult),
                                 reads=[pk, "bc_ga"], writes=["tmpA"])
                        P.op("dve", lambda e, xt=xt: e.tensor_tensor(tmpA[:, :], tmpA[:, :], xt[:, :], ALU.add),
                             reads=["tmpA", xk], writes=["tmpA"])
                        P.op("sp", lambda e, n=n: e.dma_start(out=x1d[n * 128:(n + 1) * 128, :], in_=tmpA[:, :]),
                             reads=["tmpA"], writes=["x1d"], dma_sem="d_x1d")
                P.flush()
        x1src = x1d if do_attn else x

        if do_peer:
          with ExitStack() as st:
            wq = sb(st, "wq", [128, 8, 2048], BF16)
            bc_sc1f = sb(st, "bc_sc1f", [128, D], F32)
            bc_shf = sb(st, "bc_shf", [128, D], F32)
            bc_gf = sb(st, "bc_gf", [128, D], F32)
            bc_fin = sb(st, "bc_fin", [128, D], F32)
            for dst, dk, off in ((bc_shf, "bc_shf", 3 * D), (bc_sc1f, "bc_sc1f", 4 * D), (bc_gf, "bc_gf", 5 * D)):
                P.op("sp", lambda e, dst=dst, off=off: e.dma_start(
                    out=dst[:, :], in_=modd[0:1, off:off + D].partition_broadcast(128)),
                    reads=["modd"], writes=[dk], dma_sem="d_" + dk)
            P.op("sp", lambda e: e.dma_start(out=bc_fin[:, :], in_=final_norm.partition_broadcast(128)),
                 writes=["bc_fin"], dma_sem="d_fin")
            keysT = sb(st, "keysT", [128, 16, 128], BF16)
            NS = 16
            G = 4
            ug = [sb(st, f"ug{i}", [128, 2 * D], BF16) for i in range(NS)]
            dg = [sb(st, f"dg{i}", [128, 128], BF16) for i in range(4)]
            x1t = [sb(st, f"x1t{i}", [128, D], F32) for i in range(2)]
            hh = [sb(st, f"hh{i}", [128, D], F32) for i in range(2)]
            junkb = sb(st, "junkb", [128, D], BF16)
            junkf = sb(st, "junkf", [128, D], BF16)
            hb = sb(st, "hb", [128, D], BF16)
            hT = sb(st, "hT", [128, 8, 128], BF16)
            qT = sb(st, "qT", [128, 16, 128], BF16)
            bufS = sb(st, "bufS", [128, 2048], F32)
            bufS2 = sb(st, "bufS2", [128, 2048], F32)
            tv = sb(st, "tv", [128, 16, 16], F32)
            ti = sb(st, "ti", [128, 16, 16], U32)
            tif = sb(st, "tif", [128, 16, 16], F32)
            best = sb(st, "best", [128, 8, 16], F32)
            pos = sb(st, "pos", [128, 8, 16], U32)
            posa = sb(st, "posa", [128, 8, 16], U32)
            posb = sb(st, "posb", [128, 8, 16], U32)
            paf = sb(st, "paf", [128, 8, 16], F32)
            pbf = sb(st, "pbf", [128, 8, 16], F32)
            If = sb(st, "If", [128, 8, 16], F32)
            Jf = sb(st, "Jf", [128, 8, 16], F32)
            ef = sb(st, "ef", [128, 128], F32)
            ei = [sb(st, f"ei{i}", [128, 128], I32) for i in range(2)]
            gg = [sb(st, f"gg{i}", [128, 8, 16], F32) for i in range(2)]
            nmx = sb(st, "nmx", [128, 8], F32)
            zs = sb(st, "zs", [128, 8], F32)
            rz = sb(st, "rz", [128, 8], F32)
            Aa = sb(st, "Aa", [128, 128], F32)
            ga = sb(st, "ga", [128, 128], F32)
            ww = sb(st, "ww", [128, 128], F32)
            acc = sb(st, "acc", [128, D], F32)
            st8 = sb(st, "st8", [128, 8], F32)
            yt = sb(st, "yt", [128, D], F32)
            kst = sb(st, "kst", [128, 16, 128], F32)
            wstg = [sb(st, f"wstg{i}", [128, 2048], F32) for i in range(2)]
            pS = ps(st, "pS", [128, 2048], F32)
            pQ = ps(st, "pQ", [128, 4, 128], F32)
            pAcc = ps(st, "pAcc", [128, D], F32)
            pT = ps(st, "pT", [128, 8, 128], BF16)

            wqv = w_pq.rearrange("(k p) n -> p k n", p=128)
            for kc in range(8):
                wt = wstg[kc % 2]
                wk = f"wstg{kc % 2}"
                P.op("sp", lambda e, wt=wt, kc=kc: e.dma_start(out=wt[:, :], in_=wqv[:, kc, :]),
                     writes=[wk], dma_sem="d_" + wk)
                if kc % 2 == 0:
                    P.op("dve", lambda e, wt=wt, kc=kc: e.tensor_copy(wq[:, kc, :], wt[:, :]), reads=[wk], writes=["wq"])
                else:
                    P.op("act", lambda e, wt=wt, kc=kc: e.copy(wq[:, kc, :], wt[:, :]), reads=[wk], writes=["wq"])
            P.op("sp", lambda e: e.dma_start(out=kst[:, :, :], in_=sub_keys.rearrange("g n d -> n g d")),
                 writes=["kst"], dma_sem="d_kst")
            for g4 in range(4):
                def tr(e, g4=g4):
                    for j in range(4):
                        g = g4 * 4 + j
                        i = e.transpose(pS[:, j * 128:(j + 1) * 128], kst[:, g, :], ident_f[:, :])
                    return i
                P.op("pe", tr, reads=["kst", "ident_f"], writes=["pS"])
                P.op("act", lambda e, g4=g4: e.copy(keysT[:, g4 * 4:(g4 + 1) * 4, :],
                                                  pS[:, 0:512].rearrange("p (g n) -> p g n", g=4)),
                     reads=["pS"], writes=["keysT"])

            def top16(src, srck, dst2, dst2k, nseg, seglen, tvv, tvk, tii, tik):
                def r1(e):
                    for g in range(nseg):
                        i = e.max(tvv[:, g, 0:8], src[:, g * seglen:(g + 1) * seglen])
                    return i
                P.op("dve", r1, reads=[srck], writes=[tvk])

                def r2(e):
                    for g in range(nseg):
                        i = e.match_replace(dst2[:, g * seglen:(g + 1) * seglen], tvv[:, g, 0:8],
                                            src[:, g * seglen:(g + 1) * seglen], NEG)
                    return i
                P.op("dve", r2, reads=[srck, tvk], writes=[dst2k])

                def r3(e):
                    for g in range(nseg):
                        i = e.max(tvv[:, g, 8:16], dst2[:, g * seglen:(g + 1) * seglen])
                    return i
                P.op("dve", r3, reads=[dst2k], writes=[tvk])

                def r4(e):
                    for g in range(nseg):
                        e.max_index(tii[:, g, 0:8], tvv[:, g, 0:8], src[:, g * seglen:(g + 1) * seglen])
                        i = e.max_index(tii[:, g, 8:16], tvv[:, g, 8:16], dst2[:, g * seglen:(g + 1) * seglen])
                    return i
                P.op("dve", r4, reads=[srck, dst2k, tvk], writes=[tik])

            def sel(n):
                b = n % 2
                xt, xk = x1t[b], f"x1t{b}"
                h, hk = hh[b], f"hh{b}"
                P.op("sp", lambda e: e.dma_start(out=xt[:, :], in_=x1src[n * 128:(n + 1) * 128, :]),
                     reads=["x1d"], writes=[xk], dma_sem="d_" + xk)
                P.op("act", lambda e: e.activation(junkb[:, :], xt[:, :], AF.Square, accum_out=st8[:, 0:1]),
                     reads=[xk], writes=["junkb", "st8a"])
                P.op("act", lambda e: e.activation(st8[:, 1:2], st8[:, 0:1], AF.Sqrt, bias=EPS, scale=1.0 / D),
                     reads=["st8a"], writes=["st8b"])
                P.op("dve", lambda e: e.reciprocal(st8[:, 2:3], st8[:, 1:2]), reads=["st8b"], writes=["st8c"])
                P.op("dve", lambda e: e.scalar_tensor_tensor(h[:, :], xt[:, :], st8[:, 2:3], bc_sc1f[:, :], ALU.mult, ALU.mult),
                     reads=[xk, "st8c", "bc_sc1f"], writes=[hk])
                P.op("dve", lambda e: e.tensor_tensor(h[:, :], h[:, :], bc_shf[:, :], ALU.add),
                     reads=[hk, "bc_shf"], writes=[hk])
                P.op("act", lambda e: e.copy(hb[:, :], h[:, :]), reads=[hk], writes=["hb"])

                def tr(e):
                    for kc in range(8):
                        i = e.transpose(pT[:, kc, :], hb[:, kc * 128:(kc + 1) * 128], ident_b[:, :])
                    return i
                P.op("pe", tr, reads=["hb", "ident_b"], writes=["pT"])
                P.op("act", lambda e: e.copy(hT[:, :, :], pT[:, :, :]), reads=["pT"], writes=["hT"])
                for r in range(4):
                    def qmm(e, r=r):
                        for j in range(4):
                            g = r * 4 + j
                            for kc in range(8):
                                i = e.matmul(pQ[:, j, :], wq[:, kc, g * 128:(g + 1) * 128], hT[:, kc, :],
                                             start=(kc == 0), stop=(kc == 7))
                        return i
                    P.op("pe", qmm, reads=["wq", "hT"], writes=["pQ"])
                    P.op("act", lambda e, r=r: e.copy(qT[:, r * 4:(r + 1) * 4, :], pQ[:, :, :]), reads=["pQ"], writes=["qT"])

                def smm(e):
                    for g in range(16):
                        i = e.matmul(pS[:, g * 128:(g + 1) * 128], qT[:, g, :], keysT[:, g, :], start=True, stop=True)
                    return i
                P.op("pe", smm, reads=["qT", "keysT"], writes=["pS"])
                P.op("act", lambda e: e.copy(bufS[:, :], pS[:, :]), reads=["pS"], writes=["bufS"])
                top16(bufS, "bufS", bufS2, "bufS2", 16, 128, tv, "tv", ti, "ti")
                P.op("dve", lambda e: e.tensor_copy(tif[:, :, :], ti[:, :, :]), reads=["ti"], writes=["tif"])
                tv4 = tv[:, :, :].rearrange("p (h t) k -> p h t k", t=2)
                tif4 = tif[:, :, :].rearrange("p (h t) k -> p h t k", t=2)
                cand = bufS[:, :].rearrange("p (h a b) -> p h a b", h=8, a=16)
                P.op("dve", lambda e: e.tensor_tensor(
                    cand, tv4[:, :, 0, :].rearrange("p h (a o) -> p h a o", o=1).to_broadcast([128, 8, 16, 16]),
                    tv4[:, :, 1:2, :].to_broadcast([128, 8, 16, 16]), ALU.add),
                    reads=["tv"], writes=["bufS"])
                top16(bufS, "bufS", bufS2, "bufS2", 8, 256, best, "best", pos, "pos")
                P.op("dve", lambda e: e.tensor_scalar_mul(nmx[:, :], best[:, :, 0], -1.0), reads=["best"], writes=["nmx"])
                g_ = gg[b]
                gk = f"gg{b}"

                def ex(e):
                    for hd in range(8):
                        i = e.activation(g_[:, hd, :], best[:, hd, :], AF.Exp, bias=nmx[:, hd:hd + 1],
                                         accum_out=zs[:, hd:hd + 1])
                    return i
                P.op("act", ex, reads=["best", "nmx"], writes=[gk, "zs"])
                P.op("dve", lambda e: e.reciprocal(rz[:, :], zs[:, :]), reads=["zs"], writes=["rz"])
                P.op("dve", lambda e: e.tensor_tensor(
                    g_[:, :, :], g_[:, :, :], rz[:, :].rearrange("p (h o) -> p h o", o=1).to_broadcast([128, 8, 16]), ALU.mult),
                    reads=[gk, "rz"], writes=[gk])
                P.op("dve", lambda e: e.tensor_single_scalar(posa[:, :, :], pos[:, :, :], 4, ALU.logical_shift_right),
                     reads=["pos"], writes=["posa"])
                P.op("dve", lambda e: e.tensor_single_scalar(posb[:, :, :], pos[:, :, :], 15, ALU.bitwise_and),
                     reads=["pos"], writes=["posb"])
                P.op("dve", lambda e: e.tensor_copy(paf[:, :, :], posa[:, :, :]), reads=["posa"], writes=["paf"])
                P.op("dve", lambda e: e.tensor_copy(pbf[:, :, :], posb[:, :, :]), reads=["posb"], writes=["pbf"])
                oh = bufS[:, :].rearrange("p (h k a) -> p h k a", h=8, k=16)
                oh2 = bufS2[:, :].rearrange("p (h k a) -> p h k a", h=8, k=16)
                io4 = iota16[:, :].rearrange("p (h k a) -> p h k a", h=1, k=1).to_broadcast([128, 8, 16, 16])
                for (pf, pfk, t, dst, dstk) in ((paf, "paf", 0, If, "If"), (pbf, "pbf", 1, Jf, "Jf")):
                    P.op("dve", lambda e, pf=pf: e.tensor_tensor(
                        oh, io4, pf[:, :, :].rearrange("p h (k o) -> p h k o", o=1).to_broadcast([128, 8, 16, 16]),
                        ALU.is_equal), reads=[pfk, "iota16"], writes=["bufS"])
                    P.op("dve", lambda e, t=t: e.tensor_tensor(
                        oh2, oh, tif4[:, :, t:t + 1, :].to_broadcast([128, 8, 16, 16]), ALU.mult),
                        reads=["bufS", "tif"], writes=["bufS2"])
                    P.op("dve", lambda e, dst=dst: e.tensor_reduce(dst[:, :, :], oh2, AX.X, ALU.add),
                         reads=["bufS2"], writes=[dstk])
                P.op("dve", lambda e: e.scalar_tensor_tensor(
                    ef[:, :], If[:, :, :].rearrange("p h k -> p (h k)"), 128.0,
                    Jf[:, :, :].rearrange("p h k -> p (h k)"), ALU.mult, ALU.add),
                    reads=["If", "Jf"], writes=["ef"])
                P.op("dve", lambda e: e.tensor_copy(ei[b][:, :], ef[:, :]), reads=["ef"], writes=[f"ei{b}"])

            slot = [0]

            def gather(n_b, j):
                s_ = slot[0] % NS
                slot[0] += 1
                P.op("pool", lambda e: e.indirect_dma_start(
                    out=ug[s_][:, :], out_offset=None, in_=uvd,
                    in_offset=bass.IndirectOffsetOnAxis(ap=ei[n_b][:, j:j + 1], axis=0)),
                    reads=[f"ei{n_b}"] + UVK, writes=[f"ug{s_}"], dma_sem=f"d_ug{s_}")
                return s_

            dgi = [0]

            def experts(n):
                b = n % 2
                xt, xk = x1t[b], f"x1t{b}"
                h, hk = hh[b], f"hh{b}"
                gflat = gg[b][:, :, :].rearrange("p h k -> p (h k)")
                ngrp = 128 // G
                pend = None

                def vside(grp, slots):
                    cs = slice(grp * G, (grp + 1) * G)
                    kq = grp % 4
                    P.op("dve", lambda e: e.tensor_tensor(ww[:, cs], ga[:, cs], gflat[:, cs], ALU.mult),
                         reads=[f"ga{kq}", f"gg{b}"], writes=[f"ww{kq}"])
                    for jj, s_ in enumerate(slots):
                        j = grp * G + jj
                        di = dgi[0] % 4
                        dgi[0] += 1
                        P.op("act", lambda e, di=di, j=j: e.activation(dg[di][:, :], ident_b[:, :], AF.Copy, scale=ww[:, j:j + 1]),
                             reads=[f"ww{kq}", "ident_b"], writes=[f"dg{di}"])

                        def mm(e, di=di, s_=s_, j=j):
                            e.matmul(pAcc[:, 0:512], dg[di][:, :], ug[s_][:, D:D + 512], start=(j == 0), stop=(j == 127))
                            return e.matmul(pAcc[:, 512:1024], dg[di][:, :], ug[s_][:, D + 512:2 * D], start=(j == 0), stop=(j == 127))
                        P.op("pe", mm, reads=[f"dg{di}", f"ug{s_}"], writes=["pAcc"])

                for grp in range(ngrp):
                    cs = slice(grp * G, (grp + 1) * G)
                    kq = grp % 4
                    slots = []
                    for jj in range(G):
                        j = grp * G + jj
                        s_ = gather(b, j)
                        slots.append(s_)
                        P.op("dve", lambda e, s_=s_, j=j: e.scalar_tensor_tensor(
                            junkf[:, :], ug[s_][:, 0:D], 1.0, h[:, :], ALU.mult, ALU.mult, accum_out=Aa[:, j:j + 1]),
                            reads=[f"ug{s_}", hk], writes=["junkf", f"Aa{kq}"])
                    P.op("act", lambda e, cs=cs: e.activation(ga[:, cs], Aa[:, cs], AF.Gelu), reads=[f"Aa{kq}"], writes=[f"ga{kq}"])
                    if pend is not None:
                        vside(*pend)
                    pend = (grp, slots)
                    if grp >= 1:
                        k_ = (len(pending_sel) + (ngrp - 1 - grp) - 1) // max(ngrp - 1 - grp, 1) if grp < ngrp - 1 else len(pending_sel)
                        P.replay(pending_sel[:k_])
                        del pending_sel[:k_]
                vside(*pend)
                P.op("dve", lambda e: e.tensor_tensor(acc[:, :], pAcc[:, :], bc_gf[:, :], ALU.mult),
                     reads=["pAcc", "bc_gf"], writes=["acc"])
                P.op("dve", lambda e: e.tensor_tensor(acc[:, :], acc[:, :], xt[:, :], ALU.add),
                     reads=["acc", xk], writes=["acc"])
                P.op("act", lambda e: e.activation(junkb[:, :], acc[:, :], AF.Square, accum_out=st8[:, 3:4]),
                     reads=["acc"], writes=["junkb", "st8d"])
                P.op("act", lambda e: e.activation(st8[:, 4:5], st8[:, 3:4], AF.Sqrt, bias=EPS, scale=1.0 / D),
                     reads=["st8d"], writes=["st8e"])
                P.op("dve", lambda e: e.reciprocal(st8[:, 5:6], st8[:, 4:5]), reads=["st8e"], writes=["st8f"])
                P.op("dve", lambda e: e.scalar_tensor_tensor(yt[:, :], acc[:, :], st8[:, 5:6], bc_fin[:, :], ALU.mult, ALU.mult),
                     reads=["acc", "st8f", "bc_fin"], writes=["yt"])
                P.op("sp", lambda e: e.dma_start(out=y[n * 128:(n + 1) * 128, :], in_=yt[:, :]),
                     reads=["yt"], writes=["y_out"], dma_sem="d_y")

            pending_sel = []
            sel(0)
            for n in range(n_blocks):
                if n + 1 < n_blocks:
                    pending_sel.extend(P.capture(lambda: sel(n + 1)))
                experts(n)
                P.replay(pending_sel)
                del pending_sel[:]
            P.flush()
    return nc


def _rope_tables():
    pos = np.arange(S, dtype=np.float32)
    out = {}
    for dim, name in ((64, "rope64"), (32, "rope32")):
        inv = (1.0 / (10000.0 ** (np.arange(0, dim, 2, dtype=np.float32) / dim))).astype(np.float32)
        ang = pos[:, None] * inv[None, :]
        cos = np.cos(ang).astype(np.float32).T
        sin = np.sin(ang).astype(np.float32).T
        cosf = np.concatenate([cos, cos], axis=0)
        sinf = np.concatenate([-sin, sin], axis=0)
        cosf = np.concatenate([cosf, cosf], axis=0)
        sinf = np.concatenate([sinf, sinf], axis=0)
        out[name] = np.ascontiguousarray(np.stack([cosf, sinf], axis=0))
    return out


def make_in_maps(inputs, n_cores=8):
    g = lambda k: np.ascontiguousarray(np.asarray(inputs[k], dtype=np.float32))
    rt = _rope_tables()
    shared = {
        "w_ada": g("w_ada")[0], "b_ada": g("b_ada")[0][None, :], "w_in": g("w_in")[0],
        "swa_sink": g("swa_sink")[0][None, :], "mla_q_norm": g("mla_q_norm")[0][None, :],
        "w_q_up": g("w_mla_q_up")[0], "mla_kv_norm": g("mla_kv_norm")[0][None, :],
        "w_kv_up": g("w_mla_kv_up")[0],
        "out_norm": np.ascontiguousarray(np.concatenate([g("out_norm_swa")[0], g("out_norm_mla")[0]])[None, :]),
        "w_out": g("w_out")[0], "w_pq": g("w_peer_query")[0],
        "sub_keys": np.ascontiguousarray(g("peer_sub_keys")[0].reshape(16, 128, 128)),
        "exp_u": g("peer_expert_u")[0], "exp_v": g("peer_expert_v")[0],
        "final_norm": g("final_norm")[None, :], "rope64": rt["rope64"], "rope32": rt["rope32"],
    }
    xs = g("x")
    cs = g("c")
    maps = []
    for i in range(n_cores):
        m = dict(shared)
        m["x"] = np.ascontiguousarray(xs[i])
        m["c"] = np.ascontiguousarray(cs[i].reshape(8, 128))
        maps.append(m)
    return maps


def kernel(**inputs):
    nc = build()
    in_maps = make_in_maps(inputs, 8)
    res = run_bass_kernel_spmd(nc, in_maps, core_ids=list(range(8)))
    return np.stack([np.asarray(r["y"]).reshape(S, D) for r in res.results], axis=0).astype(np.float32)
```

```python
from contextlib import ExitStack
import numpy as np
import concourse.bass as bass
import concourse.mybir as mybir
from concourse.bass_utils import run_bass_kernel_spmd

F32 = mybir.dt.float32
BF16 = mybir.dt.bfloat16
I32 = mybir.dt.int32
U32 = mybir.dt.uint32
AF = mybir.ActivationFunctionType
ALU = mybir.AluOpType
AX = mybir.AxisListType

S = 4096
D = 1024
NB = S // 128
EPS = 1e-6
NEG = -1e30


class Prog:
    ENGS = ("pe", "act", "dve", "pool", "sp")

    def __init__(self, nc, stack):
        self.nc = nc
        self.stack = stack
        self.ops = {e: [] for e in self.ENGS}
        self.sem = {}
        self.cnt = {}
        self.waited = {e: {} for e in self.ENGS}
        self.lastw = {}
        self.reads = {}
        self.defer = None
        for e in self.ENGS:
            self.newsem("c_" + e)

    def capture(self, fn):
        self.defer = []
        fn()
        lst, self.defer = self.defer, None
        return lst

    def replay(self, thunks):
        for t in thunks:
            self.op(*t)

    def newsem(self, name):
        if name not in self.sem:
            self.sem[name] = self.stack.enter_context(self.nc.semaphore(name))
            self.cnt[name] = 0
        return name

    def op(self, eng, fn, reads=(), writes=(), dma_sem=None):
        if self.defer is not None:
            self.defer.append((eng, fn, tuple(reads), tuple(writes), dma_sem))
            return None
        waits = {}

        def need(tok):
            if tok is None:
                return
            s, v = tok
            if waits.get(s, 0) < v:
                waits[s] = v

        for k in reads:
            need(self.lastw.get(k))
        for k in writes:
            need(self.lastw.get(k))
            for s, v in self.reads.get(k, {}).items():
                need((s, v))
        w = []
        for s, v in waits.items():
            if self.waited[eng].get(s, 0) < v:
                self.waited[eng][s] = v
                w.append((s, v))
        if dma_sem is None:
            s = "c_" + eng
            inc = 1
        else:
            s = self.newsem(dma_sem)
            inc = 16
        self.cnt[s] += inc
        tok = (s, self.cnt[s])
        for k in writes:
            self.lastw[k] = tok
            self.reads[k] = {}
        for k in reads:
            self.reads.setdefault(k, {})[s] = self.cnt[s]
        self.ops[eng].append((fn, w, s, inc))
        return tok

    def flush(self):
        nc = self.nc
        final = dict(self.cnt)
        with nc.Block() as block:
            def mk(engname):
                def body(eng):
                    for fn, w, s, inc in self.ops[engname]:
                        for ws, wv in w:
                            eng.wait_ge(self.sem[ws], wv)
                        fn(eng).then_inc(self.sem[s], inc)
                    for s, v in final.items():
                        if v > 0 and self.waited[engname].get(s, 0) < v:
                            eng.wait_ge(self.sem[s], v)
                            self.waited[engname][s] = v
                return body
            block.tensor(mk("pe"))
            block.scalar(mk("act"))
            block.vector(mk("dve"))
            block.gpsimd(mk("pool"))
            block.sync(mk("sp"))
        self.ops = {e: [] for e in self.ENGS}


def build(n_blocks=NB, do_attn=True, do_peer=True, n_chunks=8):
    nc = bass.Bass("TRN2", target_bir_lowering=False)
    dt_in = lambda name, shape: nc.dram_tensor(name, list(shape), F32, kind="ExternalInput").ap()
    x = dt_in("x", [S, D])
    c = dt_in("c", [8, 128])
    w_ada = dt_in("w_ada", [D, 6 * D])
    b_ada = dt_in("b_ada", [1, 6 * D])
    w_in = dt_in("w_in", [D, 1184])
    swa_sink = dt_in("swa_sink", [1, 8])
    mla_q_norm = dt_in("mla_q_norm", [1, 256])
    w_q_up = dt_in("w_q_up", [256, 768])
    mla_kv_norm = dt_in("mla_kv_norm", [1, 128])
    w_kv_up = dt_in("w_kv_up", [128, 1024])
    out_norm = dt_in("out_norm", [1, 1024])
    w_out = dt_in("w_out", [D, D])
    w_pq = dt_in("w_pq", [D, 2048])
    sub_keys = dt_in("sub_keys", [16, 128, 128])
    exp_u = dt_in("exp_u", [16384, D])
    exp_v = dt_in("exp_v", [16384, D])
    final_norm = dt_in("final_norm", [1, D])
    rope64 = dt_in("rope64", [2, 128, S])
    rope32 = dt_in("rope32", [2, 64, S])
    y = nc.dram_tensor("y", [S, D], F32, kind="ExternalOutput").ap()
    x1d = nc.dram_tensor("x1d", [S, D], F32, kind="Internal").ap()
    modd = nc.dram_tensor("modd", [1, 6 * D], F32, kind="Internal").ap()
    uvd = nc.dram_tensor("uvd", [16384, 2 * D], BF16, kind="Internal").ap()
    UVK = [f"uvd{i}" for i in range(64)]

    with ExitStack() as gs:
        P = Prog(nc, gs)
        sb = lambda st, name, shape, dt: st.enter_context(nc.sbuf_tensor(name, list(shape), dt))
        ps = lambda st, name, shape, dt: st.enter_context(nc.psum_tensor(name, list(shape), dt))

        ident_f = sb(gs, "ident_f", [128, 128], F32)
        ident_b = sb(gs, "ident_b", [128, 128], BF16)
        dif_i = sb(gs, "dif_i", [128, 128], I32)
        ones_f = sb(gs, "ones_f", [1, 128], F32)
        fm_a = sb(gs, "fm_a", [128, 16], F32)
        iota16 = sb(gs, "iota16", [128, 16], F32)

        cast_done = set()

        def issue_cast(i, rd):
            if not do_peer or i in cast_done or i >= 64:
                return
            cast_done.add(i)
            blk, ti_ = i // 2, i % 2
            tbl, off = ((exp_u, 0), (exp_v, D))[ti_]
            P.op("pool", lambda e: e.dma_start(out=uvd[blk * 512:(blk + 1) * 512, off:off + D],
                                               in_=tbl[blk * 512:(blk + 1) * 512, :]),
                 reads=rd, writes=[UVK[i]], dma_sem="d_uvd")

        with ExitStack() as st:
            c8 = sb(st, "c8", [8, 128], F32)
            modrow = sb(st, "modrow", [1, 6 * D], F32)
            cT = sb(st, "cT", [128, 8], F32)
            brow = sb(st, "brow", [1, 6 * D], F32)
            wst = [sb(st, f"wst{i}", [128, 8, 512], F32) for i in range(2)]
            iota_i = sb(st, "iota_i", [128, 16], I32)
            pA = ps(st, "pA", [128, 512], F32)
            pB = ps(st, "pB", [128, 512], F32)

            P.op("pool", lambda e: e.iota(dif_i[:, :], [[1, 128]], 0, -1), writes=["dif_i"])
            P.op("pool", lambda e: e.iota(iota_i[:, :], [[1, 16]], 0, 0), writes=["iota_i"])
            P.op("dve", lambda e: e.tensor_copy(iota16[:, :], iota_i[:, :]), reads=["iota_i"], writes=["iota16"])
            P.op("dve", lambda e: e.tensor_single_scalar(ident_f[:, :], dif_i[:, :], 0.0, ALU.is_equal),
                 reads=["dif_i"], writes=["ident_f"])
            P.op("dve", lambda e: e.tensor_single_scalar(ident_b[:, :], dif_i[:, :], 0.0, ALU.is_equal),
                 reads=["dif_i"], writes=["ident_b"])
            P.op("dve", lambda e: e.memset(ones_f[:, :], 1.0), writes=["ones_f"])
            P.op("sp", lambda e: e.dma_start(out=c8[:, :], in_=c), writes=["c8"], dma_sem="d_c8")
            P.op("sp", lambda e: e.dma_start(out=brow[:, :], in_=b_ada), writes=["brow"], dma_sem="d_brow")
            P.op("pe", lambda e: e.transpose(pA[:, 0:8], c8[:, :], ident_f[0:8, 0:8]),
                 reads=["c8", "ident_f"], writes=["pA"])
            P.op("act", lambda e: e.activation(cT[:, :], pA[:, 0:8], AF.Silu), reads=["pA"], writes=["cT"])
            wv = w_ada.rearrange("(k p) n -> p k n", p=128)
            pbuf = [pA, pB]
            for nb in range(12):
                wt = wst[nb % 2]
                wk = f"wst{nb % 2}"
                pp = pbuf[nb % 2]
                pk = "pA" if nb % 2 == 0 else "pB"
                P.op("sp", lambda e, wt=wt, nb=nb: e.dma_start(out=wt[:, :, :], in_=wv[:, :, nb * 512:(nb + 1) * 512]),
                     writes=[wk], dma_sem="d_" + wk)

                def mm(e, wt=wt, pp=pp):
                    for kc in range(8):
                        i = e.matmul(pp[0:1, :], cT[:, kc:kc + 1], wt[:, kc, :], start=(kc == 0), stop=(kc == 7))
                    return i
                P.op("pe", mm, reads=[wk, "cT"], writes=[pk])
                P.op("dve", lambda e, pp=pp, nb=nb: e.tensor_tensor(
                    modrow[0:1, nb * 512:(nb + 1) * 512], pp[0:1, :], brow[0:1, nb * 512:(nb + 1) * 512], ALU.add),
                    reads=[pk, "brow"], writes=["modrow"])
            P.op("dve", lambda e: e.tensor_scalar_add(modrow[0:1, D:2 * D], modrow[0:1, D:2 * D], 1.0),
                 reads=["modrow"], writes=["modrow"])
            P.op("dve", lambda e: e.tensor_scalar_add(modrow[0:1, 4 * D:5 * D], modrow[0:1, 4 * D:5 * D], 1.0),
                 reads=["modrow"], writes=["modrow"])
            P.op("sp", lambda e: e.dma_start(out=modd, in_=modrow[0:1, :]), reads=["modrow"], writes=["modd"],
                 dma_sem="d_modd")
            def fmm(e):
                for j in range(16):
                    off = (D if j < 8 else 0) + (j % 8) * 128
                    i = e.matmul(pA[:, j:j + 1], modrow[0:1, off:off + 128], ones_f[0:1, 0:1], start=True, stop=True)
                return i
            P.op("pe", fmm, reads=["modrow", "ones_f"], writes=["pA"])
            P.op("act", lambda e: e.copy(fm_a[:, :], pA[:, 0:16]), reads=["pA"], writes=["fm_a"])
            P.flush()

        if do_attn:
          with ExitStack() as sa:
            kvnT = sb(sa, "kvnT", [128, S], BF16)
            krT = sb(sa, "krT", [64, S], BF16)
            mla_v = sb(sa, "mla_v", [128, NB, 8, 65], BF16)
            swa_kT = sb(sa, "swa_kT", [128, S], BF16)
            swa_v = sb(sa, "swa_v", [128, NB, 2, 65], BF16)
            bc_ga = sb(sa, "bc_ga", [128, D], F32)
            bc_on = sb(sa, "bc_on", [128, D], F32)
            bc_qn = sb(sa, "bc_qn", [128, 256], F32)
            bc_kvn = sb(sa, "bc_kvn", [128, 128], F32)
            esink = sb(sa, "esink", [128, 8], F32)
            mprev = sb(sa, "mprev", [128, 128], BF16)
            mnext = sb(sa, "mnext", [128, 128], BF16)
            WkupT = sb(sa, "WkupT", [128, 4, 128], BF16)
            wvup = sb(sa, "wvup", [128, 512], BF16)
            xts = [sb(sa, f"xa{i}", [128, D], F32) for i in range(2)]
            xnb = sb(sa, "xnb", [128, D], BF16)
            hTc = sb(sa, "hTc", [128, 8, 512], BF16)
            cs64 = sb(sa, "cs64", [128, 2, 512], F32)
            cs32 = sb(sa, "cs32", [64, 2, 512], F32)
            tmpA = sb(sa, "tmpA", [128, 1024], F32)
            tmpB = sb(sa, "tmpB", [128, 512], F32)
            stA = sb(sa, "stA", [128, 8], F32)
            stB = sb(sa, "stB", [128, 8], F32)
            pT = ps(sa, "pTa", [128, 8, 128], BF16)
            pXX = ps(sa, "pXX", [128, 1024], F32)
            pSS = ps(sa, "pSS", [128, 1024], F32)
            pX = [pXX[:, i * 512:(i + 1) * 512] for i in range(2)]
            pSs = [pSS[:, i * 512:(i + 1) * 512] for i in range(2)]
            pO = [ps(sa, f"pO{i}", [128, 4, 65], F32) for i in range(2)]
            pSW = ps(sa, "pSW", [128, 512], F32)

            def bload(dst, dk, src):
                P.op("sp", lambda e: e.dma_start(out=dst[:, :], in_=src.partition_broadcast(128)),
                     reads=["modd"], writes=[dk], dma_sem="d_" + dk)
            bload(bc_ga, "bc_ga", modd[0:1, 2 * D:3 * D])
            bload(bc_on, "bc_on", out_norm)
            bload(bc_qn, "bc_qn", mla_q_norm)
            bload(bc_kvn, "bc_kvn", mla_kv_norm)
            bload(esink, "esink", swa_sink)
            P.op("act", lambda e: e.activation(esink[:, :], esink[:, :], AF.Exp), reads=["esink"], writes=["esink"])
            P.op("dve", lambda e: e.tensor_single_scalar(mprev[:, :], dif_i[:, :], 0.0, ALU.is_le),
                 reads=["dif_i"], writes=["mprev"])
            P.op("dve", lambda e: e.tensor_single_scalar(mnext[:, :], dif_i[:, :], 0.0, ALU.is_ge),
                 reads=["dif_i"], writes=["mnext"])
            P.op("dve", lambda e: e.memset(mla_v[:, :, :, 64:65], 1.0), writes=["mla_v"])
            P.op("dve", lambda e: e.memset(swa_v[:, :, :, 64:65], 1.0), writes=["swa_v"])

            pxi = [0]

            def nextpx():
                i = pxi[0] % 2
                pxi[0] += 1
                return pX[i], f"pX{i}"

            def load_norm_T(n, t):
                xt, xk = xts[n % 2], f"xa{n % 2}"
                P.op("sp", lambda e: e.dma_start(out=xt[:, :], in_=x[n * 128:(n + 1) * 128, :]),
                     writes=[xk], dma_sem="d_" + xk)
                P.op("act", lambda e: e.activation(xnb[:, :], xt[:, :], AF.Square, accum_out=stA[:, 0:1]),
                     reads=[xk], writes=["xnb", "stA0"])
                P.op("act", lambda e: e.activation(stA[:, 1:2], stA[:, 0:1], AF.Sqrt, bias=EPS, scale=1.0 / D),
                     reads=["stA0"], writes=["stA1"])
                P.op("dve", lambda e: e.reciprocal(stA[:, 2:3], stA[:, 1:2]), reads=["stA1"], writes=["stA2"])
                P.op("act", lambda e: e.activation(xnb[:, :], xt[:, :], AF.Copy, scale=stA[:, 2:3]),
                     reads=[xk, "stA2"], writes=["xnb"])

                def tr(e):
                    for kc in range(8):
                        i = e.transpose(pT[:, kc, :], xnb[:, kc * 128:(kc + 1) * 128], ident_b[:, :])
                    return i
                P.op("pe", tr, reads=["xnb", "ident_b"], writes=["pT"])

                def ev(e):
                    for kc in range(8):
                        i = e.activation(hTc[:, kc, t * 128:(t + 1) * 128], pT[:, kc, :], AF.Identity,
                                         bias=fm_a[:, 8 + kc:9 + kc], scale=fm_a[:, kc:kc + 1])
                    return i
                P.op("act", ev, reads=["pT", "fm_a"], writes=["hTc"])

            def proj_fm(w, wk, c0, M, rhs, rhsk, nk=8):
                pp, pk = nextpx()

                def mm(e):
                    for kc in range(nk):
                        i = e.matmul(pp[0:M, :], w[:, kc, c0:c0 + M], rhs[:, kc, :], start=(kc == 0), stop=(kc == nk - 1))
                    return i
                P.op("pe", mm, reads=[wk, rhsk], writes=[pk])
                return pp, pk

            def rope(pr, prk, psw, pswk, cs, csk, M, dst, dstk):
                P.op("dve", lambda e: e.tensor_tensor(tmpA[0:M, 0:512], pr[0:M, :], cs[0:M, 0, :], ALU.mult),
                     reads=[prk, csk], writes=["tmpA"])
                P.op("dve", lambda e: e.tensor_tensor(tmpB[0:M, 0:512], psw[0:M, :], cs[0:M, 1, :], ALU.mult),
                     reads=[pswk, csk], writes=["tmpB"])
                P.op("dve", lambda e: e.tensor_tensor(dst, tmpA[0:M, 0:512], tmpB[0:M, 0:512], ALU.add),
                     reads=["tmpA", "tmpB"], writes=[dstk])

            def load_rope(tc):
                c0 = tc * 512
                P.op("sp", lambda e: e.dma_start(out=cs64[:, :, :], in_=rope64[:, :, c0:c0 + 512].rearrange("t p s -> p t s")),
                     writes=["cs64"], dma_sem="d_cs64")
                P.op("sp", lambda e: e.dma_start(out=cs32[:, :, :], in_=rope32[:, :, c0:c0 + 512].rearrange("t p s -> p t s")),
                     writes=["cs32"], dma_sem="d_cs32")

            w_in_v = w_in.rearrange("(k p) n -> p k n", p=128)

            with ExitStack() as sA:
                wA = sb(sA, "wA", [128, 8, 640], BF16)
                stg = [sb(sA, f"stgA{i}", [128, 1184], F32) for i in range(2)]
                kvst = sb(sA, "kvst", [128, 1024], F32)
                knope = sb(sA, "knope", [128, 512], F32)
                kvnb = sb(sA, "kvnb", [128, 128], BF16)
                for kc in range(8):
                    sg, sk = stg[kc % 2], f"stgA{kc % 2}"
                    P.op("sp", lambda e, sg=sg, kc=kc: e.dma_start(out=sg[:, :], in_=w_in_v[:, kc, :]),
                         writes=[sk], dma_sem="d_" + sk)
                    P.op("act", lambda e, sg=sg, kc=kc: e.copy(wA[:, kc, 0:128], sg[:, 512:640]), reads=[sk], writes=["wA"])
                    P.op("act", lambda e, sg=sg, kc=kc: e.copy(wA[:, kc, 256:384], sg[:, 640:768]), reads=[sk], writes=["wA"])
                    P.op("act", lambda e, sg=sg, kc=kc: e.copy(wA[:, kc, 384:512], sg[:, 1024:1152]), reads=[sk], writes=["wA"])
                    P.op("act", lambda e, sg=sg, kc=kc: e.copy(wA[:, kc, 512:544], sg[:, 1152:1184]), reads=[sk], writes=["wA"])
                    P.op("act", lambda e, sg=sg, kc=kc: e.copy(wA[:, kc, 544:576], sg[:, 1152:1184]), reads=[sk], writes=["wA"])
                    ksrc = sg[:, 512:640].rearrange("p (g t d) -> p g t d", g=2, t=2)
                    kdst = wA[:, kc, 128:256].rearrange("p (g t d) -> p g t d", g=2, t=2)
                    P.op("dve", lambda e, a=kdst, b=ksrc: e.tensor_copy(a[:, :, 0, :], b[:, :, 1, :]), reads=[sk], writes=["wA"])
                    P.op("dve", lambda e, a=kdst, b=ksrc: e.tensor_copy(a[:, :, 1, :], b[:, :, 0, :]), reads=[sk], writes=["wA"])
                    for o_ in (576, 608):
                        P.op("dve", lambda e, sg=sg, kc=kc, o_=o_: e.tensor_copy(wA[:, kc, o_:o_ + 16], sg[:, 1168:1184]), reads=[sk], writes=["wA"])
                        P.op("dve", lambda e, sg=sg, kc=kc, o_=o_: e.tensor_copy(wA[:, kc, o_ + 16:o_ + 32], sg[:, 1152:1168]), reads=[sk], writes=["wA"])
                P.op("sp", lambda e: e.dma_start(out=kvst[:, :], in_=w_kv_up), writes=["kvst"], dma_sem="d_kvst")
                kv4 = kvst[:, :].rearrange("p (h t d) -> p h t d", h=8, t=2)
                P.op("dve", lambda e: e.tensor_copy(knope[:, :].rearrange("p (h d) -> p h d", h=8), kv4[:, :, 0, :]),
                     reads=["kvst"], writes=["knope"])
                P.op("dve", lambda e: e.tensor_copy(wvup[:, :].rearrange("p (h d) -> p h d", h=8), kv4[:, :, 1, :]),
                     reads=["kvst"], writes=["wvup"])

                def trk(e):
                    for j in range(4):
                        i = e.transpose(pX[0][:, j * 128:(j + 1) * 128], knope[:, j * 128:(j + 1) * 128], ident_f[:, :])
                    return i
                P.op("pe", trk, reads=["knope", "ident_f"], writes=["pX0"])
                P.op("act", lambda e: e.copy(WkupT[:, :, :], pX[0][:, :].rearrange("p (j r) -> p j r", j=4)),
                     reads=["pX0"], writes=["WkupT"])

                for tc in range(8):
                    c0 = tc * 512
                    load_rope(tc)
                    for t in range(4):
                        load_norm_T(tc * 4 + t, t)
                    pr, prk = proj_fm(wA, "wA", 0, 128, hTc, "hTc")
                    psw, pswk = proj_fm(wA, "wA", 128, 128, hTc, "hTc")
                    rope(pr, prk, psw, pswk, cs64, "cs64", 128, swa_kT[:, c0:c0 + 512], "swa_kT")
                    pr, prk = proj_fm(wA, "wA", 512, 64, hTc, "hTc")
                    psw, pswk = proj_fm(wA, "wA", 576, 64, hTc, "hTc")
                    rope(pr, prk, psw, pswk, cs32, "cs32", 64, krT[0:64, c0:c0 + 512], "krT")
                    for t in range(4):
                        n = tc * 4 + t
                        pp, pk = nextpx()

                        def mm(e, pp=pp, t=t):
                            for kc in range(8):
                                i = e.matmul(pp[:, 0:256], hTc[:, kc, t * 128:(t + 1) * 128], wA[:, kc, 256:512],
                                             start=(kc == 0), stop=(kc == 7))
                            return i
                        P.op("pe", mm, reads=["hTc", "wA"], writes=[pk])
                        P.op("act", lambda e, pp=pp, n=n: e.copy(swa_v[:, n, :, 0:64], pp[:, 0:128].rearrange("p (g d) -> p g d", g=2)),
                             reads=[pk], writes=["swa_v"])
                        P.op("act", lambda e, pp=pp: e.activation(tmpB[:, 0:128], pp[:, 128:256], AF.Square, accum_out=stB[:, 0:1]),
                             reads=[pk], writes=["tmpB", "stB0"])
                        P.op("act", lambda e: e.activation(stB[:, 1:2], stB[:, 0:1], AF.Sqrt, bias=EPS, scale=1.0 / 128),
                             reads=["stB0"], writes=["stB1"])
                        P.op("dve", lambda e: e.reciprocal(stB[:, 2:3], stB[:, 1:2]), reads=["stB1"], writes=["stB2"])
                        P.op("dve", lambda e, pp=pp: e.scalar_tensor_tensor(kvnb[:, :], pp[:, 128:256], stB[:, 2:3], bc_kvn[:, :],
                                                                           ALU.mult, ALU.mult),
                             reads=[pk, "stB2", "bc_kvn"], writes=["kvnb"])
                        P.op("pe", lambda e: e.transpose(pT[:, 0, :], kvnb[:, :], ident_b[:, :]),
                             reads=["kvnb", "ident_b"], writes=["pT"])
                        P.op("act", lambda e, n=n: e.copy(kvnT[:, n * 128:(n + 1) * 128], pT[:, 0, :]), reads=["pT"], writes=["kvnT"])
                        pp2, pk2 = nextpx()
                        P.op("pe", lambda e, pp2=pp2, n=n: e.matmul(pp2[:, :], kvnT[:, n * 128:(n + 1) * 128], wvup[:, :],
                                                                   start=True, stop=True),
                             reads=["kvnT", "wvup"], writes=[pk2])
                        P.op("dve", lambda e, pp2=pp2, n=n: e.tensor_copy(mla_v[:, n, :, 0:64],
                                                                         pp2[:, :].rearrange("p (h d) -> p h d", h=8)),
                             reads=[pk2], writes=["mla_v"])
                P.flush()

            with ExitStack() as sB:
                wB = sb(sB, "wB", [128, 8, 1280], BF16)
                wqn = sb(sB, "wqn", [128, 2, 512], BF16)
                wqr = sb(sB, "wqr", [128, 2, 1024], BF16)
                woutb = sb(sB, "woutb", [128, 8, 1024], BF16)
                with ExitStack() as sW:
                    stg = [sb(sW, f"stgB{i}", [128, 1184], F32) for i in range(2)]
                    for kc in range(8):
                        sg, sk = stg[kc % 2], f"stgB{kc % 2}"
                        P.op("sp", lambda e, sg=sg, kc=kc: e.dma_start(out=sg[:, :], in_=w_in_v[:, kc, :]),
                             writes=[sk], dma_sem="d_" + sk)
                        qsrc = sg[:, 0:512].rearrange("p (g j d) -> p g j d", g=2, j=4)
                        qdst = wB[:, kc, 0:512].rearrange("p (j g d) -> p g j d", j=4, g=2)
                        P.op("act", lambda e, a=qdst, b=qsrc: e.copy(a[:, 0, :, :], b[:, 0, :, :]), reads=[sk], writes=["wB"])
                        P.op("act", lambda e, a=qdst, b=qsrc: e.copy(a[:, 1, :, :], b[:, 1, :, :]), reads=[sk], writes=["wB"])
                        qsrc5 = sg[:, 0:512].rearrange("p (g j t d) -> p g j t d", g=2, j=4, t=2)
                        qdst5 = wB[:, kc, 512:1024].rearrange("p (j g t d) -> p g j t d", j=4, g=2, t=2)
                        for g_ in range(2):
                            for t_ in range(2):
                                P.op("dve", lambda e, a=qdst5, b=qsrc5, g_=g_, t_=t_: e.tensor_copy(a[:, g_, :, t_, :], b[:, g_, :, 1 - t_, :]),
                                     reads=[sk], writes=["wB"])
                        P.op("act", lambda e, sg=sg, kc=kc: e.copy(wB[:, kc, 1024:1280], sg[:, 768:1024]), reads=[sk], writes=["wB"])
                    for a_ in range(2):
                        sg, sk = stg[a_], f"stgB{a_}"
                        P.op("sp", lambda e, sg=sg, a_=a_: e.dma_start(out=sg[:, 0:768], in_=w_q_up[a_ * 128:(a_ + 1) * 128, :]),
                             writes=[sk], dma_sem="d_" + sk)
                        u3 = sg[:, 0:768].rearrange("p (h c) -> p h c", h=8)
                        P.op("act", lambda e, u3=u3, a_=a_: e.copy(wqn[:, a_, :].rearrange("p (h d) -> p h d", h=8), u3[:, :, 0:64]),
                             reads=[sk], writes=["wqn"])
                        rw4 = wqr[:, a_, 0:512].rearrange("p (h r d) -> p h r d", h=8, r=2)
                        sw4 = wqr[:, a_, 512:1024].rearrange("p (h r d) -> p h r d", h=8, r=2)
                        for r_ in range(2):
                            P.op("act", lambda e, u3=u3, rw4=rw4, r_=r_: e.copy(rw4[:, :, r_, :], u3[:, :, 64:96]), reads=[sk], writes=["wqr"])
                            P.op("dve", lambda e, u3=u3, sw4=sw4, r_=r_: e.tensor_copy(sw4[:, :, r_, 0:16], u3[:, :, 80:96]), reads=[sk], writes=["wqr"])
                            P.op("dve", lambda e, u3=u3, sw4=sw4, r_=r_: e.tensor_copy(sw4[:, :, r_, 16:32], u3[:, :, 64:80]), reads=[sk], writes=["wqr"])
                    w_out_v = w_out.rearrange("(k p) n -> p k n", p=128)
                    for kc in range(8):
                        sg, sk = stg[kc % 2], f"stgB{kc % 2}"
                        P.op("sp", lambda e, sg=sg, kc=kc: e.dma_start(out=sg[:, 0:1024], in_=w_out_v[:, kc, :]),
                             writes=[sk], dma_sem="d_" + sk)
                        if kc % 2 == 0:
                            P.op("act", lambda e, sg=sg, kc=kc: e.copy(woutb[:, kc, :], sg[:, 0:1024]), reads=[sk], writes=["woutb"])
                        else:
                            P.op("dve", lambda e, sg=sg, kc=kc: e.tensor_copy(woutb[:, kc, :], sg[:, 0:1024]), reads=[sk], writes=["woutb"])

                    P.flush()
                qT_swa = sb(sB, "qT_swa", [128, 4, 512], BF16)
                qnb = sb(sB, "qnb", [128, 256], BF16)
                qnT = sb(sB, "qnT", [128, 2, 512], BF16)
                qnopeT = sb(sB, "qnopeT", [128, 4, 512], BF16)
                qabsT = sb(sB, "qabsT", [128, 8, 512], BF16)
                qropeT = sb(sB, "qropeT", [64, 8, 512], BF16)
                E3 = [sb(sB, f"E3_{i}", [128, 3, 512], BF16) for i in range(2)]
                Em = [sb(sB, f"Em{i}", [128, 1024], BF16) for i in range(2)]
                mixed = sb(sB, "mixed", [128, 4, 1024], BF16)
                mixn = sb(sB, "mixn", [128, 1024], BF16)
                mixT = sb(sB, "mixT", [128, 8, 128], BF16)
                ssq = sb(sB, "ssq", [128, 4, 16], F32)
                sqt = sb(sB, "sqt", [128, 4, 64], F32)
                zt = sb(sB, "zt", [128, 4], F32)
                rzz = sb(sB, "rzz", [128, 4], F32)
                zts = sb(sB, "zts", [128, 4], F32)
                rzs = sb(sB, "rzs", [128, 4], F32)
                sqs = sb(sB, "sqs", [128, 4, 64], F32)
                ssab = sb(sB, "ssab", [128, 2, 4], F32)
                rsab = sb(sB, "rsab", [128, 2, 4], F32)

                SC_SWA = 64 ** -0.5
                SC_MLA = 96 ** -0.5
                for tc in range(n_chunks):
                    c0 = tc * 512
                    load_rope(tc)
                    for t in range(4):
                        load_norm_T(tc * 4 + t, t)
                    for j in range(4):
                        pr, prk = proj_fm(wB, "wB", j * 128, 128, hTc, "hTc")
                        psw, pswk = proj_fm(wB, "wB", 512 + j * 128, 128, hTc, "hTc")
                        rope(pr, prk, psw, pswk, cs64, "cs64", 128, qT_swa[:, j, :], "qT_swa")
                    for t in range(4):
                        pp, pk = nextpx()

                        def mm(e, pp=pp, t=t):
                            for kc in range(8):
                                i = e.matmul(pp[:, 0:256], hTc[:, kc, t * 128:(t + 1) * 128], wB[:, kc, 1024:1280],
                                             start=(kc == 0), stop=(kc == 7))
                            return i
                        P.op("pe", mm, reads=["hTc", "wB"], writes=[pk])
                        P.op("act", lambda e, pp=pp: e.activation(tmpB[:, 0:256], pp[:, 0:256], AF.Square, accum_out=stB[:, 0:1]),
                             reads=[pk], writes=["tmpB", "stB0"])
                        P.op("act", lambda e: e.activation(stB[:, 1:2], stB[:, 0:1], AF.Sqrt, bias=EPS, scale=1.0 / 256),
                             reads=["stB0"], writes=["stB1"])
                        P.op("dve", lambda e: e.reciprocal(stB[:, 2:3], stB[:, 1:2]), reads=["stB1"], writes=["stB2"])
                        P.op("dve", lambda e, pp=pp: e.scalar_tensor_tensor(qnb[:, :], pp[:, 0:256], stB[:, 2:3], bc_qn[:, :],
                                                                           ALU.mult, ALU.mult),
                             reads=[pk, "stB2", "bc_qn"], writes=["qnb"])

                        def tr2(e):
                            for a_ in range(2):
                                i = e.transpose(pT[:, a_, :], qnb[:, a_ * 128:(a_ + 1) * 128], ident_b[:, :])
                            return i
                        P.op("pe", tr2, reads=["qnb", "ident_b"], writes=["pT"])
                        P.op("act", lambda e, t=t: e.copy(qnT[:, :, t * 128:(t + 1) * 128], pT[:, 0:2, :]), reads=["pT"], writes=["qnT"])
                    for j in range(4):
                        pp, pk = proj_fm(wqn, "wqn", j * 128, 128, qnT, "qnT", nk=2)
                        P.op("act", lambda e, pp=pp, j=j: e.copy(qnopeT[:, j, :], pp[:, :]), reads=[pk], writes=["qnopeT"])
                    for h in range(8):
                        j, m = h // 2, h % 2
                        pp, pk = nextpx()
                        P.op("pe", lambda e, pp=pp, j=j, m=m: e.matmul(pp[:, :], WkupT[m * 64:(m + 1) * 64, j, :],
                                                                      qnopeT[m * 64:(m + 1) * 64, j, :], start=True, stop=True),
                             reads=["WkupT", "qnopeT"], writes=[pk])
                        if h % 2 == 0:
                            P.op("act", lambda e, pp=pp, h=h: e.copy(qabsT[:, h, :], pp[:, :]), reads=[pk], writes=["qabsT"])
                        else:
                            P.op("dve", lambda e, pp=pp, h=h: e.tensor_copy(qabsT[:, h, :], pp[:, :]), reads=[pk], writes=["qabsT"])
                    for h in range(8):
                        pr, prk = proj_fm(wqr, "wqr", h * 64, 64, qnT, "qnT", nk=2)
                        psw, pswk = proj_fm(wqr, "wqr", 512 + h * 64, 64, qnT, "qnT", nk=2)
                        rope(pr, prk, psw, pswk, cs32, "cs32", 64, qropeT[0:64, h, :], "qropeT")

                    def swa_chunk():
                        for t in range(4):
                            n = tc * 4 + t
                            js = [j for j in (n - 1, n, n + 1) if 0 <= j < NB]
                            for g in range(2):
                                e3, e3k = E3[g], f"E3_{g}"
                                for idx, j in enumerate(js):
                                    psc, psk = pSW, "pSW"
                                    P.op("pe", lambda e, psc=psc, g=g, j=j, t=t: e.matmul(
                                        psc[:, :], swa_kT[g * 64:(g + 1) * 64, j * 128:(j + 1) * 128],
                                        qT_swa[g * 64:(g + 1) * 64, :, t * 128:(t + 1) * 128], start=True, stop=True),
                                        reads=["swa_kT", "qT_swa"], writes=[psk])
                                    P.op("act", lambda e, psc=psc, e3=e3, idx=idx: e.activation(e3[:, idx, :], psc[:, :], AF.Exp, scale=SC_SWA),
                                         reads=[psk], writes=[e3k])
                                    if j != n:
                                        mk_, mkk = (mprev, "mprev") if j == n - 1 else (mnext, "mnext")
                                        P.op("dve", lambda e, e3=e3, idx=idx, mk_=mk_: e.tensor_tensor(
                                            e3[:, idx, :].rearrange("p (h q) -> p h q", h=4),
                                            e3[:, idx, :].rearrange("p (h q) -> p h q", h=4),
                                            mk_[:, :].rearrange("p (o q) -> p o q", o=1).to_broadcast([128, 4, 128]), ALU.mult),
                                            reads=[e3k, mkk], writes=[e3k])
                                po, pok = pO[1], "pO1"

                                def pv(e, po=po, e3=e3, g=g, js=js):
                                    for hh in range(4):
                                        for idx, j in enumerate(js):
                                            i = e.matmul(po[:, hh, :], e3[:, idx, hh * 128:(hh + 1) * 128], swa_v[:, j, g, :],
                                                         start=(idx == 0), stop=(idx == len(js) - 1))
                                    return i
                                P.op("pe", pv, reads=[e3k, "swa_v"], writes=[pok])
                                P.op("dve", lambda e, po=po, g=g: e.tensor_tensor(zts[:, :], po[:, :, 64], esink[:, 4 * g:4 * g + 4], ALU.add),
                                     reads=[pok, "esink"], writes=["zts"])
                                P.op("dve", lambda e: e.reciprocal(rzs[:, :], zts[:, :]), reads=["zts"], writes=["rzs"])
                                P.op("dve", lambda e, po=po, g=g, t=t: e.tensor_tensor(
                                    sqs[:, :, :], po[:, :, 0:64],
                                    rzs[:, :].rearrange("p (h o) -> p h o", o=1).to_broadcast([128, 4, 64]), ALU.mult),
                                    reads=[pok, "rzs"], writes=["sqs"])
                                P.op("act", lambda e, g=g, t=t: e.copy(mixed[:, t, g * 256:(g + 1) * 256].rearrange("p (h d) -> p h d", h=4),
                                                                      sqs[:, :, :]),
                                     reads=["sqs"], writes=["mixed"])
                                P.op("dve", lambda e: e.tensor_tensor(sqs[:, :, :], sqs[:, :, :], sqs[:, :, :], ALU.mult),
                                     reads=["sqs"], writes=["sqs"])
                                P.op("dve", lambda e, g=g, t=t: e.tensor_reduce(ssq[:, t, g:g + 1], sqs[:, :, :], AX.XY, ALU.add),
                                     reads=["sqs"], writes=["ssq"])

                    swa_thunks = P.capture(swa_chunk)
                    n_iter_left = [8 * (NB // 2)]
                    sbufs = [(pSS, ["pS0", "pS1"]), (pXX, ["pX0", "pX1"])]
                    for h in range(8):
                        po, pok = pO[0], "pO0"

                        def score(kp, h=h):
                            pst, psk = sbufs[kp % 2]

                            def mm(e):
                                for u_ in range(2):
                                    kb = kp * 2 + u_
                                    e.matmul(pst[:, u_ * 512:(u_ + 1) * 512], kvnT[:, kb * 128:(kb + 1) * 128], qabsT[:, h, :],
                                             start=True, stop=False)
                                for u_ in range(2):
                                    kb = kp * 2 + u_
                                    i = e.matmul(pst[:, u_ * 512:(u_ + 1) * 512], krT[u_ * 32:(u_ + 1) * 32, kb * 128:(kb + 1) * 128],
                                                 qropeT[u_ * 32:(u_ + 1) * 32, h, :], start=False, stop=True)
                                return i
                            P.op("pe", mm, reads=["kvnT", "krT", "qabsT", "qropeT"], writes=psk)
                        score(0)
                        for kp in range(NB // 2):
                            pst, psk = sbufs[kp % 2]
                            em, emk = Em[kp % 2], f"Em{kp % 2}"
                            gok = [f"go{tc}_{h}"] if kp == 0 else []
                            P.op("act", lambda e, pst=pst, em=em: e.activation(em[:, :], pst[:, :], AF.Exp, scale=SC_MLA),
                                 reads=psk, writes=[emk] + gok)
                            if kp == 0:
                                issue_cast(tc * 8 + h, gok)
                            if kp + 1 < NB // 2:
                                score(kp + 1)

                            def pv(e, em=em, kp=kp, po=po, h=h):
                                for u_ in range(2):
                                    kb = kp * 2 + u_
                                    for t in range(4):
                                        i = e.matmul(po[:, t, :], em[:, u_ * 512 + t * 128:u_ * 512 + (t + 1) * 128], mla_v[:, kb, h, :],
                                                     start=(kb == 0 and t == 0), stop=(kb == NB - 1), skip_group_check=True)
                                return i
                            P.op("pe", pv, reads=[emk, "mla_v"], writes=[pok])
                            k_ = (len(swa_thunks) + n_iter_left[0] - 1) // n_iter_left[0]
                            n_iter_left[0] -= 1
                            P.replay(swa_thunks[:k_])
                            del swa_thunks[:k_]
                        P.op("dve", lambda e, po=po: e.reciprocal(rzz[:, :], po[:, :, 64]), reads=[pok], writes=["rzz"])
                        P.op("dve", lambda e, po=po: e.tensor_tensor(
                            sqt[:, :, :], po[:, :, 0:64],
                            rzz[:, :].rearrange("p (h o) -> p h o", o=1).to_broadcast([128, 4, 64]), ALU.mult),
                            reads=[pok, "rzz"], writes=["sqt"])
                        P.op("act", lambda e, h=h: e.copy(mixed[:, :, 512 + h * 64:512 + (h + 1) * 64], sqt[:, :, :]),
                             reads=["sqt"], writes=["mixed"])
                        P.op("dve", lambda e: e.tensor_tensor(sqt[:, :, :], sqt[:, :, :], sqt[:, :, :], ALU.mult),
                             reads=["sqt"], writes=["sqt"])
                        P.op("dve", lambda e, h=h: e.tensor_reduce(ssq[:, :, 2 + h], sqt[:, :, :], AX.X, ALU.add),
                             reads=["sqt"], writes=["ssq"])

                    P.replay(swa_thunks)
                    del swa_thunks[:]
                    P.op("dve", lambda e: e.tensor_reduce(ssab[:, 0, :], ssq[:, :, 0:2], AX.X, ALU.add), reads=["ssq"], writes=["ssab"])
                    P.op("dve", lambda e: e.tensor_reduce(ssab[:, 1, :], ssq[:, :, 2:10], AX.X, ALU.add), reads=["ssab", "ssq"], writes=["ssab"])
                    P.op("act", lambda e: e.activation(ssab[:, :, :], ssab[:, :, :], AF.Sqrt, bias=EPS, scale=1.0 / 512),
                         reads=["ssab"], writes=["ssab"])
                    P.op("dve", lambda e: e.reciprocal(rsab[:, :, :], ssab[:, :, :]), reads=["ssab"], writes=["rsab"])
                    for t in range(4):
                        n = tc * 4 + t
                        for gi in range(2):
                            P.op("dve", lambda e, t=t, gi=gi: e.scalar_tensor_tensor(
                                mixn[:, gi * 512:(gi + 1) * 512], mixed[:, t, gi * 512:(gi + 1) * 512], rsab[:, gi, t:t + 1],
                                bc_on[:, gi * 512:(gi + 1) * 512], ALU.mult, ALU.mult),
                                reads=["mixed", "rsab", "bc_on"], writes=["mixn"])

                        def trm(e):
                            for kc in range(8):
                                i = e.transpose(pT[:, kc, :], mixn[:, kc * 128:(kc + 1) * 128], ident_b[:, :])
                            return i
                        P.op("pe", trm, reads=["mixn", "ident_b"], writes=["pT"])
                        P.op("act", lambda e: e.copy(mixT[:, :, :], pT[:, :, :]), reads=["pT"], writes=["mixT"])
                        xt, xk = xts[n % 2], f"xa{n % 2}"
                        P.op("sp", lambda e, xt=xt, n=n: e.dma_start(out=xt[:, :], in_=x[n * 128:(n + 1) * 128, :]),
                             writes=[xk], dma_sem="d_" + xk)
                        for hf in range(2):
                            pp, pk = nextpx()

                            def mmo(e, pp=pp, hf=hf):
                                for kc in range(8):
                                    i = e.matmul(pp[:, :], mixT[:, kc, :], woutb[:, kc, hf * 512:(hf + 1) * 512],
                                                 start=(kc == 0), stop=(kc == 7))
                                return i
                            P.op("pe", mmo, reads=["mixT", "woutb"], writes=[pk])
                            P.op("dve", lambda e, pp=pp, hf=hf: e.tensor_tensor(tmpA[:, hf * 512:(hf + 1) * 512], pp[:, :],
                                                                               bc_ga[:, hf * 512:(hf + 1) * 512], ALU.mult),
                                 reads=[pk, "bc_ga"], writes=["tmpA"])
                        P.op("dve", lambda e, xt=xt: e.tensor_tensor(tmpA[:, :], tmpA[:, :], xt[:, :], ALU.add),
                             reads=["tmpA", xk], writes=["tmpA"])
                        P.op("sp", lambda e, n=n: e.dma_start(out=x1d[n * 128:(n + 1) * 128, :], in_=tmpA[:, :]),
                             reads=["tmpA"], writes=["x1d"], dma_sem="d_x1d")
                P.flush()
        x1src = x1d if do_attn else x
        for i_ in range(64):
            issue_cast(i_, [])

        if do_peer:
          with ExitStack() as st:
            wq = sb(st, "wq", [128, 8, 2048], BF16)
            bc_sc1f = sb(st, "bc_sc1f", [128, D], F32)
            bc_shf = sb(st, "bc_shf", [128, D], F32)
            bc_gf = sb(st, "bc_gf", [128, D], F32)
            bc_fin = sb(st, "bc_fin", [128, D], F32)
            for dst, dk, off in ((bc_shf, "bc_shf", 3 * D), (bc_sc1f, "bc_sc1f", 4 * D), (bc_gf, "bc_gf", 5 * D)):
                P.op("sp", lambda e, dst=dst, off=off: e.dma_start(
                    out=dst[:, :], in_=modd[0:1, off:off + D].partition_broadcast(128)),
                    reads=["modd"], writes=[dk], dma_sem="d_" + dk)
            P.op("sp", lambda e: e.dma_start(out=bc_fin[:, :], in_=final_norm.partition_broadcast(128)),
                 writes=["bc_fin"], dma_sem="d_fin")
            keysT = sb(st, "keysT", [128, 16, 128], BF16)
            NS = 16
            G = 4
            ug = [sb(st, f"ug{i}", [128, 2 * D], BF16) for i in range(NS)]
            dg = [sb(st, f"dg{i}", [128, 128], BF16) for i in range(4)]
            x1t = [sb(st, f"x1t{i}", [128, D], F32) for i in range(2)]
            hh = [sb(st, f"hh{i}", [128, D], F32) for i in range(2)]
            junkb = sb(st, "junkb", [128, D], BF16)
            junkf = sb(st, "junkf", [128, D], BF16)
            hb = sb(st, "hb", [128, D], BF16)
            hT = sb(st, "hT", [128, 8, 128], BF16)
            qT = sb(st, "qT", [128, 16, 128], BF16)
            bufS = sb(st, "bufS", [128, 2048], F32)
            bufS2 = sb(st, "bufS2", [128, 2048], F32)
            tv = sb(st, "tv", [128, 16, 16], F32)
            ti = sb(st, "ti", [128, 16, 16], U32)
            tif = sb(st, "tif", [128, 16, 16], F32)
            best = sb(st, "best", [128, 8, 16], F32)
            pos = sb(st, "pos", [128, 8, 16], U32)
            posa = sb(st, "posa", [128, 8, 16], U32)
            posb = sb(st, "posb", [128, 8, 16], U32)
            paf = sb(st, "paf", [128, 8, 16], F32)
            pbf = sb(st, "pbf", [128, 8, 16], F32)
            If = sb(st, "If", [128, 8, 16], F32)
            Jf = sb(st, "Jf", [128, 8, 16], F32)
            ef = sb(st, "ef", [128, 128], F32)
            ei = [sb(st, f"ei{i}", [128, 128], I32) for i in range(2)]
            gg = [sb(st, f"gg{i}", [128, 8, 16], F32) for i in range(2)]
            nmx = sb(st, "nmx", [128, 8], F32)
            zs = sb(st, "zs", [128, 8], F32)
            rz = sb(st, "rz", [128, 8], F32)
            Aa = sb(st, "Aa", [128, 128], F32)
            ga = sb(st, "ga", [128, 128], F32)
            ww = sb(st, "ww", [128, 128], F32)
            acc = sb(st, "acc", [128, D], F32)
            st8 = sb(st, "st8", [128, 8], F32)
            yt = sb(st, "yt", [128, D], F32)
            kst = sb(st, "kst", [128, 16, 128], F32)
            wstg = [sb(st, f"wstg{i}", [128, 2048], F32) for i in range(2)]
            pS = ps(st, "pS", [128, 2048], F32)
            pQ = ps(st, "pQ", [128, 4, 128], F32)
            pAcc = ps(st, "pAcc", [128, D], F32)
            pT = ps(st, "pT", [128, 8, 128], BF16)

            wqv = w_pq.rearrange("(k p) n -> p k n", p=128)
            for kc in range(8):
                wt = wstg[kc % 2]
                wk = f"wstg{kc % 2}"
                P.op("sp", lambda e, wt=wt, kc=kc: e.dma_start(out=wt[:, :], in_=wqv[:, kc, :]),
                     writes=[wk], dma_sem="d_" + wk)
                if kc % 2 == 0:
                    P.op("dve", lambda e, wt=wt, kc=kc: e.tensor_copy(wq[:, kc, :], wt[:, :]), reads=[wk], writes=["wq"])
                else:
                    P.op("act", lambda e, wt=wt, kc=kc: e.copy(wq[:, kc, :], wt[:, :]), reads=[wk], writes=["wq"])
            P.op("sp", lambda e: e.dma_start(out=kst[:, :, :], in_=sub_keys.rearrange("g n d -> n g d")),
                 writes=["kst"], dma_sem="d_kst")
            for g4 in range(4):
                def tr(e, g4=g4):
                    for j in range(4):
                        g = g4 * 4 + j
                        i = e.transpose(pS[:, j * 128:(j + 1) * 128], kst[:, g, :], ident_f[:, :])
                    return i
                P.op("pe", tr, reads=["kst", "ident_f"], writes=["pS"])
                P.op("act", lambda e, g4=g4: e.copy(keysT[:, g4 * 4:(g4 + 1) * 4, :],
                                                  pS[:, 0:512].rearrange("p (g n) -> p g n", g=4)),
                     reads=["pS"], writes=["keysT"])

            def top16(src, srck, dst2, dst2k, nseg, seglen, tvv, tvk, tii, tik):
                def r1(e):
                    for g in range(nseg):
                        i = e.max(tvv[:, g, 0:8], src[:, g * seglen:(g + 1) * seglen])
                    return i
                P.op("dve", r1, reads=[srck], writes=[tvk])

                def r2(e):
                    for g in range(nseg):
                        i = e.match_replace(dst2[:, g * seglen:(g + 1) * seglen], tvv[:, g, 0:8],
                                            src[:, g * seglen:(g + 1) * seglen], NEG)
                    return i
                P.op("dve", r2, reads=[srck, tvk], writes=[dst2k])

                def r3(e):
                    for g in range(nseg):
                        i = e.max(tvv[:, g, 8:16], dst2[:, g * seglen:(g + 1) * seglen])
                    return i
                P.op("dve", r3, reads=[dst2k], writes=[tvk])

                def r4(e):
                    for g in range(nseg):
                        e.max_index(tii[:, g, 0:8], tvv[:, g, 0:8], src[:, g * seglen:(g + 1) * seglen])
                        i = e.max_index(tii[:, g, 8:16], tvv[:, g, 8:16], dst2[:, g * seglen:(g + 1) * seglen])
                    return i
                P.op("dve", r4, reads=[srck, dst2k, tvk], writes=[tik])

            def sel(n):
                b = n % 2
                xt, xk = x1t[b], f"x1t{b}"
                h, hk = hh[b], f"hh{b}"
                P.op("sp", lambda e: e.dma_start(out=xt[:, :], in_=x1src[n * 128:(n + 1) * 128, :]),
                     reads=["x1d"], writes=[xk], dma_sem="d_" + xk)
                P.op("act", lambda e: e.activation(junkb[:, :], xt[:, :], AF.Square, accum_out=st8[:, 0:1]),
                     reads=[xk], writes=["junkb", "st8a"])
                P.op("act", lambda e: e.activation(st8[:, 1:2], st8[:, 0:1], AF.Sqrt, bias=EPS, scale=1.0 / D),
                     reads=["st8a"], writes=["st8b"])
                P.op("dve", lambda e: e.reciprocal(st8[:, 2:3], st8[:, 1:2]), reads=["st8b"], writes=["st8c"])
                P.op("dve", lambda e: e.scalar_tensor_tensor(h[:, :], xt[:, :], st8[:, 2:3], bc_sc1f[:, :], ALU.mult, ALU.mult),
                     reads=[xk, "st8c", "bc_sc1f"], writes=[hk])
                P.op("dve", lambda e: e.tensor_tensor(h[:, :], h[:, :], bc_shf[:, :], ALU.add),
                     reads=[hk, "bc_shf"], writes=[hk])
                P.op("act", lambda e: e.copy(hb[:, :], h[:, :]), reads=[hk], writes=["hb"])

                def tr(e):
                    for kc in range(8):
                        i = e.transpose(pT[:, kc, :], hb[:, kc * 128:(kc + 1) * 128], ident_b[:, :])
                    return i
                P.op("pe", tr, reads=["hb", "ident_b"], writes=["pT"])
                P.op("act", lambda e: e.copy(hT[:, :, :], pT[:, :, :]), reads=["pT"], writes=["hT"])
                for r in range(4):
                    def qmm(e, r=r):
                        for j in range(4):
                            g = r * 4 + j
                            for kc in range(8):
                                i = e.matmul(pQ[:, j, :], wq[:, kc, g * 128:(g + 1) * 128], hT[:, kc, :],
                                             start=(kc == 0), stop=(kc == 7))
                        return i
                    P.op("pe", qmm, reads=["wq", "hT"], writes=["pQ"])
                    P.op("act", lambda e, r=r: e.copy(qT[:, r * 4:(r + 1) * 4, :], pQ[:, :, :]), reads=["pQ"], writes=["qT"])

                def smm(e):
                    for g in range(16):
                        i = e.matmul(pS[:, g * 128:(g + 1) * 128], qT[:, g, :], keysT[:, g, :], start=True, stop=True)
                    return i
                P.op("pe", smm, reads=["qT", "keysT"], writes=["pS"])
                P.op("act", lambda e: e.copy(bufS[:, :], pS[:, :]), reads=["pS"], writes=["bufS"])
                top16(bufS, "bufS", bufS2, "bufS2", 16, 128, tv, "tv", ti, "ti")
                P.op("dve", lambda e: e.tensor_copy(tif[:, :, :], ti[:, :, :]), reads=["ti"], writes=["tif"])
                tv4 = tv[:, :, :].rearrange("p (h t) k -> p h t k", t=2)
                tif4 = tif[:, :, :].rearrange("p (h t) k -> p h t k", t=2)
                cand = bufS[:, :].rearrange("p (h a b) -> p h a b", h=8, a=16)
                P.op("dve", lambda e: e.tensor_tensor(
                    cand, tv4[:, :, 0, :].rearrange("p h (a o) -> p h a o", o=1).to_broadcast([128, 8, 16, 16]),
                    tv4[:, :, 1:2, :].to_broadcast([128, 8, 16, 16]), ALU.add),
                    reads=["tv"], writes=["bufS"])
                top16(bufS, "bufS", bufS2, "bufS2", 8, 256, best, "best", pos, "pos")
                P.op("dve", lambda e: e.tensor_scalar_mul(nmx[:, :], best[:, :, 0], -1.0), reads=["best"], writes=["nmx"])
                g_ = gg[b]
                gk = f"gg{b}"

                def ex(e):
                    for hd in range(8):
                        i = e.activation(g_[:, hd, :], best[:, hd, :], AF.Exp, bias=nmx[:, hd:hd + 1],
                                         accum_out=zs[:, hd:hd + 1])
                    return i
                P.op("act", ex, reads=["best", "nmx"], writes=[gk, "zs"])
                P.op("dve", lambda e: e.reciprocal(rz[:, :], zs[:, :]), reads=["zs"], writes=["rz"])
                P.op("dve", lambda e: e.tensor_tensor(
                    g_[:, :, :], g_[:, :, :], rz[:, :].rearrange("p (h o) -> p h o", o=1).to_broadcast([128, 8, 16]), ALU.mult),
                    reads=[gk, "rz"], writes=[gk])
                P.op("dve", lambda e: e.tensor_single_scalar(posa[:, :, :], pos[:, :, :], 4, ALU.logical_shift_right),
                     reads=["pos"], writes=["posa"])
                P.op("dve", lambda e: e.tensor_single_scalar(posb[:, :, :], pos[:, :, :], 15, ALU.bitwise_and),
                     reads=["pos"], writes=["posb"])
                P.op("dve", lambda e: e.tensor_copy(paf[:, :, :], posa[:, :, :]), reads=["posa"], writes=["paf"])
                P.op("dve", lambda e: e.tensor_copy(pbf[:, :, :], posb[:, :, :]), reads=["posb"], writes=["pbf"])
                oh = bufS[:, :].rearrange("p (h k a) -> p h k a", h=8, k=16)
                oh2 = bufS2[:, :].rearrange("p (h k a) -> p h k a", h=8, k=16)
                io4 = iota16[:, :].rearrange("p (h k a) -> p h k a", h=1, k=1).to_broadcast([128, 8, 16, 16])
                for (pf, pfk, t, dst, dstk) in ((paf, "paf", 0, If, "If"), (pbf, "pbf", 1, Jf, "Jf")):
                    P.op("dve", lambda e, pf=pf: e.tensor_tensor(
                        oh, io4, pf[:, :, :].rearrange("p h (k o) -> p h k o", o=1).to_broadcast([128, 8, 16, 16]),
                        ALU.is_equal), reads=[pfk, "iota16"], writes=["bufS"])
                    P.op("dve", lambda e, t=t: e.tensor_tensor(
                        oh2, oh, tif4[:, :, t:t + 1, :].to_broadcast([128, 8, 16, 16]), ALU.mult),
                        reads=["bufS", "tif"], writes=["bufS2"])
                    P.op("dve", lambda e, dst=dst: e.tensor_reduce(dst[:, :, :], oh2, AX.X, ALU.add),
                         reads=["bufS2"], writes=[dstk])
                P.op("dve", lambda e: e.scalar_tensor_tensor(
                    ef[:, :], If[:, :, :].rearrange("p h k -> p (h k)"), 128.0,
                    Jf[:, :, :].rearrange("p h k -> p (h k)"), ALU.mult, ALU.add),
                    reads=["If", "Jf"], writes=["ef"])
                P.op("dve", lambda e: e.tensor_copy(ei[b][:, :], ef[:, :]), reads=["ef"], writes=[f"ei{b}"])

            slot = [0]

            def gather(n_b, j):
                s_ = slot[0] % NS
                slot[0] += 1
                P.op("pool", lambda e: e.indirect_dma_start(
                    out=ug[s_][:, :], out_offset=None, in_=uvd,
                    in_offset=bass.IndirectOffsetOnAxis(ap=ei[n_b][:, j:j + 1], axis=0)),
                    reads=[f"ei{n_b}"] + UVK, writes=[f"ug{s_}"], dma_sem=f"d_ug{s_}")
                return s_

            dgi = [0]

            def experts(n):
                b = n % 2
                xt, xk = x1t[b], f"x1t{b}"
                h, hk = hh[b], f"hh{b}"
                gflat = gg[b][:, :, :].rearrange("p h k -> p (h k)")
                ngrp = 128 // G
                pend = None

                def vside(grp, slots):
                    cs = slice(grp * G, (grp + 1) * G)
                    kq = grp % 4
                    P.op("dve", lambda e: e.tensor_tensor(ww[:, cs], ga[:, cs], gflat[:, cs], ALU.mult),
                         reads=[f"ga{kq}", f"gg{b}"], writes=[f"ww{kq}"])
                    for jj, s_ in enumerate(slots):
                        j = grp * G + jj
                        di = dgi[0] % 4
                        dgi[0] += 1
                        P.op("act", lambda e, di=di, j=j: e.activation(dg[di][:, :], ident_b[:, :], AF.Copy, scale=ww[:, j:j + 1]),
                             reads=[f"ww{kq}", "ident_b"], writes=[f"dg{di}"])

                        def mm(e, di=di, s_=s_, j=j):
                            e.matmul(pAcc[:, 0:512], dg[di][:, :], ug[s_][:, D:D + 512], start=(j == 0), stop=(j == 127))
                            return e.matmul(pAcc[:, 512:1024], dg[di][:, :], ug[s_][:, D + 512:2 * D], start=(j == 0), stop=(j == 127))
                        P.op("pe", mm, reads=[f"dg{di}", f"ug{s_}"], writes=["pAcc"])

                for grp in range(ngrp):
                    cs = slice(grp * G, (grp + 1) * G)
                    kq = grp % 4
                    slots = []
                    for jj in range(G):
                        j = grp * G + jj
                        s_ = gather(b, j)
                        slots.append(s_)
                        P.op("dve", lambda e, s_=s_, j=j: e.scalar_tensor_tensor(
                            junkf[:, :], ug[s_][:, 0:D], 1.0, h[:, :], ALU.mult, ALU.mult, accum_out=Aa[:, j:j + 1]),
                            reads=[f"ug{s_}", hk], writes=["junkf", f"Aa{kq}"])
                    P.op("act", lambda e, cs=cs: e.activation(ga[:, cs], Aa[:, cs], AF.Gelu), reads=[f"Aa{kq}"], writes=[f"ga{kq}"])
                    if pend is not None:
                        vside(*pend)
                    pend = (grp, slots)
                    if grp >= 1:
                        k_ = (len(pending_sel) + (ngrp - 1 - grp) - 1) // max(ngrp - 1 - grp, 1) if grp < ngrp - 1 else len(pending_sel)
                        P.replay(pending_sel[:k_])
                        del pending_sel[:k_]
                vside(*pend)
                P.op("dve", lambda e: e.tensor_tensor(acc[:, :], pAcc[:, :], bc_gf[:, :], ALU.mult),
                     reads=["pAcc", "bc_gf"], writes=["acc"])
                P.op("dve", lambda e: e.tensor_tensor(acc[:, :], acc[:, :], xt[:, :], ALU.add),
                     reads=["acc", xk], writes=["acc"])
                P.op("act", lambda e: e.activation(junkb[:, :], acc[:, :], AF.Square, accum_out=st8[:, 3:4]),
                     reads=["acc"], writes=["junkb", "st8d"])
                P.op("act", lambda e: e.activation(st8[:, 4:5], st8[:, 3:4], AF.Sqrt, bias=EPS, scale=1.0 / D),
                     reads=["st8d"], writes=["st8e"])
                P.op("dve", lambda e: e.reciprocal(st8[:, 5:6], st8[:, 4:5]), reads=["st8e"], writes=["st8f"])
                P.op("dve", lambda e: e.scalar_tensor_tensor(yt[:, :], acc[:, :], st8[:, 5:6], bc_fin[:, :], ALU.mult, ALU.mult),
                     reads=["acc", "st8f", "bc_fin"], writes=["yt"])
                P.op("sp", lambda e: e.dma_start(out=y[n * 128:(n + 1) * 128, :], in_=yt[:, :]),
                     reads=["yt"], writes=["y_out"], dma_sem="d_y")

            pending_sel = []
            sel(0)
            for n in range(n_blocks):
                if n + 1 < n_blocks:
                    pending_sel.extend(P.capture(lambda: sel(n + 1)))
                experts(n)
                P.replay(pending_sel)
                del pending_sel[:]
            P.flush()
    return nc


def _rope_tables():
    pos = np.arange(S, dtype=np.float32)
    out = {}
    for dim, name in ((64, "rope64"), (32, "rope32")):
        inv = (1.0 / (10000.0 ** (np.arange(0, dim, 2, dtype=np.float32) / dim))).astype(np.float32)
        ang = pos[:, None] * inv[None, :]
        cos = np.cos(ang).astype(np.float32).T
        sin = np.sin(ang).astype(np.float32).T
        cosf = np.concatenate([cos, cos], axis=0)
        sinf = np.concatenate([-sin, sin], axis=0)
        cosf = np.concatenate([cosf, cosf], axis=0)
        sinf = np.concatenate([sinf, sinf], axis=0)
        out[name] = np.ascontiguousarray(np.stack([cosf, sinf], axis=0))
    return out


def make_in_maps(inputs, n_cores=8):
    g = lambda k: np.ascontiguousarray(np.asarray(inputs[k], dtype=np.float32))
    rt = _rope_tables()
    shared = {
        "w_ada": g("w_ada")[0], "b_ada": g("b_ada")[0][None, :], "w_in": g("w_in")[0],
        "swa_sink": g("swa_sink")[0][None, :], "mla_q_norm": g("mla_q_norm")[0][None, :],
        "w_q_up": g("w_mla_q_up")[0], "mla_kv_norm": g("mla_kv_norm")[0][None, :],
        "w_kv_up": g("w_mla_kv_up")[0],
        "out_norm": np.ascontiguousarray(np.concatenate([g("out_norm_swa")[0], g("out_norm_mla")[0]])[None, :]),
        "w_out": g("w_out")[0], "w_pq": g("w_peer_query")[0],
        "sub_keys": np.ascontiguousarray(g("peer_sub_keys")[0].reshape(16, 128, 128)),
        "exp_u": g("peer_expert_u")[0], "exp_v": g("peer_expert_v")[0],
        "final_norm": g("final_norm")[None, :], "rope64": rt["rope64"], "rope32": rt["rope32"],
    }
    xs = g("x")
    cs = g("c")
    maps = []
    for i in range(n_cores):
        m = dict(shared)
        m["x"] = np.ascontiguousarray(xs[i])
        m["c"] = np.ascontiguousarray(cs[i].reshape(8, 128))
        maps.append(m)
    return maps


def kernel(**inputs):
    nc = build()
    in_maps = make_in_maps(inputs, 8)
    res = run_bass_kernel_spmd(nc, in_maps, core_ids=list(range(8)))
    return np.stack([np.asarray(r["y"]).reshape(S, D) for r in res.results], axis=0).astype(np.float32)
```

```python
from contextlib import ExitStack
import numpy as np
import concourse.bass as bass
import concourse.mybir as mybir
from concourse.bass_utils import run_bass_kernel_spmd

F32 = mybir.dt.float32
BF16 = mybir.dt.bfloat16
I32 = mybir.dt.int32
U32 = mybir.dt.uint32
AF = mybir.ActivationFunctionType
ALU = mybir.AluOpType
AX = mybir.AxisListType

S = 4096
D = 1024
NB = S // 128
EPS = 1e-6
NEG = -1e30


class Prog:
    ENGS = ("pe", "act", "dve", "pool", "sp")

    def __init__(self, nc, stack):
        self.nc = nc
        self.stack = stack
        self.ops = {e: [] for e in self.ENGS}
        self.sem = {}
        self.cnt = {}
        self.waited = {e: {} for e in self.ENGS}
        self.lastw = {}
        self.reads = {}
        self.defer = None
        for e in self.ENGS:
            self.newsem("c_" + e)

    def capture(self, fn):
        self.defer = []
        fn()
        lst, self.defer = self.defer, None
        return lst

    def replay(self, thunks):
        for t in thunks:
            self.op(*t)

    def newsem(self, name):
        if name not in self.sem:
            self.sem[name] = self.stack.enter_context(self.nc.semaphore(name))
            self.cnt[name] = 0
        return name

    def op(self, eng, fn, reads=(), writes=(), dma_sem=None):
        if self.defer is not None:
            self.defer.append((eng, fn, tuple(reads), tuple(writes), dma_sem))
            return None
        waits = {}

        def need(tok):
            if tok is None:
                return
            s, v = tok
            if waits.get(s, 0) < v:
                waits[s] = v

        for k in reads:
            need(self.lastw.get(k))
        for k in writes:
            need(self.lastw.get(k))
            for s, v in self.reads.get(k, {}).items():
                need((s, v))
        w = []
        for s, v in waits.items():
            if self.waited[eng].get(s, 0) < v:
                self.waited[eng][s] = v
                w.append((s, v))
        if dma_sem is None:
            s = "c_" + eng
            inc = 1
        else:
            s = self.newsem(dma_sem)
            inc = 16
        self.cnt[s] += inc
        tok = (s, self.cnt[s])
        for k in writes:
            self.lastw[k] = tok
            self.reads[k] = {}
        for k in reads:
            self.reads.setdefault(k, {})[s] = self.cnt[s]
        self.ops[eng].append((fn, w, s, inc))
        return tok

    def flush(self):
        nc = self.nc
        final = dict(self.cnt)
        with nc.Block() as block:
            def mk(engname):
                def body(eng):
                    for fn, w, s, inc in self.ops[engname]:
                        for ws, wv in w:
                            eng.wait_ge(self.sem[ws], wv)
                        fn(eng).then_inc(self.sem[s], inc)
                    for s, v in final.items():
                        if v > 0 and self.waited[engname].get(s, 0) < v:
                            eng.wait_ge(self.sem[s], v)
                            self.waited[engname][s] = v
                return body
            block.tensor(mk("pe"))
            block.scalar(mk("act"))
            block.vector(mk("dve"))
            block.gpsimd(mk("pool"))
            block.sync(mk("sp"))
        self.ops = {e: [] for e in self.ENGS}


def build(n_blocks=NB, do_attn=True, do_peer=True, n_chunks=8):
    nc = bass.Bass("TRN2", target_bir_lowering=False)
    dt_in = lambda name, shape: nc.dram_tensor(name, list(shape), F32, kind="ExternalInput").ap()
    x = dt_in("x", [S, D])
    c = dt_in("c", [8, 128])
    w_ada = dt_in("w_ada", [D, 6 * D])
    b_ada = dt_in("b_ada", [1, 6 * D])
    w_in = dt_in("w_in", [D, 1184])
    swa_sink = dt_in("swa_sink", [1, 8])
    mla_q_norm = dt_in("mla_q_norm", [1, 256])
    w_q_up = dt_in("w_q_up", [256, 768])
    mla_kv_norm = dt_in("mla_kv_norm", [1, 128])
    w_kv_up = dt_in("w_kv_up", [128, 1024])
    out_norm = dt_in("out_norm", [1, 1024])
    w_out = dt_in("w_out", [D, D])
    w_pq = dt_in("w_pq", [D, 2048])
    sub_keys = dt_in("sub_keys", [16, 128, 128])
    exp_u = dt_in("exp_u", [16384, D])
    exp_v = dt_in("exp_v", [16384, D])
    final_norm = dt_in("final_norm", [1, D])
    rope64 = dt_in("rope64", [2, 128, S])
    rope32 = dt_in("rope32", [2, 64, S])
    y = nc.dram_tensor("y", [S, D], F32, kind="ExternalOutput").ap()
    x1d = nc.dram_tensor("x1d", [S, D], F32, kind="Internal").ap()
    modd = nc.dram_tensor("modd", [1, 6 * D], F32, kind="Internal").ap()
    uvd = nc.dram_tensor("uvd", [16384, 2 * D], BF16, kind="Internal").ap()
    UVK = [f"uvd{i}" for i in range(64)]

    with ExitStack() as gs:
        P = Prog(nc, gs)
        sb = lambda st, name, shape, dt: st.enter_context(nc.sbuf_tensor(name, list(shape), dt))
        ps = lambda st, name, shape, dt: st.enter_context(nc.psum_tensor(name, list(shape), dt))

        ident_f = sb(gs, "ident_f", [128, 128], F32)
        ident_b = sb(gs, "ident_b", [128, 128], BF16)
        dif_i = sb(gs, "dif_i", [128, 128], I32)
        ones_f = sb(gs, "ones_f", [1, 128], F32)
        fm_a = sb(gs, "fm_a", [128, 16], F32)
        iota16 = sb(gs, "iota16", [128, 16], F32)

        cast_done = set()

        def issue_cast(i, rd):
            if not do_peer or i in cast_done or i >= 64:
                return
            cast_done.add(i)
            blk, ti_ = i // 2, i % 2
            tbl, off = ((exp_u, 0), (exp_v, D))[ti_]
            P.op("pool", lambda e: e.dma_start(out=uvd[blk * 512:(blk + 1) * 512, off:off + D],
                                               in_=tbl[blk * 512:(blk + 1) * 512, :]),
                 reads=rd, writes=[UVK[i]], dma_sem="d_uvd")

        with ExitStack() as st:
            c8 = sb(st, "c8", [8, 128], F32)
            modrow = sb(st, "modrow", [1, 6 * D], F32)
            cT = sb(st, "cT", [128, 8], F32)
            brow = sb(st, "brow", [1, 6 * D], F32)
            wst = [sb(st, f"wst{i}", [128, 8, 512], F32) for i in range(2)]
            iota_i = sb(st, "iota_i", [128, 16], I32)
            pA = ps(st, "pA", [128, 512], F32)
            pB = ps(st, "pB", [128, 512], F32)

            P.op("pool", lambda e: e.iota(dif_i[:, :], [[1, 128]], 0, -1), writes=["dif_i"])
            P.op("pool", lambda e: e.iota(iota_i[:, :], [[1, 16]], 0, 0), writes=["iota_i"])
            P.op("dve", lambda e: e.tensor_copy(iota16[:, :], iota_i[:, :]), reads=["iota_i"], writes=["iota16"])
            P.op("dve", lambda e: e.tensor_single_scalar(ident_f[:, :], dif_i[:, :], 0.0, ALU.is_equal),
                 reads=["dif_i"], writes=["ident_f"])
            P.op("dve", lambda e: e.tensor_single_scalar(ident_b[:, :], dif_i[:, :], 0.0, ALU.is_equal),
                 reads=["dif_i"], writes=["ident_b"])
            P.op("dve", lambda e: e.memset(ones_f[:, :], 1.0), writes=["ones_f"])
            P.op("sp", lambda e: e.dma_start(out=c8[:, :], in_=c), writes=["c8"], dma_sem="d_c8")
            P.op("sp", lambda e: e.dma_start(out=brow[:, :], in_=b_ada), writes=["brow"], dma_sem="d_brow")
            P.op("pe", lambda e: e.transpose(pA[:, 0:8], c8[:, :], ident_f[0:8, 0:8]),
                 reads=["c8", "ident_f"], writes=["pA"])
            P.op("act", lambda e: e.activation(cT[:, :], pA[:, 0:8], AF.Silu), reads=["pA"], writes=["cT"])
            wv = w_ada.rearrange("(k p) n -> p k n", p=128)
            pbuf = [pA, pB]
            for nb in range(12):
                wt = wst[nb % 2]
                wk = f"wst{nb % 2}"
                pp = pbuf[nb % 2]
                pk = "pA" if nb % 2 == 0 else "pB"
                P.op("sp", lambda e, wt=wt, nb=nb: e.dma_start(out=wt[:, :, :], in_=wv[:, :, nb * 512:(nb + 1) * 512]),
                     writes=[wk], dma_sem="d_" + wk)

                def mm(e, wt=wt, pp=pp):
                    for kc in range(8):
                        i = e.matmul(pp[0:1, :], cT[:, kc:kc + 1], wt[:, kc, :], start=(kc == 0), stop=(kc == 7))
                    return i
                P.op("pe", mm, reads=[wk, "cT"], writes=[pk])
                P.op("dve", lambda e, pp=pp, nb=nb: e.tensor_tensor(
                    modrow[0:1, nb * 512:(nb + 1) * 512], pp[0:1, :], brow[0:1, nb * 512:(nb + 1) * 512], ALU.add),
                    reads=[pk, "brow"], writes=["modrow"])
            P.op("dve", lambda e: e.tensor_scalar_add(modrow[0:1, D:2 * D], modrow[0:1, D:2 * D], 1.0),
                 reads=["modrow"], writes=["modrow"])
            P.op("dve", lambda e: e.tensor_scalar_add(modrow[0:1, 4 * D:5 * D], modrow[0:1, 4 * D:5 * D], 1.0),
                 reads=["modrow"], writes=["modrow"])
            P.op("sp", lambda e: e.dma_start(out=modd, in_=modrow[0:1, :]), reads=["modrow"], writes=["modd"],
                 dma_sem="d_modd")
            def fmm(e):
                for j in range(16):
                    off = (D if j < 8 else 0) + (j % 8) * 128
                    i = e.matmul(pA[:, j:j + 1], modrow[0:1, off:off + 128], ones_f[0:1, 0:1], start=True, stop=True)
                return i
            P.op("pe", fmm, reads=["modrow", "ones_f"], writes=["pA"])
            P.op("act", lambda e: e.copy(fm_a[:, :], pA[:, 0:16]), reads=["pA"], writes=["fm_a"])
            P.flush()

        if do_attn:
          with ExitStack() as sa:
            kvnT = sb(sa, "kvnT", [128, S], BF16)
            krT = sb(sa, "krT", [64, S], BF16)
            mla_v = sb(sa, "mla_v", [128, NB, 8, 65], BF16)
            swa_kT = sb(sa, "swa_kT", [128, S], BF16)
            swa_v = sb(sa, "swa_v", [128, NB, 2, 65], BF16)
            bc_ga = sb(sa, "bc_ga", [128, D], F32)
            bc_on = sb(sa, "bc_on", [128, D], F32)
            bc_qn = sb(sa, "bc_qn", [128, 256], F32)
            bc_kvn = sb(sa, "bc_kvn", [128, 128], F32)
            esink = sb(sa, "esink", [128, 8], F32)
            mprev = sb(sa, "mprev", [128, 128], BF16)
            mnext = sb(sa, "mnext", [128, 128], BF16)
            WkupT = sb(sa, "WkupT", [128, 4, 128], BF16)
            wvup = sb(sa, "wvup", [128, 512], BF16)
            xts = [sb(sa, f"xa{i}", [128, D], F32) for i in range(2)]
            xnb = sb(sa, "xnb", [128, D], BF16)
            hTc = sb(sa, "hTc", [128, 8, 512], BF16)
            cs64 = sb(sa, "cs64", [128, 2, 512], F32)
            cs32 = sb(sa, "cs32", [64, 2, 512], F32)
            tmpA = sb(sa, "tmpA", [128, 1024], F32)
            tmpB = sb(sa, "tmpB", [128, 512], F32)
            stA = sb(sa, "stA", [128, 8], F32)
            stB = sb(sa, "stB", [128, 8], F32)
            pT = ps(sa, "pTa", [128, 8, 128], BF16)
            pXX = ps(sa, "pXX", [128, 1024], F32)
            pSS = ps(sa, "pSS", [128, 1024], F32)
            pX = [pXX[:, i * 512:(i + 1) * 512] for i in range(2)]
            pSs = [pSS[:, i * 512:(i + 1) * 512] for i in range(2)]
            pO = [ps(sa, f"pO{i}", [128, 4, 65], F32) for i in range(2)]
            pSW = ps(sa, "pSW", [128, 512], F32)

            def bload(dst, dk, src):
                P.op("sp", lambda e: e.dma_start(out=dst[:, :], in_=src.partition_broadcast(128)),
                     reads=["modd"], writes=[dk], dma_sem="d_" + dk)
            bload(bc_ga, "bc_ga", modd[0:1, 2 * D:3 * D])
            bload(bc_on, "bc_on", out_norm)
            bload(bc_qn, "bc_qn", mla_q_norm)
            bload(bc_kvn, "bc_kvn", mla_kv_norm)
            bload(esink, "esink", swa_sink)
            P.op("act", lambda e: e.activation(esink[:, :], esink[:, :], AF.Exp), reads=["esink"], writes=["esink"])
            P.op("dve", lambda e: e.tensor_single_scalar(mprev[:, :], dif_i[:, :], 0.0, ALU.is_le),
                 reads=["dif_i"], writes=["mprev"])
            P.op("dve", lambda e: e.tensor_single_scalar(mnext[:, :], dif_i[:, :], 0.0, ALU.is_ge),
                 reads=["dif_i"], writes=["mnext"])
            P.op("dve", lambda e: e.memset(mla_v[:, :, :, 64:65], 1.0), writes=["mla_v"])
            P.op("dve", lambda e: e.memset(swa_v[:, :, :, 64:65], 1.0), writes=["swa_v"])

            pxi = [0]

            def nextpx():
                i = pxi[0] % 2
                pxi[0] += 1
                return pX[i], f"pX{i}"

            def load_norm_T(n, t):
                xt, xk = xts[n % 2], f"xa{n % 2}"
                P.op("sp", lambda e: e.dma_start(out=xt[:, :], in_=x[n * 128:(n + 1) * 128, :]),
                     writes=[xk], dma_sem="d_" + xk)
                P.op("act", lambda e: e.activation(xnb[:, :], xt[:, :], AF.Square, accum_out=stA[:, 0:1]),
                     reads=[xk], writes=["xnb", "stA0"])
                P.op("act", lambda e: e.activation(stA[:, 1:2], stA[:, 0:1], AF.Sqrt, bias=EPS, scale=1.0 / D),
                     reads=["stA0"], writes=["stA1"])
                P.op("dve", lambda e: e.reciprocal(stA[:, 2:3], stA[:, 1:2]), reads=["stA1"], writes=["stA2"])
                P.op("act", lambda e: e.activation(xnb[:, :], xt[:, :], AF.Copy, scale=stA[:, 2:3]),
                     reads=[xk, "stA2"], writes=["xnb"])

                def tr(e):
                    for kc in range(8):
                        i = e.transpose(pT[:, kc, :], xnb[:, kc * 128:(kc + 1) * 128], ident_b[:, :])
                    return i
                P.op("pe", tr, reads=["xnb", "ident_b"], writes=["pT"])

                def ev(e):
                    for kc in range(8):
                        i = e.activation(hTc[:, kc, t * 128:(t + 1) * 128], pT[:, kc, :], AF.Identity,
                                         bias=fm_a[:, 8 + kc:9 + kc], scale=fm_a[:, kc:kc + 1])
                    return i
                P.op("act", ev, reads=["pT", "fm_a"], writes=["hTc"])

            def proj_fm(w, wk, c0, M, rhs, rhsk, nk=8):
                pp, pk = nextpx()

                def mm(e):
                    for kc in range(nk):
                        i = e.matmul(pp[0:M, :], w[:, kc, c0:c0 + M], rhs[:, kc, :], start=(kc == 0), stop=(kc == nk - 1))
                    return i
                P.op("pe", mm, reads=[wk, rhsk], writes=[pk])
                return pp, pk

            def rope(pr, prk, psw, pswk, cs, csk, M, dst, dstk):
                P.op("dve", lambda e: e.tensor_tensor(tmpA[0:M, 0:512], pr[0:M, :], cs[0:M, 0, :], ALU.mult),
                     reads=[prk, csk], writes=["tmpA"])
                P.op("dve", lambda e: e.tensor_tensor(tmpB[0:M, 0:512], psw[0:M, :], cs[0:M, 1, :], ALU.mult),
                     reads=[pswk, csk], writes=["tmpB"])
                P.op("dve", lambda e: e.tensor_tensor(dst, tmpA[0:M, 0:512], tmpB[0:M, 0:512], ALU.add),
                     reads=["tmpA", "tmpB"], writes=[dstk])

            def load_rope(tc):
                c0 = tc * 512
                P.op("sp", lambda e: e.dma_start(out=cs64[:, :, :], in_=rope64[:, :, c0:c0 + 512].rearrange("t p s -> p t s")),
                     writes=["cs64"], dma_sem="d_cs64")
                P.op("sp", lambda e: e.dma_start(out=cs32[:, :, :], in_=rope32[:, :, c0:c0 + 512].rearrange("t p s -> p t s")),
                     writes=["cs32"], dma_sem="d_cs32")

            w_in_v = w_in.rearrange("(k p) n -> p k n", p=128)

            with ExitStack() as sA:
                wA = sb(sA, "wA", [128, 8, 640], BF16)
                stg = [sb(sA, f"stgA{i}", [128, 1184], F32) for i in range(2)]
                kvst = sb(sA, "kvst", [128, 1024], F32)
                knope = sb(sA, "knope", [128, 512], F32)
                kvnb = sb(sA, "kvnb", [128, 128], BF16)
                for kc in range(8):
                    sg, sk = stg[kc % 2], f"stgA{kc % 2}"
                    P.op("sp", lambda e, sg=sg, kc=kc: e.dma_start(out=sg[:, :], in_=w_in_v[:, kc, :]),
                         writes=[sk], dma_sem="d_" + sk)
                    P.op("act", lambda e, sg=sg, kc=kc: e.copy(wA[:, kc, 0:128], sg[:, 512:640]), reads=[sk], writes=["wA"])
                    P.op("act", lambda e, sg=sg, kc=kc: e.copy(wA[:, kc, 256:384], sg[:, 640:768]), reads=[sk], writes=["wA"])
                    P.op("act", lambda e, sg=sg, kc=kc: e.copy(wA[:, kc, 384:512], sg[:, 1024:1152]), reads=[sk], writes=["wA"])
                    P.op("act", lambda e, sg=sg, kc=kc: e.copy(wA[:, kc, 512:544], sg[:, 1152:1184]), reads=[sk], writes=["wA"])
                    P.op("act", lambda e, sg=sg, kc=kc: e.copy(wA[:, kc, 544:576], sg[:, 1152:1184]), reads=[sk], writes=["wA"])
                    ksrc = sg[:, 512:640].rearrange("p (g t d) -> p g t d", g=2, t=2)
                    kdst = wA[:, kc, 128:256].rearrange("p (g t d) -> p g t d", g=2, t=2)
                    P.op("dve", lambda e, a=kdst, b=ksrc: e.tensor_copy(a[:, :, 0, :], b[:, :, 1, :]), reads=[sk], writes=["wA"])
                    P.op("dve", lambda e, a=kdst, b=ksrc: e.tensor_copy(a[:, :, 1, :], b[:, :, 0, :]), reads=[sk], writes=["wA"])
                    for o_ in (576, 608):
                        P.op("dve", lambda e, sg=sg, kc=kc, o_=o_: e.tensor_copy(wA[:, kc, o_:o_ + 16], sg[:, 1168:1184]), reads=[sk], writes=["wA"])
                        P.op("dve", lambda e, sg=sg, kc=kc, o_=o_: e.tensor_copy(wA[:, kc, o_ + 16:o_ + 32], sg[:, 1152:1168]), reads=[sk], writes=["wA"])
                P.op("sp", lambda e: e.dma_start(out=kvst[:, :], in_=w_kv_up), writes=["kvst"], dma_sem="d_kvst")
                kv4 = kvst[:, :].rearrange("p (h t d) -> p h t d", h=8, t=2)
                P.op("dve", lambda e: e.tensor_copy(knope[:, :].rearrange("p (h d) -> p h d", h=8), kv4[:, :, 0, :]),
                     reads=["kvst"], writes=["knope"])
                P.op("dve", lambda e: e.tensor_copy(wvup[:, :].rearrange("p (h d) -> p h d", h=8), kv4[:, :, 1, :]),
                     reads=["kvst"], writes=["wvup"])

                def trk(e):
                    for j in range(4):
                        i = e.transpose(pX[0][:, j * 128:(j + 1) * 128], knope[:, j * 128:(j + 1) * 128], ident_f[:, :])
                    return i
                P.op("pe", trk, reads=["knope", "ident_f"], writes=["pX0"])
                P.op("act", lambda e: e.copy(WkupT[:, :, :], pX[0][:, :].rearrange("p (j r) -> p j r", j=4)),
                     reads=["pX0"], writes=["WkupT"])

                for tc in range(8):
                    c0 = tc * 512
                    load_rope(tc)
                    for t in range(4):
                        load_norm_T(tc * 4 + t, t)
                    pr, prk = proj_fm(wA, "wA", 0, 128, hTc, "hTc")
                    psw, pswk = proj_fm(wA, "wA", 128, 128, hTc, "hTc")
                    rope(pr, prk, psw, pswk, cs64, "cs64", 128, swa_kT[:, c0:c0 + 512], "swa_kT")
                    pr, prk = proj_fm(wA, "wA", 512, 64, hTc, "hTc")
                    psw, pswk = proj_fm(wA, "wA", 576, 64, hTc, "hTc")
                    rope(pr, prk, psw, pswk, cs32, "cs32", 64, krT[0:64, c0:c0 + 512], "krT")
                    for t in range(4):
                        n = tc * 4 + t
                        pp, pk = nextpx()

                        def mm(e, pp=pp, t=t):
                            for kc in range(8):
                                i = e.matmul(pp[:, 0:256], hTc[:, kc, t * 128:(t + 1) * 128], wA[:, kc, 256:512],
                                             start=(kc == 0), stop=(kc == 7))
                            return i
                        P.op("pe", mm, reads=["hTc", "wA"], writes=[pk])
                        P.op("act", lambda e, pp=pp, n=n: e.copy(swa_v[:, n, :, 0:64], pp[:, 0:128].rearrange("p (g d) -> p g d", g=2)),
                             reads=[pk], writes=["swa_v"])
                        P.op("act", lambda e, pp=pp: e.activation(tmpB[:, 0:128], pp[:, 128:256], AF.Square, accum_out=stB[:, 0:1]),
                             reads=[pk], writes=["tmpB", "stB0"])
                        P.op("act", lambda e: e.activation(stB[:, 1:2], stB[:, 0:1], AF.Sqrt, bias=EPS, scale=1.0 / 128),
                             reads=["stB0"], writes=["stB1"])
                        P.op("dve", lambda e: e.reciprocal(stB[:, 2:3], stB[:, 1:2]), reads=["stB1"], writes=["stB2"])
                        P.op("dve", lambda e, pp=pp: e.scalar_tensor_tensor(kvnb[:, :], pp[:, 128:256], stB[:, 2:3], bc_kvn[:, :],
                                                                           ALU.mult, ALU.mult),
                             reads=[pk, "stB2", "bc_kvn"], writes=["kvnb"])
                        P.op("pe", lambda e: e.transpose(pT[:, 0, :], kvnb[:, :], ident_b[:, :]),
                             reads=["kvnb", "ident_b"], writes=["pT"])
                        P.op("act", lambda e, n=n: e.copy(kvnT[:, n * 128:(n + 1) * 128], pT[:, 0, :]), reads=["pT"], writes=["kvnT"])
                        pp2, pk2 = nextpx()
                        P.op("pe", lambda e, pp2=pp2, n=n: e.matmul(pp2[:, :], kvnT[:, n * 128:(n + 1) * 128], wvup[:, :],
                                                                   start=True, stop=True),
                             reads=["kvnT", "wvup"], writes=[pk2])
                        P.op("dve", lambda e, pp2=pp2, n=n: e.tensor_copy(mla_v[:, n, :, 0:64],
                                                                         pp2[:, :].rearrange("p (h d) -> p h d", h=8)),
                             reads=[pk2], writes=["mla_v"])
                P.flush()

            with ExitStack() as sB:
                wB = sb(sB, "wB", [128, 8, 1280], BF16)
                wqn = sb(sB, "wqn", [128, 2, 512], BF16)
                wqr = sb(sB, "wqr", [128, 2, 1024], BF16)
                woutb = sb(sB, "woutb", [128, 8, 1024], BF16)
                with ExitStack() as sW:
                    stg = [sb(sW, f"stgB{i}", [128, 1184], F32) for i in range(2)]
                    for kc in range(8):
                        sg, sk = stg[kc % 2], f"stgB{kc % 2}"
                        P.op("sp", lambda e, sg=sg, kc=kc: e.dma_start(out=sg[:, :], in_=w_in_v[:, kc, :]),
                             writes=[sk], dma_sem="d_" + sk)
                        qsrc = sg[:, 0:512].rearrange("p (g j d) -> p g j d", g=2, j=4)
                        qdst = wB[:, kc, 0:512].rearrange("p (j g d) -> p g j d", j=4, g=2)
                        P.op("act", lambda e, a=qdst, b=qsrc: e.copy(a[:, 0, :, :], b[:, 0, :, :]), reads=[sk], writes=["wB"])
                        P.op("act", lambda e, a=qdst, b=qsrc: e.copy(a[:, 1, :, :], b[:, 1, :, :]), reads=[sk], writes=["wB"])
                        qsrc5 = sg[:, 0:512].rearrange("p (g j t d) -> p g j t d", g=2, j=4, t=2)
                        qdst5 = wB[:, kc, 512:1024].rearrange("p (j g t d) -> p g j t d", j=4, g=2, t=2)
                        for g_ in range(2):
                            for t_ in range(2):
                                P.op("dve", lambda e, a=qdst5, b=qsrc5, g_=g_, t_=t_: e.tensor_copy(a[:, g_, :, t_, :], b[:, g_, :, 1 - t_, :]),
                                     reads=[sk], writes=["wB"])
                        P.op("act", lambda e, sg=sg, kc=kc: e.copy(wB[:, kc, 1024:1280], sg[:, 768:1024]), reads=[sk], writes=["wB"])
                    for a_ in range(2):
                        sg, sk = stg[a_], f"stgB{a_}"
                        P.op("sp", lambda e, sg=sg, a_=a_: e.dma_start(out=sg[:, 0:768], in_=w_q_up[a_ * 128:(a_ + 1) * 128, :]),
                             writes=[sk], dma_sem="d_" + sk)
                        u3 = sg[:, 0:768].rearrange("p (h c) -> p h c", h=8)
                        P.op("act", lambda e, u3=u3, a_=a_: e.copy(wqn[:, a_, :].rearrange("p (h d) -> p h d", h=8), u3[:, :, 0:64]),
                             reads=[sk], writes=["wqn"])
                        rw4 = wqr[:, a_, 0:512].rearrange("p (h r d) -> p h r d", h=8, r=2)
                        sw4 = wqr[:, a_, 512:1024].rearrange("p (h r d) -> p h r d", h=8, r=2)
                        for r_ in range(2):
                            P.op("act", lambda e, u3=u3, rw4=rw4, r_=r_: e.copy(rw4[:, :, r_, :], u3[:, :, 64:96]), reads=[sk], writes=["wqr"])
                            P.op("dve", lambda e, u3=u3, sw4=sw4, r_=r_: e.tensor_copy(sw4[:, :, r_, 0:16], u3[:, :, 80:96]), reads=[sk], writes=["wqr"])
                            P.op("dve", lambda e, u3=u3, sw4=sw4, r_=r_: e.tensor_copy(sw4[:, :, r_, 16:32], u3[:, :, 64:80]), reads=[sk], writes=["wqr"])
                    w_out_v = w_out.rearrange("(k p) n -> p k n", p=128)
                    for kc in range(8):
                        sg, sk = stg[kc % 2], f"stgB{kc % 2}"
                        P.op("sp", lambda e, sg=sg, kc=kc: e.dma_start(out=sg[:, 0:1024], in_=w_out_v[:, kc, :]),
                             writes=[sk], dma_sem="d_" + sk)
                        if kc % 2 == 0:
                            P.op("act", lambda e, sg=sg, kc=kc: e.copy(woutb[:, kc, :], sg[:, 0:1024]), reads=[sk], writes=["woutb"])
                        else:
                            P.op("dve", lambda e, sg=sg, kc=kc: e.tensor_copy(woutb[:, kc, :], sg[:, 0:1024]), reads=[sk], writes=["woutb"])

                    P.flush()
                qT_swa = sb(sB, "qT_swa", [128, 4, 512], BF16)
                qnb = sb(sB, "qnb", [128, 256], BF16)
                qnT = sb(sB, "qnT", [128, 2, 512], BF16)
                qnopeT = sb(sB, "qnopeT", [128, 4, 512], BF16)
                qabsT = sb(sB, "qabsT", [128, 8, 512], BF16)
                qropeT = sb(sB, "qropeT", [64, 8, 512], BF16)
                E3 = [sb(sB, "E3_0", [128, 3, 512], BF16)] * 2
                Em = [sb(sB, f"Em{i}", [128, 1024], BF16) for i in range(2)]
                mixeds = [sb(sB, f"mixed{i}", [128, 4, 1024], BF16) for i in range(2)]
                mixn = sb(sB, "mixn", [128, 1024], BF16)
                mixT = sb(sB, "mixT", [128, 8, 128], BF16)
                ssqs = [sb(sB, f"ssq{i}", [128, 4, 16], F32) for i in range(2)]
                sqt = sb(sB, "sqt", [128, 4, 64], BF16)
                zt = sb(sB, "zt", [128, 4], F32)
                rzz = sb(sB, "rzz", [128, 4], F32)
                zts = sb(sB, "zts", [128, 4], F32)
                rzs = sb(sB, "rzs", [128, 4], F32)
                sqs = sb(sB, "sqs", [128, 4, 64], BF16)
                ssab = sb(sB, "ssab", [128, 2, 4], F32)
                rsab = sb(sB, "rsab", [128, 2, 4], F32)

                SC_SWA = 64 ** -0.5
                SC_MLA = 96 ** -0.5
                def chunk_body(tc, mixed, ssq, mxk, sqk, epi_prev):
                    c0 = tc * 512
                    load_rope(tc)
                    for t in range(4):
                        load_norm_T(tc * 4 + t, t)
                    for j in range(4):
                        pr, prk = proj_fm(wB, "wB", j * 128, 128, hTc, "hTc")
                        psw, pswk = proj_fm(wB, "wB", 512 + j * 128, 128, hTc, "hTc")
                        rope(pr, prk, psw, pswk, cs64, "cs64", 128, qT_swa[:, j, :], "qT_swa")
                    for t in range(4):
                        pp, pk = nextpx()

                        def mm(e, pp=pp, t=t):
                            for kc in range(8):
                                i = e.matmul(pp[:, 0:256], hTc[:, kc, t * 128:(t + 1) * 128], wB[:, kc, 1024:1280],
                                             start=(kc == 0), stop=(kc == 7))
                            return i
                        P.op("pe", mm, reads=["hTc", "wB"], writes=[pk])
                        P.op("act", lambda e, pp=pp: e.activation(tmpB[:, 0:256], pp[:, 0:256], AF.Square, accum_out=stB[:, 0:1]),
                             reads=[pk], writes=["tmpB", "stB0"])
                        P.op("act", lambda e: e.activation(stB[:, 1:2], stB[:, 0:1], AF.Sqrt, bias=EPS, scale=1.0 / 256),
                             reads=["stB0"], writes=["stB1"])
                        P.op("dve", lambda e: e.reciprocal(stB[:, 2:3], stB[:, 1:2]), reads=["stB1"], writes=["stB2"])
                        P.op("dve", lambda e, pp=pp: e.scalar_tensor_tensor(qnb[:, :], pp[:, 0:256], stB[:, 2:3], bc_qn[:, :],
                                                                           ALU.mult, ALU.mult),
                             reads=[pk, "stB2", "bc_qn"], writes=["qnb"])

                        def tr2(e):
                            for a_ in range(2):
                                i = e.transpose(pT[:, a_, :], qnb[:, a_ * 128:(a_ + 1) * 128], ident_b[:, :])
                            return i
                        P.op("pe", tr2, reads=["qnb", "ident_b"], writes=["pT"])
                        P.op("act", lambda e, t=t: e.copy(qnT[:, :, t * 128:(t + 1) * 128], pT[:, 0:2, :]), reads=["pT"], writes=["qnT"])
                    for j in range(4):
                        pp, pk = proj_fm(wqn, "wqn", j * 128, 128, qnT, "qnT", nk=2)
                        P.op("act", lambda e, pp=pp, j=j: e.copy(qnopeT[:, j, :], pp[:, :]), reads=[pk], writes=["qnopeT"])
                    for h in range(8):
                        j, m = h // 2, h % 2
                        pp, pk = nextpx()
                        P.op("pe", lambda e, pp=pp, j=j, m=m: e.matmul(pp[:, :], WkupT[m * 64:(m + 1) * 64, j, :],
                                                                      qnopeT[m * 64:(m + 1) * 64, j, :], start=True, stop=True),
                             reads=["WkupT", "qnopeT"], writes=[pk])
                        if h % 2 == 0:
                            P.op("act", lambda e, pp=pp, h=h: e.copy(qabsT[:, h, :], pp[:, :]), reads=[pk], writes=["qabsT"])
                        else:
                            P.op("dve", lambda e, pp=pp, h=h: e.tensor_copy(qabsT[:, h, :], pp[:, :]), reads=[pk], writes=["qabsT"])
                    for h in range(8):
                        pr, prk = proj_fm(wqr, "wqr", h * 64, 64, qnT, "qnT", nk=2)
                        psw, pswk = proj_fm(wqr, "wqr", 512 + h * 64, 64, qnT, "qnT", nk=2)
                        rope(pr, prk, psw, pswk, cs32, "cs32", 64, qropeT[0:64, h, :], "qropeT")

                    def swa_chunk():
                        for t in range(4):
                            n = tc * 4 + t
                            js = [j for j in (n - 1, n, n + 1) if 0 <= j < NB]
                            for g in range(2):
                                e3, e3k = E3[0], "E3_0"
                                for idx, j in enumerate(js):
                                    psc, psk = pSW, "pSW"
                                    P.op("pe", lambda e, psc=psc, g=g, j=j, t=t: e.matmul(
                                        psc[:, :], swa_kT[g * 64:(g + 1) * 64, j * 128:(j + 1) * 128],
                                        qT_swa[g * 64:(g + 1) * 64, :, t * 128:(t + 1) * 128], start=True, stop=True),
                                        reads=["swa_kT", "qT_swa"], writes=[psk])
                                    P.op("act", lambda e, psc=psc, e3=e3, idx=idx: e.activation(e3[:, idx, :], psc[:, :], AF.Exp, scale=SC_SWA),
                                         reads=[psk], writes=[e3k])
                                    if j != n:
                                        mk_, mkk = (mprev, "mprev") if j == n - 1 else (mnext, "mnext")
                                        P.op("dve", lambda e, e3=e3, idx=idx, mk_=mk_: e.tensor_tensor(
                                            e3[:, idx, :].rearrange("p (h q) -> p h q", h=4),
                                            e3[:, idx, :].rearrange("p (h q) -> p h q", h=4),
                                            mk_[:, :].rearrange("p (o q) -> p o q", o=1).to_broadcast([128, 4, 128]), ALU.mult),
                                            reads=[e3k, mkk], writes=[e3k])
                                po, pok = pO[1], "pO1"

                                def pv(e, po=po, e3=e3, g=g, js=js):
                                    for hh in range(4):
                                        for idx, j in enumerate(js):
                                            i = e.matmul(po[:, hh, :], e3[:, idx, hh * 128:(hh + 1) * 128], swa_v[:, j, g, :],
                                                         start=(idx == 0), stop=(idx == len(js) - 1))
                                    return i
                                P.op("pe", pv, reads=[e3k, "swa_v"], writes=[pok])
                                P.op("dve", lambda e, po=po, g=g: e.tensor_tensor(zts[:, :], po[:, :, 64], esink[:, 4 * g:4 * g + 4], ALU.add),
                                     reads=[pok, "esink"], writes=["zts"])
                                P.op("dve", lambda e: e.reciprocal(rzs[:, :], zts[:, :]), reads=["zts"], writes=["rzs"])
                                P.op("dve", lambda e, po=po, g=g, t=t: e.tensor_tensor(
                                    sqs[:, :, :], po[:, :, 0:64],
                                    rzs[:, :].rearrange("p (h o) -> p h o", o=1).to_broadcast([128, 4, 64]), ALU.mult),
                                    reads=[pok, "rzs"], writes=["sqs"])
                                P.op("act", lambda e, g=g, t=t: e.copy(mixed[:, t, g * 256:(g + 1) * 256].rearrange("p (h d) -> p h d", h=4),
                                                                      sqs[:, :, :]),
                                     reads=["sqs"], writes=[mxk])
                                P.op("dve", lambda e: e.tensor_tensor(sqs[:, :, :], sqs[:, :, :], sqs[:, :, :], ALU.mult),
                                     reads=["sqs"], writes=["sqs"])
                                P.op("dve", lambda e, g=g, t=t: e.tensor_reduce(ssq[:, t, g:g + 1], sqs[:, :, :], AX.XY, ALU.add),
                                     reads=["sqs"], writes=[sqk])

                    swa_thunks = list(epi_prev) + P.capture(swa_chunk)
                    n_iter_left = [8 * (NB // 2)]
                    sbufs = [(pSS, ["pS0", "pS1"]), (pXX, ["pX0", "pX1"])]
                    for h in range(8):
                        po, pok = pO[0], "pO0"

                        def score(kp, h=h):
                            pst, psk = sbufs[kp % 2]

                            def mm(e):
                                for u_ in range(2):
                                    kb = kp * 2 + u_
                                    e.matmul(pst[:, u_ * 512:(u_ + 1) * 512], kvnT[:, kb * 128:(kb + 1) * 128], qabsT[:, h, :],
                                             start=True, stop=False)
                                for u_ in range(2):
                                    kb = kp * 2 + u_
                                    i = e.matmul(pst[:, u_ * 512:(u_ + 1) * 512], krT[u_ * 32:(u_ + 1) * 32, kb * 128:(kb + 1) * 128],
                                                 qropeT[u_ * 32:(u_ + 1) * 32, h, :], start=False, stop=True)
                                return i
                            P.op("pe", mm, reads=["kvnT", "krT", "qabsT", "qropeT"], writes=psk)
                        score(0)
                        for kp in range(NB // 2):
                            pst, psk = sbufs[kp % 2]
                            em, emk = Em[kp % 2], f"Em{kp % 2}"
                            gok = [f"go{tc}_{h}"] if kp == 0 else []
                            P.op("act", lambda e, pst=pst, em=em: e.activation(em[:, :], pst[:, :], AF.Exp, scale=SC_MLA),
                                 reads=psk, writes=[emk] + gok)
                            if kp == 0:
                                issue_cast(tc * 8 + h, gok)
                            if kp + 1 < NB // 2:
                                score(kp + 1)

                            def pv(e, em=em, kp=kp, po=po, h=h):
                                for u_ in range(2):
                                    kb = kp * 2 + u_
                                    for t in range(4):
                                        i = e.matmul(po[:, t, :], em[:, u_ * 512 + t * 128:u_ * 512 + (t + 1) * 128], mla_v[:, kb, h, :],
                                                     start=(kb == 0 and t == 0), stop=(kb == NB - 1), skip_group_check=True)
                                return i
                            P.op("pe", pv, reads=[emk, "mla_v"], writes=[pok])
                            k_ = (len(swa_thunks) + n_iter_left[0] - 1) // n_iter_left[0]
                            n_iter_left[0] -= 1
                            P.replay(swa_thunks[:k_])
                            del swa_thunks[:k_]
                        P.op("dve", lambda e, po=po: e.reciprocal(rzz[:, :], po[:, :, 64]), reads=[pok], writes=["rzz"])
                        P.op("dve", lambda e, po=po: e.tensor_tensor(
                            sqt[:, :, :], po[:, :, 0:64],
                            rzz[:, :].rearrange("p (h o) -> p h o", o=1).to_broadcast([128, 4, 64]), ALU.mult),
                            reads=[pok, "rzz"], writes=["sqt"])
                        P.op("act", lambda e, h=h: e.copy(mixed[:, :, 512 + h * 64:512 + (h + 1) * 64], sqt[:, :, :]),
                             reads=["sqt"], writes=[mxk])
                        P.op("dve", lambda e: e.tensor_tensor(sqt[:, :, :], sqt[:, :, :], sqt[:, :, :], ALU.mult),
                             reads=["sqt"], writes=["sqt"])
                        P.op("dve", lambda e, h=h: e.tensor_reduce(ssq[:, :, 2 + h], sqt[:, :, :], AX.X, ALU.add),
                             reads=["sqt"], writes=[sqk])

                    P.replay(swa_thunks)
                    del swa_thunks[:]

                    def epilogue():
                        P.op("dve", lambda e: e.tensor_reduce(ssab[:, 0, :], ssq[:, :, 0:2], AX.X, ALU.add), reads=[sqk], writes=["ssab"])
                        P.op("dve", lambda e: e.tensor_reduce(ssab[:, 1, :], ssq[:, :, 2:10], AX.X, ALU.add), reads=["ssab", sqk], writes=["ssab"])
                        P.op("act", lambda e: e.activation(ssab[:, :, :], ssab[:, :, :], AF.Sqrt, bias=EPS, scale=1.0 / 512),
                             reads=["ssab"], writes=["ssab"])
                        P.op("dve", lambda e: e.reciprocal(rsab[:, :, :], ssab[:, :, :]), reads=["ssab"], writes=["rsab"])
                        for t in range(4):
                            n = tc * 4 + t
                            for gi in range(2):
                                P.op("dve", lambda e, t=t, gi=gi: e.scalar_tensor_tensor(
                                    mixn[:, gi * 512:(gi + 1) * 512], mixed[:, t, gi * 512:(gi + 1) * 512], rsab[:, gi, t:t + 1],
                                    bc_on[:, gi * 512:(gi + 1) * 512], ALU.mult, ALU.mult),
                                    reads=[mxk, "rsab", "bc_on"], writes=["mixn"])

                            def trm(e):
                                for kc in range(8):
                                    i = e.transpose(pT[:, kc, :], mixn[:, kc * 128:(kc + 1) * 128], ident_b[:, :])
                                return i
                            P.op("pe", trm, reads=["mixn", "ident_b"], writes=["pT"])
                            P.op("act", lambda e: e.copy(mixT[:, :, :], pT[:, :, :]), reads=["pT"], writes=["mixT"])
                            xt, xk = xts[n % 2], f"xa{n % 2}"
                            P.op("sp", lambda e, xt=xt, n=n: e.dma_start(out=xt[:, :], in_=x[n * 128:(n + 1) * 128, :]),
                                 writes=[xk], dma_sem="d_" + xk)
                            for hf in range(2):
                                pp, pk = pSW, "pSW"

                                def mmo(e, pp=pp, hf=hf):
                                    for kc in range(8):
                                        i = e.matmul(pp[:, :], mixT[:, kc, :], woutb[:, kc, hf * 512:(hf + 1) * 512],
                                                     start=(kc == 0), stop=(kc == 7))
                                    return i
                                P.op("pe", mmo, reads=["mixT", "woutb"], writes=[pk])
                                P.op("dve", lambda e, pp=pp, hf=hf: e.tensor_tensor(tmpA[:, hf * 512:(hf + 1) * 512], pp[:, :],
                                                                                   bc_ga[:, hf * 512:(hf + 1) * 512], ALU.mult),
                                     reads=[pk, "bc_ga"], writes=["tmpA"])
                            P.op("dve", lambda e, xt=xt: e.tensor_tensor(tmpA[:, :], tmpA[:, :], xt[:, :], ALU.add),
                                 reads=["tmpA", xk], writes=["tmpA"])
                            P.op("sp", lambda e, n=n: e.dma_start(out=x1d[n * 128:(n + 1) * 128, :], in_=tmpA[:, :]),
                                 reads=["tmpA"], writes=["x1d"], dma_sem="d_x1d")

                    return P.capture(epilogue)
                epi_prev = []
                for tc in range(n_chunks):
                    epi_prev = chunk_body(tc, mixeds[tc % 2], ssqs[tc % 2], f"mixed{tc % 2}", f"ssq{tc % 2}", epi_prev)
                P.replay(epi_prev)
                P.flush()
        x1src = x1d if do_attn else x
        for i_ in range(64):
            issue_cast(i_, [])

        if do_peer:
          with ExitStack() as st:
            wq = sb(st, "wq", [128, 8, 2048], BF16)
            bc_sc1f = sb(st, "bc_sc1f", [128, D], F32)
            bc_shf = sb(st, "bc_shf", [128, D], F32)
            bc_gf = sb(st, "bc_gf", [128, D], F32)
            bc_fin = sb(st, "bc_fin", [128, D], F32)
            for dst, dk, off in ((bc_shf, "bc_shf", 3 * D), (bc_sc1f, "bc_sc1f", 4 * D), (bc_gf, "bc_gf", 5 * D)):
                P.op("sp", lambda e, dst=dst, off=off: e.dma_start(
                    out=dst[:, :], in_=modd[0:1, off:off + D].partition_broadcast(128)),
                    reads=["modd"], writes=[dk], dma_sem="d_" + dk)
            P.op("sp", lambda e: e.dma_start(out=bc_fin[:, :], in_=final_norm.partition_broadcast(128)),
                 writes=["bc_fin"], dma_sem="d_fin")
            keysT = sb(st, "keysT", [128, 16, 128], BF16)
            pS = ps(st, "pS", [128, 2048], F32)
            pQ = ps(st, "pQ", [128, 4, 128], F32)
            pAcc = ps(st, "pAcc", [128, D], F32)
            pT = ps(st, "pT", [128, 8, 128], BF16)
            with ExitStack() as sW:
                kst = sb(sW, "kst", [128, 16, 128], F32)
                wstg = [sb(sW, f"wstg{i}", [128, 2048], F32) for i in range(2)]
                wqv = w_pq.rearrange("(k p) n -> p k n", p=128)
                for kc in range(8):
                    wt = wstg[kc % 2]
                    wk = f"wstg{kc % 2}"
                    P.op("sp", lambda e, wt=wt, kc=kc: e.dma_start(out=wt[:, :], in_=wqv[:, kc, :]),
                         writes=[wk], dma_sem="d_" + wk)
                    if kc % 2 == 0:
                        P.op("dve", lambda e, wt=wt, kc=kc: e.tensor_copy(wq[:, kc, :], wt[:, :]), reads=[wk], writes=["wq"])
                    else:
                        P.op("act", lambda e, wt=wt, kc=kc: e.copy(wq[:, kc, :], wt[:, :]), reads=[wk], writes=["wq"])
                P.op("sp", lambda e: e.dma_start(out=kst[:, :, :], in_=sub_keys.rearrange("g n d -> n g d")),
                     writes=["kst"], dma_sem="d_kst")
                for g4 in range(4):
                    def tr(e, g4=g4):
                        for j in range(4):
                            g = g4 * 4 + j
                            i = e.transpose(pS[:, j * 128:(j + 1) * 128], kst[:, g, :], ident_f[:, :])
                        return i
                    P.op("pe", tr, reads=["kst", "ident_f"], writes=["pS"])
                    P.op("act", lambda e, g4=g4: e.copy(keysT[:, g4 * 4:(g4 + 1) * 4, :],
                                                      pS[:, 0:512].rearrange("p (g n) -> p g n", g=4)),
                         reads=["pS"], writes=["keysT"])

                P.flush()
            NS = 20
            G = 4
            ug = [sb(st, f"ug{i}", [128, 2 * D], BF16) for i in range(NS)]
            dg = [sb(st, f"dg{i}", [128, 128], BF16) for i in range(4)]
            x1t = [sb(st, f"x1t{i}", [128, D], F32) for i in range(2)]
            hh = [sb(st, f"hh{i}", [128, D], F32) for i in range(2)]
            junkb = sb(st, "junkb", [128, D], BF16)
            junkf = sb(st, "junkf", [128, D], BF16)
            hb = sb(st, "hb", [128, D], BF16)
            hT = sb(st, "hT", [128, 8, 128], BF16)
            qT = sb(st, "qT", [128, 16, 128], BF16)
            bufS = sb(st, "bufS", [128, 2048], F32)
            bufS2 = sb(st, "bufS2", [128, 2048], F32)
            tv = sb(st, "tv", [128, 16, 16], F32)
            ti = sb(st, "ti", [128, 16, 16], U32)
            tif = sb(st, "tif", [128, 16, 16], F32)
            best = sb(st, "best", [128, 8, 16], F32)
            pos = sb(st, "pos", [128, 8, 16], U32)
            posa = sb(st, "posa", [128, 8, 16], U32)
            posb = sb(st, "posb", [128, 8, 16], U32)
            paf = sb(st, "paf", [128, 8, 16], F32)
            pbf = sb(st, "pbf", [128, 8, 16], F32)
            If = sb(st, "If", [128, 8, 16], F32)
            Jf = sb(st, "Jf", [128, 8, 16], F32)
            ef = sb(st, "ef", [128, 128], F32)
            ei = [sb(st, f"ei{i}", [128, 128], I32) for i in range(2)]
            gg = [sb(st, f"gg{i}", [128, 8, 16], F32) for i in range(2)]
            nmx = sb(st, "nmx", [128, 8], F32)
            zs = sb(st, "zs", [128, 8], F32)
            rz = sb(st, "rz", [128, 8], F32)
            Aa = sb(st, "Aa", [128, 128], F32)
            ga = sb(st, "ga", [128, 128], F32)
            ww = sb(st, "ww", [128, 128], F32)
            acc = sb(st, "acc", [128, D], F32)
            st8 = sb(st, "st8", [128, 8], F32)
            yt = sb(st, "yt", [128, D], F32)

            def top16(src, srck, dst2, dst2k, nseg, seglen, tvv, tvk, tii, tik):
                def r1(e):
                    for g in range(nseg):
                        i = e.max(tvv[:, g, 0:8], src[:, g * seglen:(g + 1) * seglen])
                    return i
                P.op("dve", r1, reads=[srck], writes=[tvk])

                def r2(e):
                    for g in range(nseg):
                        i = e.match_replace(dst2[:, g * seglen:(g + 1) * seglen], tvv[:, g, 0:8],
                                            src[:, g * seglen:(g + 1) * seglen], NEG)
                    return i
                P.op("dve", r2, reads=[srck, tvk], writes=[dst2k])

                def r3(e):
                    for g in range(nseg):
                        i = e.max(tvv[:, g, 8:16], dst2[:, g * seglen:(g + 1) * seglen])
                    return i
                P.op("dve", r3, reads=[dst2k], writes=[tvk])

                def r4(e):
                    for g in range(nseg):
                        e.max_index(tii[:, g, 0:8], tvv[:, g, 0:8], src[:, g * seglen:(g + 1) * seglen])
                        i = e.max_index(tii[:, g, 8:16], tvv[:, g, 8:16], dst2[:, g * seglen:(g + 1) * seglen])
                    return i
                P.op("dve", r4, reads=[srck, dst2k, tvk], writes=[tik])

            def sel(n):
                b = n % 2
                xt, xk = x1t[b], f"x1t{b}"
                h, hk = hh[b], f"hh{b}"
                P.op("sp", lambda e: e.dma_start(out=xt[:, :], in_=x1src[n * 128:(n + 1) * 128, :]),
                     reads=["x1d"], writes=[xk], dma_sem="d_" + xk)
                P.op("act", lambda e: e.activation(junkb[:, :], xt[:, :], AF.Square, accum_out=st8[:, 0:1]),
                     reads=[xk], writes=["junkb", "st8a"])
                P.op("act", lambda e: e.activation(st8[:, 1:2], st8[:, 0:1], AF.Sqrt, bias=EPS, scale=1.0 / D),
                     reads=["st8a"], writes=["st8b"])
                P.op("dve", lambda e: e.reciprocal(st8[:, 2:3], st8[:, 1:2]), reads=["st8b"], writes=["st8c"])
                P.op("dve", lambda e: e.scalar_tensor_tensor(h[:, :], xt[:, :], st8[:, 2:3], bc_sc1f[:, :], ALU.mult, ALU.mult),
                     reads=[xk, "st8c", "bc_sc1f"], writes=[hk])
                P.op("dve", lambda e: e.tensor_tensor(h[:, :], h[:, :], bc_shf[:, :], ALU.add),
                     reads=[hk, "bc_shf"], writes=[hk])
                P.op("act", lambda e: e.copy(hb[:, :], h[:, :]), reads=[hk], writes=["hb"])

                def tr(e):
                    for kc in range(8):
                        i = e.transpose(pT[:, kc, :], hb[:, kc * 128:(kc + 1) * 128], ident_b[:, :])
                    return i
                P.op("pe", tr, reads=["hb", "ident_b"], writes=["pT"])
                P.op("act", lambda e: e.copy(hT[:, :, :], pT[:, :, :]), reads=["pT"], writes=["hT"])
                for r in range(4):
                    def qmm(e, r=r):
                        for j in range(4):
                            g = r * 4 + j
                            for kc in range(8):
                                i = e.matmul(pQ[:, j, :], wq[:, kc, g * 128:(g + 1) * 128], hT[:, kc, :],
                                             start=(kc == 0), stop=(kc == 7))
                        return i
                    P.op("pe", qmm, reads=["wq", "hT"], writes=["pQ"])
                    P.op("act", lambda e, r=r: e.copy(qT[:, r * 4:(r + 1) * 4, :], pQ[:, :, :]), reads=["pQ"], writes=["qT"])

                def smm(e):
                    for g in range(16):
                        i = e.matmul(pS[:, g * 128:(g + 1) * 128], qT[:, g, :], keysT[:, g, :], start=True, stop=True)
                    return i
                P.op("pe", smm, reads=["qT", "keysT"], writes=["pS"])
                P.op("act", lambda e: e.copy(bufS[:, :], pS[:, :]), reads=["pS"], writes=["bufS"])
                top16(bufS, "bufS", bufS2, "bufS2", 16, 128, tv, "tv", ti, "ti")
                P.op("dve", lambda e: e.tensor_copy(tif[:, :, :], ti[:, :, :]), reads=["ti"], writes=["tif"])
                tv4 = tv[:, :, :].rearrange("p (h t) k -> p h t k", t=2)
                tif4 = tif[:, :, :].rearrange("p (h t) k -> p h t k", t=2)
                cand = bufS[:, :].rearrange("p (h a b) -> p h a b", h=8, a=16)
                P.op("dve", lambda e: e.tensor_tensor(
                    cand, tv4[:, :, 0, :].rearrange("p h (a o) -> p h a o", o=1).to_broadcast([128, 8, 16, 16]),
                    tv4[:, :, 1:2, :].to_broadcast([128, 8, 16, 16]), ALU.add),
                    reads=["tv"], writes=["bufS"])
                top16(bufS, "bufS", bufS2, "bufS2", 8, 256, best, "best", pos, "pos")
                P.op("dve", lambda e: e.tensor_scalar_mul(nmx[:, :], best[:, :, 0], -1.0), reads=["best"], writes=["nmx"])
                g_ = gg[b]
                gk = f"gg{b}"

                def ex(e):
                    for hd in range(8):
                        i = e.activation(g_[:, hd, :], best[:, hd, :], AF.Exp, bias=nmx[:, hd:hd + 1],
                                         accum_out=zs[:, hd:hd + 1])
                    return i
                P.op("act", ex, reads=["best", "nmx"], writes=[gk, "zs"])
                P.op("dve", lambda e: e.reciprocal(rz[:, :], zs[:, :]), reads=["zs"], writes=["rz"])
                P.op("dve", lambda e: e.tensor_tensor(
                    g_[:, :, :], g_[:, :, :], rz[:, :].rearrange("p (h o) -> p h o", o=1).to_broadcast([128, 8, 16]), ALU.mult),
                    reads=[gk, "rz"], writes=[gk])
                P.op("dve", lambda e: e.tensor_single_scalar(posa[:, :, :], pos[:, :, :], 4, ALU.logical_shift_right),
                     reads=["pos"], writes=["posa"])
                P.op("dve", lambda e: e.tensor_single_scalar(posb[:, :, :], pos[:, :, :], 15, ALU.bitwise_and),
                     reads=["pos"], writes=["posb"])
                P.op("dve", lambda e: e.tensor_copy(paf[:, :, :], posa[:, :, :]), reads=["posa"], writes=["paf"])
                P.op("dve", lambda e: e.tensor_copy(pbf[:, :, :], posb[:, :, :]), reads=["posb"], writes=["pbf"])
                oh = bufS[:, :].rearrange("p (h k a) -> p h k a", h=8, k=16)
                oh2 = bufS2[:, :].rearrange("p (h k a) -> p h k a", h=8, k=16)
                io4 = iota16[:, :].rearrange("p (h k a) -> p h k a", h=1, k=1).to_broadcast([128, 8, 16, 16])
                for (pf, pfk, t, dst, dstk) in ((paf, "paf", 0, If, "If"), (pbf, "pbf", 1, Jf, "Jf")):
                    P.op("dve", lambda e, pf=pf: e.tensor_tensor(
                        oh, io4, pf[:, :, :].rearrange("p h (k o) -> p h k o", o=1).to_broadcast([128, 8, 16, 16]),
                        ALU.is_equal), reads=[pfk, "iota16"], writes=["bufS"])
                    P.op("dve", lambda e, t=t: e.tensor_tensor(
                        oh2, oh, tif4[:, :, t:t + 1, :].to_broadcast([128, 8, 16, 16]), ALU.mult),
                        reads=["bufS", "tif"], writes=["bufS2"])
                    P.op("dve", lambda e, dst=dst: e.tensor_reduce(dst[:, :, :], oh2, AX.X, ALU.add),
                         reads=["bufS2"], writes=[dstk])
                P.op("dve", lambda e: e.scalar_tensor_tensor(
                    ef[:, :], If[:, :, :].rearrange("p h k -> p (h k)"), 128.0,
                    Jf[:, :, :].rearrange("p h k -> p (h k)"), ALU.mult, ALU.add),
                    reads=["If", "Jf"], writes=["ef"])
                P.op("dve", lambda e: e.tensor_copy(ei[b][:, :], ef[:, :]), reads=["ef"], writes=[f"ei{b}"])

            slot = [0]

            def gather(n_b, j):
                s_ = slot[0] % NS
                slot[0] += 1
                P.op("pool", lambda e: e.indirect_dma_start(
                    out=ug[s_][:, :], out_offset=None, in_=uvd,
                    in_offset=bass.IndirectOffsetOnAxis(ap=ei[n_b][:, j:j + 1], axis=0)),
                    reads=[f"ei{n_b}"] + UVK, writes=[f"ug{s_}"], dma_sem=f"d_ug{s_}")
                return s_

            dgi = [0]

            def experts(n):
                b = n % 2
                xt, xk = x1t[b], f"x1t{b}"
                h, hk = hh[b], f"hh{b}"
                gflat = gg[b][:, :, :].rearrange("p h k -> p (h k)")
                ngrp = 128 // G
                pend = None

                def vside(grp, slots):
                    cs = slice(grp * G, (grp + 1) * G)
                    kq = grp % 4
                    P.op("dve", lambda e: e.tensor_tensor(ww[:, cs], ga[:, cs], gflat[:, cs], ALU.mult),
                         reads=[f"ga{kq}", f"gg{b}"], writes=[f"ww{kq}"])
                    for jj, s_ in enumerate(slots):
                        j = grp * G + jj
                        di = dgi[0] % 4
                        dgi[0] += 1
                        P.op("act", lambda e, di=di, j=j: e.activation(dg[di][:, :], ident_b[:, :], AF.Copy, scale=ww[:, j:j + 1]),
                             reads=[f"ww{kq}", "ident_b"], writes=[f"dg{di}"])

                        def mm(e, di=di, s_=s_, j=j):
                            e.matmul(pAcc[:, 0:512], dg[di][:, :], ug[s_][:, D:D + 512], start=(j == 0), stop=(j == 127))
                            return e.matmul(pAcc[:, 512:1024], dg[di][:, :], ug[s_][:, D + 512:2 * D], start=(j == 0), stop=(j == 127))
                        P.op("pe", mm, reads=[f"dg{di}", f"ug{s_}"], writes=["pAcc"])

                for grp in range(ngrp):
                    cs = slice(grp * G, (grp + 1) * G)
                    kq = grp % 4
                    slots = []
                    for jj in range(G):
                        j = grp * G + jj
                        s_ = gather(b, j)
                        slots.append(s_)
                        P.op("dve", lambda e, s_=s_, j=j: e.scalar_tensor_tensor(
                            junkf[:, :], ug[s_][:, 0:D], 1.0, h[:, :], ALU.mult, ALU.mult, accum_out=Aa[:, j:j + 1]),
                            reads=[f"ug{s_}", hk], writes=["junkf", f"Aa{kq}"])
                    P.op("act", lambda e, cs=cs: e.activation(ga[:, cs], Aa[:, cs], AF.Gelu), reads=[f"Aa{kq}"], writes=[f"ga{kq}"])
                    if pend is not None:
                        vside(*pend)
                    pend = (grp, slots)
                    if grp >= 1:
                        k_ = (len(pending_sel) + (ngrp - 1 - grp) - 1) // max(ngrp - 1 - grp, 1) if grp < ngrp - 1 else len(pending_sel)
                        P.replay(pending_sel[:k_])
                        del pending_sel[:k_]
                vside(*pend)
                P.op("dve", lambda e: e.tensor_tensor(acc[:, :], pAcc[:, :], bc_gf[:, :], ALU.mult),
                     reads=["pAcc", "bc_gf"], writes=["acc"])
                P.op("dve", lambda e: e.tensor_tensor(acc[:, :], acc[:, :], xt[:, :], ALU.add),
                     reads=["acc", xk], writes=["acc"])
                P.op("act", lambda e: e.activation(junkb[:, :], acc[:, :], AF.Square, accum_out=st8[:, 3:4]),
                     reads=["acc"], writes=["junkb", "st8d"])
                P.op("act", lambda e: e.activation(st8[:, 4:5], st8[:, 3:4], AF.Sqrt, bias=EPS, scale=1.0 / D),
                     reads=["st8d"], writes=["st8e"])
                P.op("dve", lambda e: e.reciprocal(st8[:, 5:6], st8[:, 4:5]), reads=["st8e"], writes=["st8f"])
                P.op("dve", lambda e: e.scalar_tensor_tensor(yt[:, :], acc[:, :], st8[:, 5:6], bc_fin[:, :], ALU.mult, ALU.mult),
                     reads=["acc", "st8f", "bc_fin"], writes=["yt"])
                P.op("sp", lambda e: e.dma_start(out=y[n * 128:(n + 1) * 128, :], in_=yt[:, :]),
                     reads=["yt"], writes=["y_out"], dma_sem="d_y")

            pending_sel = []
            sel(0)
            for n in range(n_blocks):
                if n + 1 < n_blocks:
                    pending_sel.extend(P.capture(lambda: sel(n + 1)))
                experts(n)
                P.replay(pending_sel)
                del pending_sel[:]
            P.flush()
    return nc


def _rope_tables():
    pos = np.arange(S, dtype=np.float32)
    out = {}
    for dim, name in ((64, "rope64"), (32, "rope32")):
        inv = (1.0 / (10000.0 ** (np.arange(0, dim, 2, dtype=np.float32) / dim))).astype(np.float32)
        ang = pos[:, None] * inv[None, :]
        cos = np.cos(ang).astype(np.float32).T
        sin = np.sin(ang).astype(np.float32).T
        cosf = np.concatenate([cos, cos], axis=0)
        sinf = np.concatenate([-sin, sin], axis=0)
        cosf = np.concatenate([cosf, cosf], axis=0)
        sinf = np.concatenate([sinf, sinf], axis=0)
        out[name] = np.ascontiguousarray(np.stack([cosf, sinf], axis=0))
    return out


def make_in_maps(inputs, n_cores=8):
    g = lambda k: np.ascontiguousarray(np.asarray(inputs[k], dtype=np.float32))
    rt = _rope_tables()
    shared = {
        "w_ada": g("w_ada")[0], "b_ada": g("b_ada")[0][None, :], "w_in": g("w_in")[0],
        "swa_sink": g("swa_sink")[0][None, :], "mla_q_norm": g("mla_q_norm")[0][None, :],
        "w_q_up": g("w_mla_q_up")[0], "mla_kv_norm": g("mla_kv_norm")[0][None, :],
        "w_kv_up": g("w_mla_kv_up")[0],
        "out_norm": np.ascontiguousarray(np.concatenate([g("out_norm_swa")[0], g("out_norm_mla")[0]])[None, :]),
        "w_out": g("w_out")[0], "w_pq": g("w_peer_query")[0],
        "sub_keys": np.ascontiguousarray(g("peer_sub_keys")[0].reshape(16, 128, 128)),
        "exp_u": g("peer_expert_u")[0], "exp_v": g("peer_expert_v")[0],
        "final_norm": g("final_norm")[None, :], "rope64": rt["rope64"], "rope32": rt["rope32"],
    }
    xs = g("x")
    cs = g("c")
    maps = []
    for i in range(n_cores):
        m = dict(shared)
        m["x"] = np.ascontiguousarray(xs[i])
        m["c"] = np.ascontiguousarray(cs[i].reshape(8, 128))
        maps.append(m)
    return maps


def kernel(**inputs):
    nc = build()
    in_maps = make_in_maps(inputs, 8)
    res = run_bass_kernel_spmd(nc, in_maps, core_ids=list(range(8)))
    return np.stack([np.asarray(r["y"]).reshape(S, D) for r in res.results], axis=0).astype(np.float32)
```
